# Optimizing a Trainium2 kernel written in Bass

```python
import math
import jax, jax.numpy as jnp
from jax import lax
import numpy as np

D_MODEL = 2048
BATCH = 4
SEQ = 2048
DEPTH = 2

N_MIXERS = 2
EPS = 1e-6

DIL_PAIRS = ((128, 1), (512, 4), (2048, 16))
N_DIL_GROUPS = 3
DSA_HEADS_PER_GROUP = 8
DSA_HEAD_DIM = 128
DSA_WIDTH = DSA_HEADS_PER_GROUP * DSA_HEAD_DIM
DSA_PROJ = N_DIL_GROUPS * 3 * DSA_WIDTH
BAND = 128

MLA_HEADS = 16
MLA_Q_LORA = 512
MLA_KV_LORA = 512
MLA_NOPE = 128
MLA_ROPE = 64
MLA_V = 128
MLA_QK = MLA_NOPE + MLA_ROPE
MLA_PROJ = MLA_Q_LORA + MLA_KV_LORA + MLA_ROPE
ROPE_THETA = 10000.0
ATTN_BLOCK = 128

N_GROUPS = 4
EXPERTS_PER_GROUP = 16
N_EXPERTS = N_GROUPS * EXPERTS_PER_GROUP
TOP_K = 2
D_EXPERT = 768
EXPERT_BLOCK = 128

kernel_name = "hybrid_dilated_mla_hmoe_adaln"


def rmsnorm(x, g):
    xf = x.astype(jnp.float32)
    y = xf * lax.rsqrt(jnp.mean(xf * xf, axis=-1, keepdims=True) + EPS)
    return (y * g.astype(jnp.float32)).astype(x.dtype)


def modulate(xn, shift, scale):
    return xn * (1 + scale[:, None, :]) + shift[:, None, :]


def alibi_slopes(n):
    return 2.0 ** (-8.0 * (jnp.arange(n, dtype=jnp.float32) + 1.0) / n)


def rope(x):
    S, R = x.shape[1], x.shape[-1]
    half = R // 2
    inv = ROPE_THETA ** (-jnp.arange(half, dtype=jnp.float32) / half)
    ang = jnp.arange(S, dtype=jnp.float32)[:, None] * inv[None, :]
    cos = jnp.cos(ang)[None, :, None, :]
    sin = jnp.sin(ang)[None, :, None, :]
    x1 = x[..., :half].astype(jnp.float32)
    x2 = x[..., half:].astype(jnp.float32)
    return jnp.concatenate([x1 * cos - x2 * sin, x1 * sin + x2 * cos], axis=-1).astype(x.dtype)


def dilated_group(q, k, v, window, dilation, slopes):
    B, S, H, E = q.shape
    steps = window // dilation
    L = S // dilation
    nb = -(-L // BAND)
    Lp = nb * BAND

    def to_sub(t):
        t = t.reshape(B, L, dilation, H, E).transpose(0, 2, 1, 3, 4)
        return jnp.pad(t, ((0, 0), (0, 0), (0, Lp - L), (0, 0), (0, 0)))

    def band(t):
        prev = jnp.pad(t, ((0, 0), (0, 0), (BAND, 0), (0, 0), (0, 0)))[:, :, :Lp]
        prev = prev.reshape(B, dilation, nb, BAND, H, E)
        cur = t.reshape(B, dilation, nb, BAND, H, E)
        return jnp.concatenate([prev, cur], axis=3)

    qb = to_sub(q).reshape(B, dilation, nb, BAND, H, E)
    kb = band(to_sub(k))
    vb = band(to_sub(v))
    scale = 1.0 / math.sqrt(E)
    s = jnp.einsum('brnqhe,brnkhe->brnhqk', qb, kb).astype(jnp.float32) * scale
    qi = jnp.arange(BAND)[:, None]
    kj = jnp.arange(2 * BAND)[None, :]
    delta = qi + BAND - kj
    key_m = jnp.arange(nb)[:, None, None] * BAND - BAND + kj[None]
    valid = (delta >= 0)[None] & (delta <= steps)[None] & (key_m >= 0)
    dist = (delta * dilation).astype(jnp.float32)
    bias = -slopes.astype(jnp.float32)[:, None, None] * dist[None]
    s = jnp.where(valid[None, None, :, None], s + bias[None, None, None], -jnp.inf)
    lse = jax.nn.logsumexp(s, axis=-1)
    p = jnp.exp(s - lse[..., None])
    o = jnp.einsum('brnhqk,brnkhe->brnqhe', p.astype(v.dtype), vb)
    o = o.reshape(B, dilation, Lp, H, E)[:, :, :L].transpose(0, 2, 1, 3, 4).reshape(B, S, H, E)
    lse = lse.transpose(0, 1, 2, 4, 3).reshape(B, dilation, Lp, H)[:, :, :L]
    lse = lse.transpose(0, 2, 1, 3).reshape(B, S, H)
    return o, lse


def dilated_attention(h, w_in, q_gain, k_gain, w_out):
    B, S, _ = h.shape
    qkv = (h @ w_in).reshape(B, S, N_DIL_GROUPS, 3, DSA_HEADS_PER_GROUP, DSA_HEAD_DIM)
    slopes = alibi_slopes(N_DIL_GROUPS * DSA_HEADS_PER_GROUP).reshape(
        DSA_HEADS_PER_GROUP, N_DIL_GROUPS).T
    outs, lses = [], []
    for g, (window, dilation) in enumerate(DIL_PAIRS):
        q = rmsnorm(qkv[:, :, g, 0], q_gain)
        k = rmsnorm(qkv[:, :, g, 1], k_gain)
        v = qkv[:, :, g, 2]
        o, lse = dilated_group(q, k, v, window, dilation, slopes[g])
        outs.append(o)
        lses.append(lse)
    alpha = jax.nn.softmax(jnp.stack(lses), axis=0)
    o = jnp.sum(alpha[..., None].astype(h.dtype) * jnp.stack(outs), axis=0)
    return o.reshape(B, S, DSA_WIDTH) @ w_out


def causal_block_attention(q, k, v):
    B, S, H, E = q.shape
    nq = S // ATTN_BLOCK
    scale = 1.0 / math.sqrt(E)
    qb = q.reshape(B, nq, ATTN_BLOCK, H, E).transpose(1, 0, 2, 3, 4)
    kpos = jnp.arange(S)

    def step(args):
        qblk, start = args
        s = jnp.einsum('bqhe,bkhe->bhqk', qblk, k).astype(jnp.float32) * scale
        qpos = start + jnp.arange(ATTN_BLOCK)
        s = jnp.where(kpos[None, :] <= qpos[:, None], s, -jnp.inf)
        p = jax.nn.softmax(s, axis=-1)
        return jnp.einsum('bhqk,bkhv->bqhv', p.astype(v.dtype), v)

    o = lax.map(step, (qb, jnp.arange(nq) * ATTN_BLOCK))
    return o.transpose(1, 0, 2, 3, 4).reshape(B, S, H, v.shape[-1])


def mla_attention(h, w_in, cq_gain, ckv_gain, w_q_up, w_kv_up, q_gain, k_gain, w_out):
    B, S, _ = h.shape
    proj = h @ w_in
    c_q = rmsnorm(proj[..., :MLA_Q_LORA], cq_gain)
    c_kv = rmsnorm(proj[..., MLA_Q_LORA:MLA_Q_LORA + MLA_KV_LORA], ckv_gain)
    k_pe = proj[..., MLA_Q_LORA + MLA_KV_LORA:]
    q = (c_q @ w_q_up).reshape(B, S, MLA_HEADS, MLA_QK)
    kv = (c_kv @ w_kv_up).reshape(B, S, MLA_HEADS, MLA_NOPE + MLA_V)
    v = kv[..., MLA_NOPE:]
    k = jnp.concatenate(
        [kv[..., :MLA_NOPE], jnp.broadcast_to(k_pe[:, :, None, :], (B, S, MLA_HEADS, MLA_ROPE))],
        axis=-1)
    q = rmsnorm(q, q_gain)
    k = rmsnorm(k, k_gain)
    q = jnp.concatenate([q[..., :MLA_NOPE], rope(q[..., MLA_NOPE:])], axis=-1)
    k = jnp.concatenate([k[..., :MLA_NOPE], rope(k[..., MLA_NOPE:])], axis=-1)
    o = causal_block_attention(q, k, v)
    return o.reshape(B, S, MLA_HEADS * MLA_V) @ w_out


def grouped_expert_ffn(xf, expert_id, weight, w_gate, w_up, w_down):
    T, D = xf.shape
    A = T * TOP_K
    e_flat = expert_id.reshape(A)
    w_flat = weight.reshape(A)
    tok_flat = jnp.arange(A) // TOP_K
    order = jnp.argsort(e_flat)
    e_sorted = e_flat[order]
    counts = jnp.bincount(e_flat, length=N_EXPERTS)
    padded = (counts + EXPERT_BLOCK - 1) // EXPERT_BLOCK * EXPERT_BLOCK
    starts = jnp.cumsum(counts) - counts
    pends = jnp.cumsum(padded)
    pstarts = pends - padded
    dest = pstarts[e_sorted] + jnp.arange(A) - starts[e_sorted]
    n_blocks = (A + N_EXPERTS * (EXPERT_BLOCK - 1) + EXPERT_BLOCK - 1) // EXPERT_BLOCK
    P = n_blocks * EXPERT_BLOCK
    row_tok = jnp.zeros((P,), jnp.int32).at[dest].set(tok_flat[order].astype(jnp.int32))
    row_w = jnp.zeros((P,), jnp.float32).at[dest].set(w_flat[order])
    blk_exp = jnp.minimum(
        jnp.searchsorted(pends, jnp.arange(n_blocks) * EXPERT_BLOCK, side='right'),
        N_EXPERTS - 1)
    xs = xf[row_tok].reshape(n_blocks, EXPERT_BLOCK, D)

    def expert_block(args):
        xb, e = args
        hid = jax.nn.silu(xb @ w_gate[e]) * (xb @ w_up[e])
        return hid @ w_down[e]

    ys = lax.map(expert_block, (xs, blk_exp)).reshape(P, D)
    ys = ys * row_w[:, None].astype(ys.dtype)
    return jnp.zeros((T, D), ys.dtype).at[row_tok].add(ys)


def hier_moe(h, w_rg, b_rg, w_re, b_re, w_gate, w_up, w_down):
    B, S, D = h.shape
    T = B * S
    xf = h.reshape(T, D)
    g_logits = (xf @ w_rg).astype(jnp.float32) + b_rg.astype(jnp.float32)
    g_probs = jax.nn.softmax(g_logits, axis=-1)
    _, g_idx = lax.top_k(g_logits, 1)
    g_p = jnp.take_along_axis(g_probs, g_idx, axis=1)[:, 0]
    e_logits = ((xf @ w_re).astype(jnp.float32) + b_re.astype(jnp.float32)).reshape(
        T, N_GROUPS, EXPERTS_PER_GROUP)
    e_in_group = jnp.take_along_axis(e_logits, g_idx[:, :, None], axis=1)[:, 0]
    top_v, top_i = lax.top_k(e_in_group, TOP_K)
    w = jax.nn.softmax(top_v, axis=-1) * g_p[:, None]
    expert_id = g_idx * EXPERTS_PER_GROUP + top_i
    return grouped_expert_ffn(xf, expert_id, w, w_gate, w_up, w_down).reshape(B, S, D)


def setup_inputs(seed: int = 0) -> dict:
    key = jax.random.key(seed)
    ks = iter(jax.random.split(key, 40))
    n_a = len(range(0, DEPTH, N_MIXERS))
    n_b = len(range(1, DEPTH, N_MIXERS))
    D = D_MODEL

    def nrm(shape, scale):
        return jax.random.normal(next(ks), shape, jnp.float32) * scale

    def gain(shape):
        return 1.0 + nrm(shape, 0.02)

    return {
        "x": nrm((BATCH, SEQ, D), 1.0),
        "c": nrm((BATCH, D), 1.0),
        "ada_w": nrm((DEPTH, D, 6 * D), 0.5 * D ** -0.5),
        "ada_b": nrm((DEPTH, 6 * D), 0.02),
        "norm1_g": gain((DEPTH, D)),
        "norm2_g": gain((DEPTH, D)),
        "dsa_w_in": nrm((n_a, D, DSA_PROJ), D ** -0.5),
        "dsa_q_gain": gain((n_a, DSA_HEAD_DIM)),
        "dsa_k_gain": gain((n_a, DSA_HEAD_DIM)),
        "dsa_w_out": nrm((n_a, DSA_WIDTH, D), DSA_WIDTH ** -0.5),
        "mla_w_in": nrm((n_b, D, MLA_PROJ), D ** -0.5),
        "mla_cq_gain": gain((n_b, MLA_Q_LORA)),
        "mla_ckv_gain": gain((n_b, MLA_KV_LORA)),
        "mla_w_q_up": nrm((n_b, MLA_Q_LORA, MLA_HEADS * MLA_QK), MLA_Q_LORA ** -0.5),
        "mla_w_kv_up": nrm((n_b, MLA_KV_LORA, MLA_HEADS * (MLA_NOPE + MLA_V)), MLA_KV_LORA ** -0.5),
        "mla_q_gain": gain((n_b, MLA_QK)),
        "mla_k_gain": gain((n_b, MLA_QK)),
        "mla_w_out": nrm((n_b, MLA_HEADS * MLA_V, D), (MLA_HEADS * MLA_V) ** -0.5),
        "router_group_w": nrm((DEPTH, D, N_GROUPS), D ** -0.5),
        "router_group_b": nrm((DEPTH, N_GROUPS), 0.01),
        "router_expert_w": nrm((DEPTH, D, N_EXPERTS), D ** -0.5),
        "router_expert_b": nrm((DEPTH, N_EXPERTS), 0.01),
        "expert_w_gate": nrm((DEPTH, N_EXPERTS, D, D_EXPERT), D ** -0.5),
        "expert_w_up": nrm((DEPTH, N_EXPERTS, D, D_EXPERT), D ** -0.5),
        "expert_w_down": nrm((DEPTH, N_EXPERTS, D_EXPERT, D), D_EXPERT ** -0.5),
    }


def reference(x, c, ada_w, ada_b, norm1_g, norm2_g, dsa_w_in, dsa_q_gain, dsa_k_gain,
              dsa_w_out, mla_w_in, mla_cq_gain, mla_ckv_gain, mla_w_q_up, mla_w_kv_up,
              mla_q_gain, mla_k_gain, mla_w_out, router_group_w, router_group_b,
              router_expert_w, router_expert_b, expert_w_gate, expert_w_up, expert_w_down):
    c_act = jax.nn.silu(c)
    for i in range(DEPTH):
        mod = c_act @ ada_w[i] + ada_b[i]
        shift1, scale1, gate1, shift2, scale2, gate2 = jnp.split(mod, 6, axis=-1)
        h = modulate(rmsnorm(x, norm1_g[i]), shift1, scale1)
        j = i // N_MIXERS
        if i % N_MIXERS == 0:
            y = dilated_attention(h, dsa_w_in[j], dsa_q_gain[j], dsa_k_gain[j], dsa_w_out[j])
        else:
            y = mla_attention(h, mla_w_in[j], mla_cq_gain[j], mla_ckv_gain[j], mla_w_q_up[j],
                              mla_w_kv_up[j], mla_q_gain[j], mla_k_gain[j], mla_w_out[j])
        x = x + gate1[:, None, :] * y
        h = modulate(rmsnorm(x, norm2_g[i]), shift2, scale2)
        y = hier_moe(h, router_group_w[i], router_group_b[i], router_expert_w[i],
                     router_expert_b[i], expert_w_gate[i], expert_w_up[i], expert_w_down[i])
        x = x + gate2[:, None, :] * y
    return x
```

```python
import numpy as np
import concourse.bass as bass
import concourse.mybir as mybir
from concourse.bass_utils import run_bass_kernel_spmd

F32 = mybir.dt.float32
BF16 = mybir.dt.bfloat16
ALU = mybir.AluOpType
AF = mybir.ActivationFunctionType
AX = mybir.AxisListType

D = 2048
NTOK = 1024
NCH = 8
EPS = 1e-6
NEXP = 64
DEXP = 768
CAP = 128
EB_ = 4
SLOT_ELEMS = 8192
NSLOT = 3


class Dep:
    __slots__ = ("w", "r")

    def __init__(self):
        self.w = None
        self.r = []


class Eng:
    def __init__(self, name, h, sem):
        self.name, self.h, self.sem = name, h, sem
        self.count = 0
        self.seen = {}

    def wait(self, ev):
        if ev is None:
            return
        sem, val, key = ev
        if key == "pe" and self.name == "pe":
            return
        if self.seen.get(key, 0) >= val:
            return
        self.h.wait_ge(sem, val)
        self.seen[key] = val


class FW:
    def __init__(self, nc):
        self.nc = nc
        self.engs = []
        for name, h in (("pe", nc.tensor), ("act", nc.scalar), ("dve", nc.vector),
                        ("pool", nc.gpsimd), ("sp", nc.sync)):
            e = Eng(name, h, nc.alloc_semaphore("sem_" + name))
            setattr(self, name, e)
            self.engs.append(e)
        self.dsems = []
        self.off = nc.sbuf_base
        self.top = nc.sbuf_top
        self.nt = 0

    def sb(self, shape, dtype, name=None):
        nbytes = int(np.prod(shape[1:])) * (2 if dtype == BF16 else 4)
        nbytes = (nbytes + 63) // 64 * 64
        off = (self.off + 63) // 64 * 64
        assert off + nbytes <= self.top, ("SBUF overflow", name, off + nbytes - self.top)
        self.nt += 1
        t = self.nc.alloc_sbuf_tensor_at("%s_%d" % (name or "t", self.nt), list(shape), dtype, offset=off)
        self.off = off + nbytes
        self.lastoff = off
        return t

    def mark(self):
        return self.off

    def release(self, m):
        self.off = m

    def _stamp(self, e, ins, reads, writes):
        e.count += 1
        assert e.count < 60000, e.name
        ins.then_inc(e.sem, 1)
        ev = (e.sem, e.count, e.name)
        for d in writes:
            d.w = ev
            d.r = []
        for d in reads:
            d.r.append(ev)

    def _waits(self, e, reads, writes):
        for d in reads:
            e.wait(d.w)
        for d in writes:
            e.wait(d.w)
            for ev in d.r:
                e.wait(ev)

    def op(self, e, fn, reads=(), writes=()):
        self._waits(e, reads, writes)
        ins = fn(e.h)
        self._stamp(e, ins, reads, writes)

    def mm(self, out_ap, pairs, reads, write, start=True, stop=True):
        e = self.pe
        self._waits(e, reads, [write])
        n = len(pairs)
        ins = None
        for i, (l, r) in enumerate(pairs):
            ins = e.h.matmul(out_ap, lhsT=l, rhs=r, start=(start and i == 0), stop=(stop and i == n - 1))
        self._stamp(e, ins, reads, [write])

    def tr(self, out_ap, in_ap, ident_ap, reads, write):
        e = self.pe
        self._waits(e, reads, [write])
        ins = e.h.transpose(out_ap, in_ap, ident_ap)
        self._stamp(e, ins, reads, [write])

    def new_dsem(self, name="d"):
        s = [self.nc.alloc_semaphore("ds_%s_%d" % (name, len(self.dsems))), 0, "ds%d" % len(self.dsems)]
        self.dsems.append(s)
        return s

    def dma(self, q, dsem, out_ap, in_ap, reads=(), writes=(), nowait=False):
        if not nowait:
            self._waits(q, reads, writes)
        ins = q.h.dma_start(out=out_ap, in_=in_ap)
        dsem[1] += 16
        ins.then_inc(dsem[0], 16)
        ev = (dsem[0], dsem[1], dsem[2])
        for d in writes:
            d.w = ev
            d.r = []
        for d in reads:
            d.r.append(ev)
        return ev

    def barrier(self):
        evs = [(e.sem, e.count, e.name) for e in self.engs if e.count > 0]
        evs += [(s[0], s[1], s[2]) for s in self.dsems if s[1] > 0]
        for e in self.engs:
            for ev in evs:
                if ev[2] != e.name:
                    e.wait(ev)


class Stream:
    def __init__(self, fw, slots, units):
        self.fw, self.slots, self.units = fw, slots, units
        self.ni = 0
        self.nu = 0

    def issue(self):
        if self.ni >= len(self.units):
            return
        t, dep, dsem = self.slots[self.ni % len(self.slots)]
        parts = self.units[self.ni](t)
        for j, (o, a) in enumerate(parts):
            self.fw.dma(self.fw.pool, dsem, o, a, writes=[dep], nowait=(j > 0))
        self.ni += 1

    def prime(self):
        for _ in range(len(self.slots)):
            self.issue()

    def use(self):
        s = self.slots[self.nu % len(self.slots)]
        self.nu += 1
        return s[0], s[1]

    def done(self):
        self.issue()


def _consts(fw, dr):
    c = {}
    ds = fw.new_dsem("c")
    c["dep"] = Dep()
    for name, shape, dt in (("ident_f", [128, 128], F32), ("iota_f", [128, 128], F32)):
        t = fw.sb(shape, dt, name)
        fw.dma(fw.sp, ds, t[:], dr[name], writes=[c["dep"]], nowait=True)
        c[name] = t
    for name in ("ident", "ustrict", "ones", "tri"):
        tf = fw.sb([128, 128], F32, name + "_f")
        fw.dma(fw.sp, ds, tf[:], dr[name], writes=[c["dep"]], nowait=True)
        tb = fw.sb([128, 128], BF16, name + "_b")
        c[name + "_f32"] = tf
        c[name] = tb
    for name in ("ident", "ustrict", "ones", "tri"):
        fw.op(fw.dve, lambda h, a=c[name], b=c[name + "_f32"]: h.tensor_copy(out=a[:], in_=b[:]),
              reads=[c["dep"]], writes=[c["dep"]])
    return c


def _adaln(fw, stream, ps, ps_dep, crep, crep_dep, tiles, b_dram, consume, bst):
    for j in tiles:
        bt, bd, bs = bst[j % 2]
        fw.dma(fw.sp, bs, bt[:], b_dram[:, j * 512:(j + 1) * 512], writes=[bd])
        w, wd = stream.use()
        wv = w[:, 0:8192].rearrange("p (k n) -> p k n", k=16)
        fw.mm(ps[:, 0:512], [(crep[:, kc, :], wv[:, kc, :]) for kc in range(16)], reads=[crep_dep, wd], write=ps_dep)
        stream.done()
        consume(j, ps[:, 0:512], bt, bd)


def build_moe(H):
    nc = bass.Bass("TRN2", target_bir_lowering=False)
    fw = FW(nc)
    dt = nc.dram_tensor
    x_in = dt("x_in", [NTOK, D], F32, kind="ExternalInput").ap()
    cT = dt("cT", [128, 16], F32, kind="ExternalInput").ap()
    ada_w = dt("ada_w", [D, 4 * D], F32, kind="ExternalInput").ap()
    ada_b = dt("ada_b", [128, 4 * D], F32, kind="ExternalInput").ap()
    oT_in = dt("oT_in", [H, 128, NTOK], F32, kind="ExternalInput").ap()
    w_out = dt("w_out", [H * 128, D], F32, kind="ExternalInput").ap()
    g_bc = dt("g_bc", [128, D], F32, kind="ExternalInput").ap()
    w_r = dt("w_r", [D, 68], F32, kind="ExternalInput").ap()
    b_r = dt("b_r", [128, 68], F32, kind="ExternalInput").ap()
    wg = dt("wg", [NEXP, D, DEXP], F32, kind="ExternalInput").ap()
    wu = dt("wu", [NEXP, D, DEXP], F32, kind="ExternalInput").ap()
    wd = dt("wd", [NEXP, DEXP, D], F32, kind="ExternalInput").ap()
    cdr = {n: dt(n, [128, 128], F32, kind="ExternalInput").ap()
           for n in ("ident_f", "iota_f", "ident", "ustrict", "ones", "tri")}
    x_out = dt("x_out", [NTOK, D], F32, kind="ExternalOutput").ap()

    pP = nc.alloc_psum_tensor("pP", [128, 512], F32)
    pA = nc.alloc_psum_tensor("pA", [128, 512], F32)
    pB = nc.alloc_psum_tensor("pB", [128, 512], F32)
    pT1 = nc.alloc_psum_tensor("pT1", [128, 1024], BF16)
    pT2 = nc.alloc_psum_tensor("pT2", [128, 1024], BF16)
    pD0 = nc.alloc_psum_tensor("pD0", [128, 512], F32)
    pD1 = nc.alloc_psum_tensor("pD1", [128, 512], F32)
    pC = nc.alloc_psum_tensor("pC", [128, 512], F32)
    dP, dA, dB, dT1, dT2, dD0, dD1, dC = (Dep() for _ in range(8))

    x_res = fw.sb([128, NCH, D], F32, "x_res")
    x_dep = [Dep() for _ in range(NCH)]
    h2 = fw.sb([128, NCH, D], BF16, "h2")
    h2_dep = [Dep() for _ in range(NCH)]
    gate2b = fw.sb([128, D], BF16, "gate2b")
    gate2_dep = Dep()
    ring = [(fw.sb([128, SLOT_ELEMS], BF16, "ring"), Dep(), fw.new_dsem("ring")) for _ in range(NSLOT)]
    C = _consts(fw, cdr)
    A_f = fw.sb([128, NCH, NEXP], F32, "A_f")
    Wt = fw.sb([128, NCH, NEXP], F32, "Wt")
    rankp = fw.sb([128, NCH, NEXP], F32, "rankp")
    rt_dep = Dep()

    units = []

    def ada_unit(j):
        return lambda t, j=j: [(t[:, 0:8192].rearrange("p (k n) -> p k n", k=16),
                                ada_w[:, j * 512:(j + 1) * 512].rearrange("(k p) n -> p k n", p=128))]
    for j in range(4):
        units.append(ada_unit(j))
    for ft in range(4):
        units.append(lambda t, ft=ft: [(t[:, 0:H * 512].rearrange("p (h n) -> p h n", h=H),
                                        w_out[:, ft * 512:(ft + 1) * 512].rearrange("(h p) n -> p h n", p=128))])
    for j in range(4, 16):
        units.append(ada_unit(j))
    for e in range(NEXP):
        for c in range(2):
            for wsrc in (wg, wu):
                units.append(lambda t, e=e, c=c, wsrc=wsrc: [(
                    t[:, 0:6144].rearrange("p (k n) -> p k n", k=16),
                    wsrc[e, :, c * 384:(c + 1) * 384].rearrange("(k p) n -> p k n", p=128))])
        for c in range(2):
            units.append(lambda t, e=e, c=c: [(
                t[:, 0:6144].rearrange("p (k n) -> p k n", k=6),
                wd[e, :, c * 1024:(c + 1) * 1024].rearrange("(k p) n -> p k n", p=128))])
    stream = Stream(fw, ring, units)
    stream.prime()

    xs = fw.new_dsem("x")
    xv = x_in.rearrange("(c p) f -> p c f", p=128)
    for tc in range(NCH):
        fw.dma(fw.sp, xs, x_res[:, tc, :], xv[:, tc, :], writes=[x_dep[tc]], nowait=True)

    crep = fw.sb([128, 16, 128], BF16, "crep")
    cact = fw.sb([128, 16], F32, "cact")
    bst = [(fw.sb([128, 512], F32, "abias"), Dep(), fw.new_dsem("ab")) for _ in range(2)]
    d_crep, d_misc = Dep(), Dep()
    ms = fw.new_dsem("misc")
    fw.dma(fw.sp, ms, cact[:], cT, writes=[d_misc], nowait=True)
    fw.op(fw.act, lambda h: h.activation(out=cact[:], in_=cact[:], func=AF.Silu), reads=[d_misc], writes=[d_misc])
    for kc in range(16):
        fw.op(fw.dve, lambda h, kc=kc: h.tensor_scalar(out=crep[:, kc, :], in0=C["ones_f32"][:], scalar1=cact[:, kc:kc + 1],
                                                        scalar2=None, op0=ALU.mult),
              reads=[d_misc, C["dep"]], writes=[d_crep])
    m0 = fw.mark()

    gt1 = fw.sb([128, D], F32, "gt1")
    d_gt1 = Dep()
    oTc = [(fw.sb([128, H, 128], BF16, "oTc"), Dep(), fw.new_dsem("oTc")) for _ in range(2)]
    tmpb = fw.sb([128, 512], F32, "tmpb")
    d_tmpb = Dep()

    def consumeA(j, ps, bt, bd):
        cs = slice(j * 512, (j + 1) * 512)
        fw.op(fw.dve, lambda h: h.tensor_tensor(out=gt1[:, cs], in0=ps, in1=bt[:], op=ALU.add),
              reads=[dP, bd], writes=[d_gt1])
    _adaln(fw, stream, pP, dP, crep, d_crep, range(4), ada_b, consumeA, bst)
    pDl = [(pD0, dD0), (pD1, dD1)]
    n = 0
    for ft in range(4):
        wo, wod = stream.use()
        wov = wo[:, 0:H * 512].rearrange("p (h n) -> p h n", h=H)
        cs = slice(ft * 512, (ft + 1) * 512)
        for tc in range(NCH):
            ot, otd, ots = oTc[n % 2]
            pd, dd = pDl[n % 2]
            n += 1
            fw.dma(fw.pool, ots, ot[:], oT_in[:, :, tc * 128:(tc + 1) * 128].rearrange("h p t -> p h t"), writes=[otd])
            fw.mm(pd[:, 0:512], [(ot[:, hh, :], wov[:, hh, :]) for hh in range(H)], reads=[otd, wod], write=dd)
            fw.op(fw.dve, lambda h, pd=pd: h.tensor_tensor(out=tmpb[:], in0=pd[:, 0:512], in1=gt1[:, cs], op=ALU.mult),
                  reads=[dd, d_gt1], writes=[d_tmpb])
            fw.op(fw.dve, lambda h, tc=tc: h.tensor_tensor(out=x_res[:, tc, cs], in0=x_res[:, tc, cs], in1=tmpb[:], op=ALU.add),
                  reads=[d_tmpb, x_dep[tc]], writes=[x_dep[tc]])
        stream.done()
    fw.barrier()
    fw.release(m0)

    gs2 = fw.sb([128, D], F32, "gs2")
    sh2 = fw.sb([128, D], F32, "sh2")
    mod_dep = Dep()
    h2f = fw.sb([128, D], F32, "h2f")
    h2fT = fw.sb([128, 16, 128], F32, "h2fT")
    off_h2fT = fw.lastoff
    w_r_sb = fw.sb([128, 16, 68], F32, "w_r_sb")
    b_r_sb = fw.sb([128, 68], F32, "b_r_sb")
    gst = [(fw.sb([128, 512], F32, "gst"), Dep(), fw.new_dsem("gst")) for _ in range(2)]
    small = fw.sb([128, 256], F32, "small")
    d_small, d_h2f, d_h2fT = (Dep() for _ in range(3))
    d_misc2 = Dep()
    fw.dma(fw.sp, ms, w_r_sb[:], w_r.rearrange("(k p) n -> p k n", p=128), writes=[d_misc2], nowait=True)
    fw.dma(fw.sp, ms, b_r_sb[:], b_r, writes=[d_misc2], nowait=True)

    def consume(j, ps, bt, bd):
        j -= 4
        q = j // 4
        cs = slice((j % 4) * 512, (j % 4 + 1) * 512)
        if q == 0:
            fw.op(fw.dve, lambda h: h.tensor_tensor(out=sh2[:, cs], in0=ps, in1=bt[:], op=ALU.add),
                  reads=[dP, bd], writes=[mod_dep])
        elif q == 1:
            gt, gd, gsem = gst[j % 2]
            fw.dma(fw.sp, gsem, gt[:], g_bc[:, cs], writes=[gd])
            fw.op(fw.dve, lambda h: h.tensor_tensor(out=gs2[:, cs], in0=ps, in1=bt[:], op=ALU.add),
                  reads=[dP, bd], writes=[mod_dep])
            fw.op(fw.dve, lambda h: h.scalar_tensor_tensor(out=gs2[:, cs], in0=gs2[:, cs], scalar=1.0, in1=gt[:],
                                                           op0=ALU.add, op1=ALU.mult),
                  reads=[gd, mod_dep], writes=[mod_dep])
        else:
            fw.op(fw.dve, lambda h: h.tensor_tensor(out=gate2b[:, cs], in0=ps, in1=bt[:], op=ALU.add),
                  reads=[dP, bd], writes=[gate2_dep])

    _adaln(fw, stream, pP, dP, crep, d_crep, range(4, 16), ada_b, consume, bst)

    sm = small

    def col(i, n=1):
        return sm[:, i:i + n]
    for tc in range(NCH):
        xc = x_res[:, tc, :]
        fw.op(fw.dve, lambda h: h.memset(sm[:, 0:8], 0.0), reads=[d_small], writes=[d_small])
        fw.op(fw.act, lambda h: h.activation(out=h2f[:], in_=xc, func=AF.Square, accum_out=col(0)),
              reads=[x_dep[tc]], writes=[d_h2f, d_small])
        fw.op(fw.act, lambda h: h.activation(out=col(1), in_=col(0), func=AF.Sqrt, scale=1.0 / D, bias=EPS),
              reads=[d_small], writes=[d_small])
        fw.op(fw.dve, lambda h: h.reciprocal(out=col(1), in_=col(1)), reads=[d_small], writes=[d_small])
        fw.op(fw.dve, lambda h: h.scalar_tensor_tensor(out=h2f[:], in0=xc, scalar=col(1), in1=gs2[:],
                                                       op0=ALU.mult, op1=ALU.mult),
              reads=[x_dep[tc], d_small, mod_dep], writes=[d_h2f])
        fw.op(fw.dve, lambda h: h.tensor_tensor(out=h2f[:], in0=h2f[:], in1=sh2[:], op=ALU.add),
              reads=[d_h2f, mod_dep], writes=[d_h2f])
        fw.op(fw.act, lambda h: h.activation(out=h2[:, tc, :], in_=h2f[:], func=AF.Copy),
              reads=[d_h2f], writes=[h2_dep[tc]])
        for q in range(4):
            for j in range(4):
                kc = q * 4 + j
                fw.tr(pA[:, j * 128:(j + 1) * 128], h2f[:, kc * 128:(kc + 1) * 128], C["ident_f"][:],
                      reads=[d_h2f, C["dep"]], write=dA)
            fw.op(fw.act, lambda h, q=q: h.activation(out=h2fT[:, q * 4:(q + 1) * 4, :],
                                                      in_=pA[:, 0:512].rearrange("p (a b) -> p a b", a=4), func=AF.Copy),
                  reads=[dA], writes=[d_h2fT])
        fw.mm(pB[:, 0:68], [(h2fT[:, kc, :], w_r_sb[:, kc, :]) for kc in range(16)], reads=[d_h2fT, d_misc2], write=dB)
        lg = sm[:, 8:76]
        fw.op(fw.dve, lambda h: h.tensor_tensor(out=lg, in0=pB[:, 0:68], in1=b_r_sb[:], op=ALU.add),
              reads=[dB, d_misc2], writes=[d_small])
        R_ = [d_small]

        def dv(fn):
            fw.op(fw.dve, fn, reads=R_, writes=R_)

        def ac(fn):
            fw.op(fw.act, fn, reads=R_, writes=R_)
        gl = sm[:, 8:12]
        el = sm[:, 12:76]
        m64 = sm[:, 80:144]
        m64b = sm[:, 144:208]
        dv(lambda h: h.reduce_max(out=col(2), in_=gl, axis=AX.X))
        dv(lambda h: h.tensor_scalar(out=sm[:, 76:80], in0=gl, scalar1=col(2), scalar2=None, op0=ALU.is_equal))
        dv(lambda h: h.tensor_scalar(out=col(3), in0=col(2), scalar1=-1.0, scalar2=None, op0=ALU.mult))
        ac(lambda h: h.activation(out=sm[:, 208:212], in_=gl, func=AF.Exp, bias=col(3), scale=1.0, accum_out=col(4)))
        dv(lambda h: h.reciprocal(out=col(4), in_=col(4)))
        dv(lambda h: h.tensor_scalar(out=sm[:, 76:80], in0=sm[:, 76:80], scalar1=1e9, scalar2=-1e9,
                                     op0=ALU.mult, op1=ALU.add))
        for g in range(4):
            dv(lambda h, g=g: h.tensor_scalar(out=m64[:, g * 16:(g + 1) * 16], in0=el[:, g * 16:(g + 1) * 16],
                                              scalar1=sm[:, 76 + g:77 + g], scalar2=None, op0=ALU.add))
        A1 = A_f[:, tc, :]
        W1 = Wt[:, tc, :]
        oh2 = sm[:, 144:208]
        dv(lambda h: h.reduce_max(out=col(5), in_=m64, axis=AX.X))
        fw.op(fw.dve, lambda h: h.tensor_scalar(out=A1, in0=m64, scalar1=col(5), scalar2=None, op0=ALU.is_equal),
              reads=R_, writes=R_ + [rt_dep])
        dv(lambda h: h.scalar_tensor_tensor(out=m64b, in0=A1, scalar=-1e9, in1=m64, op0=ALU.mult, op1=ALU.add))
        dv(lambda h: h.reduce_max(out=col(6), in_=m64b, axis=AX.X))
        dv(lambda h: h.tensor_scalar(out=oh2, in0=m64b, scalar1=col(6), scalar2=None, op0=ALU.is_equal))
        dv(lambda h: h.tensor_tensor(out=col(7), in0=col(6), in1=col(5), op=ALU.subtract))
        ac(lambda h: h.activation(out=col(7), in_=col(7), func=AF.Exp))
        dv(lambda h: h.tensor_scalar(out=col(2), in0=col(7), scalar1=1.0, scalar2=None, op0=ALU.add))
        dv(lambda h: h.reciprocal(out=col(2), in_=col(2)))
        dv(lambda h: h.tensor_tensor(out=col(2), in0=col(2), in1=col(4), op=ALU.mult))
        dv(lambda h: h.tensor_tensor(out=col(3), in0=col(2), in1=col(7), op=ALU.mult))
        fw.op(fw.dve, lambda h: h.tensor_scalar(out=W1, in0=A1, scalar1=col(2), scalar2=None, op0=ALU.mult),
              reads=R_ + [rt_dep], writes=R_ + [rt_dep])
        fw.op(fw.dve, lambda h: h.scalar_tensor_tensor(out=W1, in0=oh2, scalar=col(3), in1=W1, op0=ALU.mult, op1=ALU.add),
              reads=R_ + [rt_dep], writes=R_ + [rt_dep])
        fw.op(fw.dve, lambda h: h.tensor_tensor(out=A1, in0=A1, in1=oh2, op=ALU.add),
              reads=R_ + [rt_dep], writes=R_ + [rt_dep])

    A_b = nc.alloc_sbuf_tensor_at("A_b_alias", [128, NCH, NEXP], BF16, offset=off_h2fT)
    fw.op(fw.dve, lambda h: h.tensor_copy(out=A_b[:], in_=A_f[:]), reads=[rt_dep], writes=[rt_dep, d_h2fT])
    for tc in range(NCH):
        pairs = [(C["ones"][:], A_b[:, t2, :]) for t2 in range(tc)] + [(C["ustrict"][:], A_b[:, tc, :])]
        fw.mm(pB[:, 0:64], pairs, reads=[rt_dep, C["dep"]], write=dB)
        fw.op(fw.dve, lambda h: h.tensor_scalar(out=rankp[:, tc, :], in0=A_f[:, tc, :], scalar1=-1e6, scalar2=1e6,
                                                op0=ALU.mult, op1=ALU.add), reads=[rt_dep], writes=[rt_dep])
        fw.op(fw.dve, lambda h: h.tensor_tensor(out=rankp[:, tc, :], in0=rankp[:, tc, :], in1=pB[:, 0:64], op=ALU.add),
              reads=[rt_dep, dB], writes=[rt_dep])

    fw.barrier()
    fw.release(m0)

    ybuf = fw.sb([128, EB_, D], BF16, "ybuf")
    selwt = fw.sb([128, EB_, NTOK], BF16, "selwt")
    sel = fw.sb([128, NCH, CAP], BF16, "sel")
    selw = fw.sb([128, NCH, CAP], BF16, "selw")
    xgT = fw.sb([128, 16, CAP], BF16, "xgT")
    hid = fw.sb([128, DEXP], BF16, "hid")
    sg = fw.sb([128, 384], F32, "sg")
    hidT = fw.sb([128, 6, CAP], BF16, "hidT")
    d_y = [Dep() for _ in range(EB_)]
    d_swt = [Dep() for _ in range(EB_)]
    d_sel, d_selw, d_xgT, d_hid, d_sg, d_hidT = (Dep() for _ in range(6))
    pD = [(pD0, dD0), (pD1, dD1)]
    iota = C["iota_f"]

    for e in range(NEXP):
        eb = e % EB_
        for tc in range(NCH):
            fw.op(fw.dve, lambda h, tc=tc: h.tensor_scalar(out=sel[:, tc, :], in0=iota[:], scalar1=rankp[:, tc, e:e + 1],
                                                           scalar2=None, op0=ALU.is_equal),
                  reads=[C["dep"], rt_dep], writes=[d_sel])
        for tc in range(NCH):
            fw.op(fw.dve, lambda h, tc=tc: h.tensor_scalar(out=selw[:, tc, :], in0=iota[:], scalar1=rankp[:, tc, e:e + 1],
                                                           scalar2=Wt[:, tc, e:e + 1], op0=ALU.is_equal, op1=ALU.mult),
                  reads=[C["dep"], rt_dep], writes=[d_selw])
        for tc in range(NCH):
            fw.tr(pT1[:, tc * 128:(tc + 1) * 128], selw[:, tc, :], C["ident"][:], reads=[d_selw, C["dep"]], write=dT1)
        fw.op(fw.act, lambda h: h.activation(out=selwt[:, eb, :], in_=pT1[:, :], func=AF.Copy),
              reads=[dT1], writes=[d_swt[eb]])
        for q in range(4):
            for j in range(4):
                fc = q * 4 + j
                fw.mm(pP[:, j * 128:(j + 1) * 128],
                      [(h2[:, tc, fc * 128:(fc + 1) * 128], sel[:, tc, :]) for tc in range(NCH)],
                      reads=h2_dep + [d_sel], write=dP)
            fw.op(fw.act, lambda h, q=q: h.activation(out=xgT[:, q * 4:(q + 1) * 4, :],
                                                      in_=pP[:, 0:512].rearrange("p (a b) -> p a b", a=4), func=AF.Copy),
                  reads=[dP], writes=[d_xgT])
        for c in range(2):
            wgt, wgd = stream.use()
            wgv = wgt[:, 0:6144].rearrange("p (k n) -> p k n", k=16)
            fw.mm(pA[:, 0:384], [(xgT[:, kc, :], wgv[:, kc, :]) for kc in range(16)], reads=[d_xgT, wgd], write=dA)
            stream.done()
            wut, wud = stream.use()
            wuv = wut[:, 0:6144].rearrange("p (k n) -> p k n", k=16)
            fw.mm(pB[:, 0:384], [(xgT[:, kc, :], wuv[:, kc, :]) for kc in range(16)], reads=[d_xgT, wud], write=dB)
            stream.done()
            fw.op(fw.act, lambda h: h.activation(out=sg[:], in_=pA[:, 0:384], func=AF.Silu), reads=[dA], writes=[d_sg])
            fw.op(fw.dve, lambda h, c=c: h.tensor_tensor(out=hid[:, c * 384:(c + 1) * 384], in0=pB[:, 0:384], in1=sg[:],
                                                         op=ALU.mult), reads=[dB, d_sg], writes=[d_hid])
        for j in range(6):
            fw.tr(pT2[:, j * 128:(j + 1) * 128], hid[:, j * 128:(j + 1) * 128], C["ident"][:], reads=[d_hid, C["dep"]], write=dT2)
        fw.op(fw.act, lambda h: h.activation(out=hidT[:], in_=pT2[:, 0:768].rearrange("p (a b) -> p a b", a=6), func=AF.Copy),
              reads=[dT2], writes=[d_hidT])
        for c in range(2):
            wdt, wdd = stream.use()
            wdv = wdt[:, 0:6144].rearrange("p (k n) -> p k n", k=6)
            for j in range(2):
                pd, dd = pD[j]
                fw.mm(pd[:, 0:512], [(hidT[:, kc, :], wdv[:, kc, j * 512:(j + 1) * 512]) for kc in range(6)],
                      reads=[d_hidT, wdd], write=dd)
                cs = slice(c * 1024 + j * 512, c * 1024 + (j + 1) * 512)
                fw.op(fw.dve, lambda h, cs=cs, pd=pd: h.tensor_tensor(out=ybuf[:, eb, cs], in0=pd[:, 0:512], in1=gate2b[:, cs],
                                                                      op=ALU.mult),
                      reads=[dd, gate2_dep], writes=[d_y[eb]])
            stream.done()
        if eb == EB_ - 1:
            for tc in range(NCH):
                for ft in range(4):
                    cs = slice(ft * 512, (ft + 1) * 512)
                    fw.mm(pC[:, 0:512], [(selwt[:, b, tc * 128:(tc + 1) * 128], ybuf[:, b, cs]) for b in range(EB_)],
                          reads=d_swt + d_y, write=dC)
                    fw.op(fw.dve, lambda h, tc=tc, cs=cs: h.tensor_tensor(out=x_res[:, tc, cs], in0=pC[:, 0:512],
                                                                          in1=x_res[:, tc, cs], op=ALU.add),
                          reads=[dC, x_dep[tc]], writes=[x_dep[tc]])

    os_ = fw.new_dsem("out")
    ov = x_out.rearrange("(c p) f -> p c f", p=128)
    ev = None
    for tc in range(NCH):
        ev = fw.dma(fw.sp, os_, ov[:, tc, :], x_res[:, tc, :], reads=[x_dep[tc]])
    fw.sp.wait(ev)
    return nc


def _const_inputs():
    i = np.arange(128)
    return {
        "ident_f": np.eye(128, dtype=np.float32),
        "ident": np.eye(128, dtype=np.float32),
        "iota_f": np.broadcast_to(i[None, :], (128, 128)).astype(np.float32).copy(),
        "ustrict": (i[:, None] < i[None, :]).astype(np.float32),
        "ones": np.ones((128, 128), np.float32),
        "tri": (i[None, :] >= i[:, None]).astype(np.float32),
    }


def _rep(v, n=128):
    return np.ascontiguousarray(np.broadcast_to(np.asarray(v, np.float32)[None, :], (n, v.shape[-1])))


_NC = {}


def run_moe(x_cur, oT_full, w_out, c, ada_w_l, ada_b_l, g2, w_rg, b_rg, w_re, b_re, wg, wu, wd):
    H = oT_full.shape[1]
    key = "moe%d" % H
    if key not in _NC:
        _NC[key] = build_moe(H)
    nc = _NC[key]
    consts = _const_inputs()
    ada_w2 = np.ascontiguousarray(ada_w_l[:, 2 * D:6 * D])
    ada_b2 = _rep(ada_b_l[2 * D:6 * D])
    g_bc = _rep(g2)
    w_r = np.ascontiguousarray(np.concatenate([w_rg, w_re], axis=1))
    b_r = _rep(np.concatenate([b_rg, b_re]))
    w_out = np.ascontiguousarray(w_out)
    wg = np.ascontiguousarray(wg)
    wu = np.ascontiguousarray(wu)
    wd = np.ascontiguousarray(wd)
    in_maps = []
    for core in range(8):
        b, p = core // 2, core % 2
        m = dict(consts)
        m.update({
            "x_in": np.ascontiguousarray(x_cur[b, p * NTOK:(p + 1) * NTOK, :]),
            "cT": np.ascontiguousarray(c[b].reshape(16, 128).T),
            "oT_in": np.ascontiguousarray(oT_full[b, :, :, p * NTOK:(p + 1) * NTOK]),
            "w_out": w_out,
            "ada_w": ada_w2, "ada_b": ada_b2, "g_bc": g_bc, "w_r": w_r, "b_r": b_r,
            "wg": wg, "wu": wu, "wd": wd,
        })
        in_maps.append(m)
    res = run_bass_kernel_spmd(nc, in_maps, core_ids=list(range(8)))
    out = np.empty_like(x_cur)
    for core in range(8):
        b, p = core // 2, core % 2
        out[b, p * NTOK:(p + 1) * NTOK, :] = res.results[core]["x_out"]
    return out


def _attn_prologue(nc, fw, C, stream, x_in, cT, ada_b, g_bc, hT, pP, dP, pT, dT):
    m0 = fw.mark()
    crep = fw.sb([128, 16, 128], BF16, "crep")
    cact = fw.sb([128, 16], F32, "cact")
    bst = [(fw.sb([128, 512], F32, "abias"), Dep(), fw.new_dsem("ab")) for _ in range(2)]
    gst = [(fw.sb([128, 512], F32, "gst"), Dep(), fw.new_dsem("gst")) for _ in range(2)]
    gs1 = fw.sb([128, D], F32, "gs1")
    sh1 = fw.sb([128, D], F32, "sh1")
    hf = fw.sb([128, D], F32, "hf")
    hb = fw.sb([128, D], BF16, "hb")
    small = fw.sb([128, 8], F32, "small")
    xst = [(fw.sb([128, D], F32, "xst"), Dep(), fw.new_dsem("xst")) for _ in range(2)]
    d_crep, d_misc, mod_dep, d_hf, d_hb, d_small = (Dep() for _ in range(6))
    ms = fw.new_dsem("misc")
    fw.dma(fw.sp, ms, cact[:], cT, writes=[d_misc], nowait=True)
    fw.op(fw.act, lambda h: h.activation(out=cact[:], in_=cact[:], func=AF.Silu), reads=[d_misc], writes=[d_misc])
    for kc in range(16):
        fw.op(fw.dve, lambda h, kc=kc: h.tensor_scalar(out=crep[:, kc, :], in0=C["ones_f32"][:], scalar1=cact[:, kc:kc + 1],
                                                        scalar2=None, op0=ALU.mult),
              reads=[d_misc, C["dep"]], writes=[d_crep])

    def consume(j, ps, bt, bd):
        q = j // 4
        cs = slice((j % 4) * 512, (j % 4 + 1) * 512)
        if q == 0:
            fw.op(fw.dve, lambda h: h.tensor_tensor(out=sh1[:, cs], in0=ps, in1=bt[:], op=ALU.add),
                  reads=[dP, bd], writes=[mod_dep])
        else:
            gt, gd, gsem = gst[j % 2]
            fw.dma(fw.sp, gsem, gt[:], g_bc[:, cs], writes=[gd])
            fw.op(fw.dve, lambda h: h.tensor_tensor(out=gs1[:, cs], in0=ps, in1=bt[:], op=ALU.add),
                  reads=[dP, bd], writes=[mod_dep])
            fw.op(fw.dve, lambda h: h.scalar_tensor_tensor(out=gs1[:, cs], in0=gs1[:, cs], scalar=1.0, in1=gt[:],
                                                           op0=ALU.add, op1=ALU.mult),
                  reads=[gd, mod_dep], writes=[mod_dep])
    _adaln(fw, stream, pP, dP, crep, d_crep, range(8), ada_b, consume, bst)

    hT_dep = Dep()
    for uc in range(16):
        xt, xd, xs = xst[uc % 2]
        fw.dma(fw.sp, xs, xt[:], x_in[uc * 128:(uc + 1) * 128, :], writes=[xd])
        fw.op(fw.dve, lambda h: h.memset(small[:, 0:2], 0.0), reads=[d_small], writes=[d_small])
        fw.op(fw.act, lambda h: h.activation(out=hf[:], in_=xt[:], func=AF.Square, accum_out=small[:, 0:1]),
              reads=[xd, d_small], writes=[d_hf, d_small])
        fw.op(fw.act, lambda h: h.activation(out=small[:, 1:2], in_=small[:, 0:1], func=AF.Sqrt, scale=1.0 / D, bias=EPS),
              reads=[d_small], writes=[d_small])
        fw.op(fw.dve, lambda h: h.reciprocal(out=small[:, 1:2], in_=small[:, 1:2]), reads=[d_small], writes=[d_small])
        fw.op(fw.dve, lambda h: h.scalar_tensor_tensor(out=hf[:], in0=xt[:], scalar=small[:, 1:2], in1=gs1[:],
                                                       op0=ALU.mult, op1=ALU.mult),
              reads=[xd, d_small, mod_dep, d_hf], writes=[d_hf])
        fw.op(fw.dve, lambda h: h.tensor_tensor(out=hb[:], in0=hf[:], in1=sh1[:], op=ALU.add),
              reads=[d_hf, mod_dep], writes=[d_hb])
        for q in range(2):
            for j in range(8):
                kc = q * 8 + j
                fw.tr(pT[:, j * 128:(j + 1) * 128], hb[:, kc * 128:(kc + 1) * 128], C["ident"][:],
                      reads=[d_hb, C["dep"]], write=dT)
            fw.op(fw.act, lambda h, q=q: h.activation(out=hT[:, q * 8:(q + 1) * 8, uc * 128:(uc + 1) * 128],
                                                      in_=pT[:, :].rearrange("p (a b) -> p a b", a=8), func=AF.Copy),
                  reads=[dT], writes=[hT_dep])
    fw.barrier()
    fw.release(m0)


S_ = 2048
DIL = (1, 4, 16)


def build_dsa():
    nc = bass.Bass("TRN2", target_bir_lowering=False)
    fw = FW(nc)
    dt = nc.dram_tensor
    x_in = dt("x_in", [S_, D], F32, kind="ExternalInput").ap()
    cT = dt("cT", [128, 16], F32, kind="ExternalInput").ap()
    ada_w = dt("ada_w", [D, 2 * D], F32, kind="ExternalInput").ap()
    ada_b = dt("ada_b", [128, 2 * D], F32, kind="ExternalInput").ap()
    g_bc = dt("g_bc", [128, D], F32, kind="ExternalInput").ap()
    w_perm = dt("w_perm", [4, D, 1152], F32, kind="ExternalInput").ap()
    qk_gain = dt("qk_gain", [128, 2], F32, kind="ExternalInput").ap()
    eb_in = dt("eb", [128, 12 * 256], F32, kind="ExternalInput").ap()
    cdr = {n: dt(n, [128, 128], F32, kind="ExternalInput").ap()
           for n in ("ident_f", "iota_f", "ident", "ustrict", "ones", "tri")}
    oT_out = dt("oT", [4, 128, S_], F32, kind="ExternalOutput").ap()

    pP = nc.alloc_psum_tensor("pP", [128, 512], F32)
    pSS = nc.alloc_psum_tensor("pSS", [128, 512], F32)
    pS0 = nc.alloc_psum_tensor("pS0", [128, 512], F32)
    pS1 = nc.alloc_psum_tensor("pS1", [128, 512], F32)
    pT = nc.alloc_psum_tensor("pT", [128, 1024], BF16)
    pV = nc.alloc_psum_tensor("pV", [128, 512], F32)
    pO = nc.alloc_psum_tensor("pO", [128, 512], F32)
    pDn = nc.alloc_psum_tensor("pDn", [128, 512], F32)
    dP, dSS, dS0, dS1, dT, dV, dO, dDn = (Dep() for _ in range(8))

    hT = fw.sb([128, 16, S_], BF16, "hT")
    ring = [(fw.sb([128, SLOT_ELEMS], BF16, "ring"), Dep(), fw.new_dsem("ring")) for _ in range(NSLOT)]
    C = _consts(fw, cdr)
    eb = fw.sb([128, 12, 2, 128], F32, "eb")
    gains = fw.sb([128, 2], F32, "gains")
    d_c2 = Dep()
    cs2 = fw.new_dsem("c2")
    fw.dma(fw.sp, cs2, eb[:], eb_in.rearrange("p (a v q) -> p a v q", a=12, v=2), writes=[d_c2], nowait=True)
    fw.dma(fw.sp, cs2, gains[:], qk_gain, writes=[d_c2], nowait=True)

    units = []
    for j in range(8):
        units.append(lambda t, j=j: [(t[:, 0:8192].rearrange("p (k n) -> p k n", k=16),
                                      ada_w[:, j * 512:(j + 1) * 512].rearrange("(k p) n -> p k n", p=128))])
    for sl in range(4):
        for g in range(3):
            units.append(lambda t, sl=sl, g=g: [(t[:, 0:6144].rearrange("p (k n) -> p k n", k=16),
                                                 w_perm[sl, :, g * 384:(g + 1) * 384].rearrange("(k p) n -> p k n", p=128))])
    stream = Stream(fw, ring, units)
    stream.prime()

    _attn_prologue(nc, fw, C, stream, x_in, cT, ada_b, g_bc, hT, pP, dP, pT, dT)

    QT = fw.sb([128, S_], BF16, "QT")
    KT = fw.sb([128, S_], BF16, "KT")
    V = fw.sb([128, 16, 128], BF16, "V")
    sq = fw.sb([128, 512], BF16, "sq")
    rstd = fw.sb([128, 512], F32, "rstd")
    expS = [fw.sb([128, 256], F32, "expS") for _ in range(2)]
    PT = [fw.sb([128, 256], BF16, "PT") for _ in range(2)]
    Oacc = fw.sb([128, 2, 1024], F32, "Oacc")
    Dacc = fw.sb([128, 2, 1024], F32, "Dacc")
    oTs = fw.sb([128, 1024], F32, "oTs")
    d_QT, d_KT, d_V, d_sq, d_rstd, d_acc, d_oTs = (Dep() for _ in range(7))
    d_expS = [Dep(), Dep()]
    d_PT = [Dep(), Dep()]
    os_ = fw.new_dsem("out")
    SC = 1.0 / float(np.sqrt(128.0))
    nblk = 0
    last_ev = None

    def flush(g, b, qh):
        d = DIL[g]
        for (ps, dps, acc) in ((pO, dO, Oacc), (pDn, dDn, Dacc)):
            if d == 1:
                ov = acc[:, qh, b * 512:(b + 1) * 512]
                iv = ps[:, 0:512]
            else:
                ov = acc[:, qh, :].rearrange("e (m r) -> e m r", r=d)[:, :, b * d // 2:(b + 1) * d // 2]
                iv = ps[:, 0:512].rearrange("e (r m) -> e m r", r=d // 2)
            if g == 0:
                fw.op(fw.dve, lambda h: h.tensor_copy(out=ov, in_=iv), reads=[dps], writes=[d_acc])
            else:
                fw.op(fw.dve, lambda h: h.tensor_tensor(out=ov, in0=ov, in1=iv, op=ALU.add), reads=[dps, d_acc], writes=[d_acc])

    for sl in range(4):
        for g in range(3):
            d = DIL[g]
            w, wdp = stream.use()
            wv = w[:, 0:6144].rearrange("p (k n) -> p k n", k=16)
            for which, dst, ddst, gcol in ((0, QT, d_QT, 0), (1, KT, d_KT, 1)):
                dview = dst[:, :].rearrange("e (r m) -> e m r", r=d)
                for i in range(4):
                    fw.mm(pP[:, 0:512], [(wv[:, kc, which * 128:(which + 1) * 128], hT[:, kc, i * 512:(i + 1) * 512])
                                         for kc in range(16)], reads=[wdp], write=dP)
                    fw.op(fw.act, lambda h: h.activation(out=sq[:], in_=pP[:, 0:512], func=AF.Square),
                          reads=[dP], writes=[d_sq])
                    fw.mm(pSS[:, 0:512], [(C["ones"][:], sq[:])], reads=[d_sq, C["dep"]], write=dSS)
                    fw.op(fw.act, lambda h: h.activation(out=rstd[:], in_=pSS[:, 0:512], func=AF.Sqrt, scale=1.0 / 128.0, bias=EPS),
                          reads=[dSS], writes=[d_rstd])
                    fw.op(fw.dve, lambda h: h.reciprocal(out=rstd[:], in_=rstd[:]), reads=[d_rstd], writes=[d_rstd])
                    fw.op(fw.dve, lambda h, i=i, dview=dview, gcol=gcol: h.scalar_tensor_tensor(
                        out=dview[:, i * 512 // d:(i + 1) * 512 // d, :],
                        in0=pP[:, 0:512].rearrange("e (m r) -> e m r", r=d), scalar=gains[:, gcol:gcol + 1],
                        in1=rstd[:, :].rearrange("e (m r) -> e m r", r=d), op0=ALU.mult, op1=ALU.mult),
                        reads=[dP, d_rstd, d_c2], writes=[ddst])
            nb = 16 // d
            for vb in range(16):
                r, mb = divmod(vb, nb)
                st = mb * 128 * d + r
                fw.mm(pV[:, (vb % 4) * 128:(vb % 4 + 1) * 128],
                      [(hT[:, kc, st:st + 127 * d + 1:d], wv[:, kc, 256:384]) for kc in range(16)], reads=[wdp], write=dV)
                if vb % 4 == 3:
                    fw.op(fw.act, lambda h, vb=vb: h.activation(out=V[:, vb - 3:vb + 1, :],
                                                                in_=pV[:, 0:512].rearrange("p (a b) -> p a b", a=4), func=AF.Copy),
                          reads=[dV], writes=[d_V])
            stream.done()
            ebi = sl * 3 + g
            for qh in range(2):
                blocks = []
                if g == 0:
                    for qb in range(8):
                        gb = 8 * qh + qb
                        blocks.append((gb * 128, 128, (gb - 1) * 128 if gb > 0 else None, gb * 128, 128, gb - 1, gb, qb * 128, 0))
                elif g == 1:
                    for r in range(4):
                        for j in range(2):
                            mb = 2 * qh + j
                            col = r * 512 + mb * 128
                            blocks.append((col, 128, col - 128 if mb > 0 else None, col, 128, r * 4 + mb - 1, r * 4 + mb,
                                           r * 256 + j * 128, 0))
                else:
                    for r in range(16):
                        nk = 64 if qh == 0 else 128
                        blocks.append((r * 128 + 64 * qh, 64, None, r * 128, nk, None, r, r * 64, 64 * qh))
                for (qcol, nq, pk, ck, nk, vp, vc, ocol, ebq0) in blocks:
                    pS, dS = (pS0, dS0) if nblk % 2 == 0 else (pS1, dS1)
                    eS, deS = expS[nblk % 2], d_expS[nblk % 2]
                    pt, dpt = PT[nblk % 2], d_PT[nblk % 2]
                    nblk += 1
                    qv = QT[:, qcol:qcol + nq]
                    if pk is not None:
                        fw.mm(pS[:, 0:nq], [(KT[:, pk:pk + 128], qv)], reads=[d_QT, d_KT], write=dS)
                    fw.mm(pS[0:nk, 128:128 + nq], [(KT[:, ck:ck + nk], qv)], reads=[d_QT, d_KT], write=dS)
                    if pk is not None:
                        fw.op(fw.act, lambda h: h.activation(out=eS[:, 0:256], in_=pS[:, 0:256], func=AF.Exp, scale=SC),
                              reads=[dS], writes=[deS])
                        fw.op(fw.dve, lambda h: h.tensor_tensor(out=pt[:, 0:256], in0=eS[:, 0:256],
                                                                in1=eb[:, ebi, :, :].rearrange("p v q -> p (v q)"), op=ALU.mult),
                              reads=[deS, d_c2], writes=[dpt])
                        pairs_o = [(V[:, vp, :], pt[:, 0:128]), (V[:, vc, :], pt[:, 128:256])]
                        pairs_d = [(C["ones"][:], pt[:, 0:128]), (C["ones"][:], pt[:, 128:256])]
                    else:
                        fw.op(fw.act, lambda h: h.activation(out=eS[0:nk, 128:128 + nq], in_=pS[0:nk, 128:128 + nq],
                                                             func=AF.Exp, scale=SC), reads=[dS], writes=[deS])
                        fw.op(fw.dve, lambda h: h.tensor_tensor(out=pt[0:nk, 128:128 + nq], in0=eS[0:nk, 128:128 + nq],
                                                                in1=eb[0:nk, ebi, 1, ebq0:ebq0 + nq], op=ALU.mult),
                              reads=[deS, d_c2], writes=[dpt])
                        pairs_o = [(V[0:nk, vc, :], pt[0:nk, 128:128 + nq])]
                        pairs_d = [(C["ones"][0:nk, :], pt[0:nk, 128:128 + nq])]
                    if ocol == 512:
                        flush(g, 0, qh)
                    oc = ocol % 512
                    fw.mm(pO[:, oc:oc + nq], pairs_o, reads=[d_V, dpt], write=dO)
                    fw.mm(pDn[:, oc:oc + nq], pairs_d, reads=[dpt, C["dep"]], write=dDn)
                flush(g, 1, qh)
                if g == 2:
                    fw.op(fw.dve, lambda h: h.reciprocal(out=Dacc[:, qh, :], in_=Dacc[:, qh, :]), reads=[d_acc], writes=[d_acc])
                    fw.op(fw.dve, lambda h: h.tensor_tensor(out=oTs[:], in0=Oacc[:, qh, :], in1=Dacc[:, qh, :], op=ALU.mult),
                          reads=[d_acc], writes=[d_oTs])
                    last_ev = fw.dma(fw.sp, os_, oT_out[sl, :, qh * 1024:(qh + 1) * 1024], oTs[:], reads=[d_oTs])
    fw.sp.wait(last_ev)
    return nc


def _alibi_eb(slots):
    k = np.arange(128)[:, None].astype(np.float64)
    q = np.arange(128)[None, :].astype(np.float64)
    out = np.zeros((128, len(slots) * 3, 2, 128), np.float32)
    for i, s in enumerate(slots):
        for g, d in enumerate(DIL):
            slope = 2.0 ** (-8.0 * (s * 3 + g + 1.0) / 24.0)
            dp = q + 128 - k
            out[:, i * 3 + g, 0, :] = np.where(dp <= 128, np.exp(-slope * d * dp), 0.0)
            dc = q - k
            out[:, i * 3 + g, 1, :] = np.where(dc >= 0, np.exp(-slope * d * np.maximum(dc, 0)), 0.0)
    return out.reshape(128, -1)


def run_dsa(x_cur, c, ada_w_l, ada_b_l, g1, w_in, q_gain, k_gain):
    if "dsa" not in _NC:
        _NC["dsa"] = build_dsa()
    nc = _NC["dsa"]
    consts = _const_inputs()
    ada_w1 = np.ascontiguousarray(ada_w_l[:, 0:2 * D])
    ada_b1 = _rep(ada_b_l[0:2 * D])
    g_bc = _rep(g1)
    wp = np.ascontiguousarray(w_in.reshape(D, 3, 3, 8, 128).transpose(3, 0, 1, 2, 4).reshape(8, D, 1152))
    gains = np.ascontiguousarray(np.stack([q_gain, k_gain], axis=1).astype(np.float32))
    in_maps = []
    for core in range(8):
        b, p = core // 2, core % 2
        m = dict(consts)
        m.update({"x_in": np.ascontiguousarray(x_cur[b]), "cT": np.ascontiguousarray(c[b].reshape(16, 128).T),
                  "ada_w": ada_w1, "ada_b": ada_b1, "g_bc": g_bc,
                  "w_perm": np.ascontiguousarray(wp[4 * p:4 * p + 4]), "qk_gain": gains,
                  "eb": _alibi_eb(range(4 * p, 4 * p + 4))})
        in_maps.append(m)
    res = run_bass_kernel_spmd(nc, in_maps, core_ids=list(range(8)))
    oT = np.empty((4, 8, 128, S_), np.float32)
    for core in range(8):
        b, p = core // 2, core % 2
        oT[b, 4 * p:4 * p + 4] = res.results[core]["oT"]
    return oT


def build_mla():
    nc = bass.Bass("TRN2", target_bir_lowering=False)
    fw = FW(nc)
    dt = nc.dram_tensor
    x_in = dt("x_in", [S_, D], F32, kind="ExternalInput").ap()
    cT = dt("cT", [128, 16], F32, kind="ExternalInput").ap()
    ada_w = dt("ada_w", [D, 2 * D], F32, kind="ExternalInput").ap()
    ada_b = dt("ada_b", [128, 2 * D], F32, kind="ExternalInput").ap()
    g_bc = dt("g_bc", [128, D], F32, kind="ExternalInput").ap()
    w_in = dt("w_in", [D, 1088], F32, kind="ExternalInput").ap()
    lat_gain = dt("lat_gain", [128, 1024], F32, kind="ExternalInput").ap()
    w_q = dt("w_q", [512, 8 * 192], F32, kind="ExternalInput").ap()
    w_kv = dt("w_kv", [512, 8 * 256], F32, kind="ExternalInput").ap()
    hg_in = dt("hg", [128, 4], F32, kind="ExternalInput").ap()
    cs_in = dt("rope_cs", [64, S_], F32, kind="ExternalInput").ap()
    sn_in = dt("rope_sn", [64, S_], F32, kind="ExternalInput").ap()
    cdr = {n: dt(n, [128, 128], F32, kind="ExternalInput").ap()
           for n in ("ident_f", "iota_f", "ident", "ustrict", "ones", "tri")}
    oT_out = dt("oT", [8, 128, S_], F32, kind="ExternalOutput").ap()

    pP = nc.alloc_psum_tensor("pP", [128, 512], F32)
    pSS = nc.alloc_psum_tensor("pSS", [128, 512], F32)
    pS0 = nc.alloc_psum_tensor("pS0", [128, 512], F32)
    pS1 = nc.alloc_psum_tensor("pS1", [128, 512], F32)
    pO0 = nc.alloc_psum_tensor("pO0", [128, 512], F32)
    pO1 = nc.alloc_psum_tensor("pO1", [128, 512], F32)
    pD0 = nc.alloc_psum_tensor("pD0", [128, 512], F32)
    pT = nc.alloc_psum_tensor("pT", [128, 1024], BF16)
    pD1 = pT[:, :].bitcast(F32)
    dP, dSS, dS0, dS1, dO0, dO1, dD0, dT = (Dep() for _ in range(8))
    dD1 = dT

    ring = [(fw.sb([128, SLOT_ELEMS], BF16, "ring"), Dep(), fw.new_dsem("ring")) for _ in range(NSLOT)]
    C = _consts(fw, cdr)
    hg = fw.sb([128, 4], F32, "hg")
    d_c2 = Dep()
    cs2 = fw.new_dsem("c2")
    fw.dma(fw.sp, cs2, hg[:], hg_in, writes=[d_c2], nowait=True)
    fw.top -= 65536
    hT = nc.alloc_sbuf_tensor_at("hT_top", [128, 16, S_], BF16, offset=fw.top)

    units = []
    for j in range(8):
        units.append(lambda t, j=j: [(t[:, 0:8192].rearrange("p (k n) -> p k n", k=16),
                                      ada_w[:, j * 512:(j + 1) * 512].rearrange("(k p) n -> p k n", p=128))])
    for (c0, n) in ((0, 512), (512, 512), (1024, 64)):
        units.append(lambda t, c0=c0, n=n: [(t[:, 0:16 * n].rearrange("p (k n) -> p k n", k=16),
                                             w_in[:, c0:c0 + n].rearrange("(k p) n -> p k n", p=128))])
    for hd in range(8):
        units.append(lambda t, hd=hd: [
            (t[:, 0:768].rearrange("p (k n) -> p k n", k=4), w_q[:, hd * 192:(hd + 1) * 192].rearrange("(k p) n -> p k n", p=128)),
            (t[:, 768:1792].rearrange("p (k n) -> p k n", k=4), w_kv[:, hd * 256:(hd + 1) * 256].rearrange("(k p) n -> p k n", p=128))])
    stream = Stream(fw, ring, units)
    stream.prime()

    _attn_prologue(nc, fw, C, stream, x_in, cT, ada_b, g_bc, hT, pP, dP, pT, dT)

    cs = fw.sb([64, S_], F32, "cs")
    sn = fw.sb([64, S_], F32, "sn")
    fw.dma(fw.sp, cs2, cs[:], cs_in, writes=[d_c2], nowait=True)
    fw.dma(fw.sp, cs2, sn[:], sn_in, writes=[d_c2], nowait=True)
    cqT = fw.sb([128, 4, S_], BF16, "cqT")
    ckvT = fw.sb([128, 4, S_], BF16, "ckvT")
    kraw = fw.sb([64, S_], F32, "kraw")
    sqpe = fw.sb([64, S_], BF16, "sqpe")
    kper = fw.sb([64, S_], F32, "kper")
    ksw = fw.sb([64, S_], F32, "ksw")
    d_cq, d_ckv, d_kraw, d_kpe = (Dep() for _ in range(4))
    m1 = fw.mark()
    lg = fw.sb([128, 1024], F32, "lat_gain")
    fw.dma(fw.sp, cs2, lg[:], lat_gain, writes=[d_c2], nowait=True)
    tmpn = fw.sb([128, 512], BF16, "tmpn")
    junk = fw.sb([128, 512], F32, "junk")
    ktok = fw.sb([128, 64], F32, "ktok")
    small = fw.sb([128, 8], F32, "small")
    d_tmpn, d_junk, d_ktok, d_small = (Dep() for _ in range(4))
    wq_t, wq_d = stream.use()
    wkv_t, wkv_d = stream.use()
    wpe_t, wpe_d = stream.use()
    wq_v = wq_t[:, 0:8192].rearrange("p (k n) -> p k n", k=16)
    wkv_v = wkv_t[:, 0:8192].rearrange("p (k n) -> p k n", k=16)
    wpe_v = wpe_t[:, 0:1024].rearrange("p (k n) -> p k n", k=16)
    for uc in range(16):
        tok = slice(uc * 128, (uc + 1) * 128)
        for (wv_, wd_, dstT, ddst, goff) in ((wq_v, wq_d, cqT, d_cq, 0), (wkv_v, wkv_d, ckvT, d_ckv, 512)):
            fw.mm(pP[:, 0:512], [(hT[:, kc, tok], wv_[:, kc, :]) for kc in range(16)], reads=[wd_], write=dP)
            fw.op(fw.dve, lambda h: h.memset(small[:, 0:2], 0.0), reads=[d_small], writes=[d_small])
            fw.op(fw.act, lambda h: h.activation(out=junk[:], in_=pP[:, 0:512], func=AF.Square, accum_out=small[:, 0:1]),
                  reads=[dP, d_small], writes=[d_junk, d_small])
            fw.op(fw.act, lambda h: h.activation(out=small[:, 1:2], in_=small[:, 0:1], func=AF.Sqrt, scale=1.0 / 512.0, bias=EPS),
                  reads=[d_small], writes=[d_small])
            fw.op(fw.dve, lambda h: h.reciprocal(out=small[:, 1:2], in_=small[:, 1:2]), reads=[d_small], writes=[d_small])
            fw.op(fw.dve, lambda h, goff=goff: h.scalar_tensor_tensor(out=tmpn[:], in0=pP[:, 0:512], scalar=small[:, 1:2],
                                                                      in1=lg[:, goff:goff + 512], op0=ALU.mult, op1=ALU.mult),
                  reads=[dP, d_small, d_c2], writes=[d_tmpn])
            for j in range(4):
                fw.tr(pT[:, j * 128:(j + 1) * 128], tmpn[:, j * 128:(j + 1) * 128], C["ident"][:], reads=[d_tmpn, C["dep"]], write=dT)
            fw.op(fw.act, lambda h, dstT=dstT: h.activation(out=dstT[:, :, tok], in_=pT[:, 0:512].rearrange("p (a b) -> p a b", a=4),
                                                            func=AF.Copy), reads=[dT], writes=[ddst])
        if uc % 4 == 0 and uc > 0:
            pass
        fw.mm(pS0[:, 0:64], [(hT[:, kc, tok], wpe_v[:, kc, :]) for kc in range(16)], reads=[wpe_d], write=dS0)
        fw.op(fw.act, lambda h: h.activation(out=ktok[:], in_=pS0[:, 0:64], func=AF.Copy), reads=[dS0], writes=[d_ktok])
        fw.tr(pSS[0:64, 0:128], ktok[:, 0:64], C["ident_f"][:], reads=[d_ktok, C["dep"]], write=dSS)
        fw.op(fw.act, lambda h: h.activation(out=kraw[:, tok], in_=pSS[0:64, 0:128], func=AF.Copy), reads=[dSS], writes=[d_kraw])
    stream.done()
    stream.done()
    stream.done()
    fw.op(fw.act, lambda h: h.activation(out=sqpe[:], in_=kraw[:], func=AF.Square), reads=[d_kraw], writes=[d_kpe])
    fw.op(fw.dve, lambda h: h.tensor_scalar(out=kper[:], in0=kraw[:], scalar1=hg[0:64, 3:4], scalar2=None, op0=ALU.mult),
          reads=[d_kraw, d_c2], writes=[d_kpe])
    fw.op(fw.dve, lambda h: h.tensor_copy(out=ksw[0:32, :], in_=kper[32:64, :]), reads=[d_kpe], writes=[d_kpe])
    fw.op(fw.dve, lambda h: h.tensor_copy(out=ksw[32:64, :], in_=kper[0:32, :]), reads=[d_kpe], writes=[d_kpe])
    fw.op(fw.dve, lambda h: h.tensor_tensor(out=kper[:], in0=kper[:], in1=cs[:], op=ALU.mult), reads=[d_kpe, d_c2], writes=[d_kpe])
    fw.op(fw.dve, lambda h: h.tensor_tensor(out=ksw[:], in0=ksw[:], in1=sn[:], op=ALU.mult), reads=[d_kpe, d_c2], writes=[d_kpe])
    fw.op(fw.dve, lambda h: h.tensor_tensor(out=kper[:], in0=kper[:], in1=ksw[:], op=ALU.add), reads=[d_kpe], writes=[d_kpe])
    fw.barrier()
    fw.release(m1)
    fw.top += 65536

    QTn = fw.sb([128, S_], BF16, "QTn")
    QTr = fw.sb([64, S_], BF16, "QTr")
    KTn = fw.sb([128, S_], BF16, "KTn")
    KTr = fw.sb([64, S_], BF16, "KTr")
    V = fw.sb([128, 16, 128], BF16, "V")
    sqn = fw.sb([128, 512], BF16, "sqn")
    sqr = fw.sb([64, 512], BF16, "sqr")
    rstd = fw.sb([128, 512], F32, "rstd")
    qg = fw.sb([64, 512], F32, "qg")
    qsw = fw.sb([64, 512], F32, "qsw")
    PT = [fw.sb([128, 512], BF16, "PT") for _ in range(2)]
    rden = fw.sb([128, 1024], F32, "rden")
    oTs = fw.sb([128, 1024], F32, "oTs")
    d_QT, d_KT, d_V, d_sq, d_rstd, d_qg, d_rden, d_oTs = (Dep() for _ in range(8))
    d_PT = [Dep(), Dep()]
    os_ = fw.new_dsem("out")
    SCm = 1.0 / float(np.sqrt(192.0))
    ntile = 0
    last_ev = None
    pOb = [(pO0, dO0), (pO1, dO1)]
    pDb = [(pD0, dD0), (pD1, dD1)]
    pSb = [(pS0, dS0), (pS1, dS1)]

    for hd in range(8):
        w, wdp = stream.use()
        wqv = w[:, 0:768].rearrange("p (k n) -> p k n", k=4)
        wkvv = w[:, 768:1792].rearrange("p (k n) -> p k n", k=4)
        for i in range(4):
            cols = slice(i * 512, (i + 1) * 512)
            fw.mm(pP[:, 0:512], [(wqv[:, j, 0:128], cqT[:, j, cols]) for j in range(4)], reads=[wdp, d_cq], write=dP)
            fw.mm(pS1[0:64, 0:512], [(wqv[:, j, 128:192], cqT[:, j, cols]) for j in range(4)], reads=[wdp, d_cq], write=dS1)
            fw.op(fw.act, lambda h: h.activation(out=sqn[:], in_=pP[:, 0:512], func=AF.Square), reads=[dP], writes=[d_sq])
            fw.op(fw.act, lambda h: h.activation(out=sqr[:], in_=pS1[0:64, 0:512], func=AF.Square), reads=[dS1], writes=[d_sq])
            fw.mm(pSS[:, 0:512], [(C["ones"][:], sqn[:]), (C["ones"][0:64, :], sqr[:])], reads=[d_sq, C["dep"]], write=dSS)
            fw.op(fw.act, lambda h: h.activation(out=rstd[:], in_=pSS[:, 0:512], func=AF.Sqrt, scale=1.0 / 192.0, bias=EPS),
                  reads=[dSS], writes=[d_rstd])
            fw.op(fw.dve, lambda h: h.reciprocal(out=rstd[:], in_=rstd[:]), reads=[d_rstd], writes=[d_rstd])
            fw.op(fw.dve, lambda h: h.scalar_tensor_tensor(out=QTn[:, cols], in0=pP[:, 0:512], scalar=hg[:, 0:1], in1=rstd[:],
                                                           op0=ALU.mult, op1=ALU.mult), reads=[dP, d_rstd, d_c2], writes=[d_QT])
            fw.op(fw.dve, lambda h: h.scalar_tensor_tensor(out=qg[:], in0=pS1[0:64, 0:512], scalar=hg[0:64, 2:3], in1=rstd[0:64, :],
                                                           op0=ALU.mult, op1=ALU.mult), reads=[dS1, d_rstd, d_c2], writes=[d_qg])
            fw.op(fw.dve, lambda h: h.tensor_copy(out=qsw[0:32, :], in_=qg[32:64, :]), reads=[d_qg], writes=[d_qg])
            fw.op(fw.dve, lambda h: h.tensor_copy(out=qsw[32:64, :], in_=qg[0:32, :]), reads=[d_qg], writes=[d_qg])
            fw.op(fw.dve, lambda h: h.tensor_tensor(out=qg[:], in0=qg[:], in1=cs[:, cols], op=ALU.mult), reads=[d_qg, d_c2], writes=[d_qg])
            fw.op(fw.dve, lambda h: h.tensor_tensor(out=qsw[:], in0=qsw[:], in1=sn[:, cols], op=ALU.mult), reads=[d_qg, d_c2], writes=[d_qg])
            fw.op(fw.dve, lambda h: h.tensor_tensor(out=QTr[:, cols], in0=qg[:], in1=qsw[:], op=ALU.add), reads=[d_qg], writes=[d_QT])
            fw.mm(pP[:, 0:512], [(wkvv[:, j, 0:128], ckvT[:, j, cols]) for j in range(4)], reads=[wdp, d_ckv], write=dP)
            fw.op(fw.act, lambda h: h.activation(out=sqn[:], in_=pP[:, 0:512], func=AF.Square), reads=[dP], writes=[d_sq])
            fw.mm(pSS[:, 0:512], [(C["ones"][:], sqn[:]), (C["ones"][0:64, :], sqpe[:, cols])], reads=[d_sq, d_kpe, C["dep"]], write=dSS)
            fw.op(fw.act, lambda h: h.activation(out=rstd[:], in_=pSS[:, 0:512], func=AF.Sqrt, scale=1.0 / 192.0, bias=EPS),
                  reads=[dSS], writes=[d_rstd])
            fw.op(fw.dve, lambda h: h.reciprocal(out=rstd[:], in_=rstd[:]), reads=[d_rstd], writes=[d_rstd])
            fw.op(fw.dve, lambda h: h.scalar_tensor_tensor(out=KTn[:, cols], in0=pP[:, 0:512], scalar=hg[:, 1:2], in1=rstd[:],
                                                           op0=ALU.mult, op1=ALU.mult), reads=[dP, d_rstd, d_c2], writes=[d_KT])
            fw.op(fw.dve, lambda h: h.tensor_tensor(out=KTr[:, cols], in0=kper[:, cols], in1=rstd[0:64, :], op=ALU.mult),
                  reads=[d_kpe, d_rstd], writes=[d_KT])
        for uc in range(16):
            fw.mm(pS0[:, (uc % 4) * 128:(uc % 4 + 1) * 128],
                  [(ckvT[:, j, uc * 128:(uc + 1) * 128], wkvv[:, j, 128:256]) for j in range(4)], reads=[wdp, d_ckv], write=dS0)
            if uc % 4 == 3:
                fw.op(fw.act, lambda h, uc=uc: h.activation(out=V[:, uc - 3:uc + 1, :],
                                                            in_=pS0[:, 0:512].rearrange("p (a b) -> p a b", a=4), func=AF.Copy),
                      reads=[dS0], writes=[d_V])
        stream.done()
        for qh in range(2):
            nkb = 8 * qh + 8
            for kb in range(nkb):
                if kb < 8 * qh:
                    ranges, diag = [(0, 512), (512, 1024)], False
                else:
                    a = (kb - 8 * qh) * 128
                    ranges = ([(a, 512)] if a < 512 else []) + [(max(a, 512), 1024)]
                    diag = True
                for ri, (c0, c1) in enumerate(ranges):
                    nq = c1 - c0
                    pS, dS = pSb[ntile % 2]
                    pt, dpt = PT[ntile % 2], d_PT[ntile % 2]
                    ntile += 1
                    q0 = qh * 1024 + c0
                    kc_ = slice(kb * 128, (kb + 1) * 128)
                    fw.mm(pS[:, 0:nq], [(KTn[:, kc_], QTn[:, q0:q0 + nq]), (KTr[:, kc_], QTr[:, q0:q0 + nq])],
                          reads=[d_QT, d_KT], write=dS)
                    fw.op(fw.act, lambda h: h.activation(out=pt[:, 0:nq], in_=pS[:, 0:nq], func=AF.Exp, scale=SCm),
                          reads=[dS], writes=[dpt])
                    if diag and ri == 0:
                        fw.op(fw.dve, lambda h: h.tensor_tensor(out=pt[:, 0:128], in0=pt[:, 0:128], in1=C["tri"][:], op=ALU.mult),
                              reads=[dpt, C["dep"]], writes=[dpt])
                    bk = c0 // 512
                    oc = c0 % 512
                    last_kb = 8 * qh + (3 if bk == 0 else 7)
                    po, dpo = pOb[bk]
                    pd, dpd = pDb[bk]
                    fw.mm(po[:, oc:oc + nq], [(V[:, kb, :], pt[:, 0:nq])], reads=[d_V, dpt], write=dpo,
                          start=(kb == 0), stop=(kb == last_kb))
                    fw.mm(pd[:, oc:oc + nq], [(C["ones"][:], pt[:, 0:nq])], reads=[dpt, C["dep"]], write=dpd,
                          start=(kb == 0), stop=(kb == last_kb))
            for bk in range(2):
                po, dpo = pOb[bk]
                pd, dpd = pDb[bk]
                sl_ = slice(bk * 512, (bk + 1) * 512)
                fw.op(fw.dve, lambda h: h.reciprocal(out=rden[:, sl_], in_=pd[:, 0:512]), reads=[dpd], writes=[d_rden])
                fw.op(fw.dve, lambda h: h.tensor_tensor(out=oTs[:, sl_], in0=po[:, 0:512], in1=rden[:, sl_], op=ALU.mult),
                      reads=[dpo, d_rden], writes=[d_oTs])
            last_ev = fw.dma(fw.sp, os_, oT_out[hd, :, qh * 1024:(qh + 1) * 1024], oTs[:], reads=[d_oTs])
    fw.sp.wait(last_ev)
    return nc


def _rope_tables():
    half = 32
    inv = 10000.0 ** (-np.arange(half, dtype=np.float64) / half)
    ang = np.arange(S_, dtype=np.float64)[None, :] * inv[:, None]
    cs = np.concatenate([np.cos(ang), np.cos(ang)], axis=0).astype(np.float32)
    sn = np.concatenate([-np.sin(ang), np.sin(ang)], axis=0).astype(np.float32)
    return np.ascontiguousarray(cs), np.ascontiguousarray(sn)


def run_mla(x_cur, c, ada_w_l, ada_b_l, g1, w_in, cq_gain, ckv_gain, w_q_up, w_kv_up, q_gain, k_gain):
    if "mla" not in _NC:
        _NC["mla"] = build_mla()
    nc = _NC["mla"]
    consts = _const_inputs()
    ada_w1 = np.ascontiguousarray(ada_w_l[:, 0:2 * D])
    ada_b1 = _rep(ada_b_l[0:2 * D])
    g_bc = _rep(g1)
    lat_gain = _rep(np.concatenate([cq_gain, ckv_gain]))
    hg = np.zeros((128, 4), np.float32)
    hg[:, 0] = q_gain[0:128]
    hg[:, 1] = k_gain[0:128]
    hg[0:64, 2] = q_gain[128:192]
    hg[0:64, 3] = k_gain[128:192]
    cs, sn = _rope_tables()
    in_maps = []
    for core in range(8):
        b, p = core // 2, core % 2
        m = dict(consts)
        m.update({"x_in": np.ascontiguousarray(x_cur[b]), "cT": np.ascontiguousarray(c[b].reshape(16, 128).T),
                  "ada_w": ada_w1, "ada_b": ada_b1, "g_bc": g_bc, "w_in": np.ascontiguousarray(w_in),
                  "lat_gain": lat_gain,
                  "w_q": np.ascontiguousarray(w_q_up[:, p * 1536:(p + 1) * 1536]),
                  "w_kv": np.ascontiguousarray(w_kv_up[:, p * 2048:(p + 1) * 2048]),
                  "hg": hg, "rope_cs": cs, "rope_sn": sn})
        in_maps.append(m)
    res = run_bass_kernel_spmd(nc, in_maps, core_ids=list(range(8)))
    oT = np.empty((4, 16, 128, S_), np.float32)
    for core in range(8):
        b, p = core // 2, core % 2
        oT[b, 8 * p:8 * p + 8] = res.results[core]["oT"]
    return oT


def kernel(x, c, ada_w, ada_b, norm1_g, norm2_g, dsa_w_in, dsa_q_gain, dsa_k_gain, dsa_w_out, mla_w_in,
           mla_cq_gain, mla_ckv_gain, mla_w_q_up, mla_w_kv_up, mla_q_gain, mla_k_gain, mla_w_out,
           router_group_w, router_group_b, router_expert_w, router_expert_b, expert_w_gate, expert_w_up,
           expert_w_down):
    f = lambda a: np.asarray(a, dtype=np.float32)
    x = f(x); c = f(c); ada_w = f(ada_w); ada_b = f(ada_b)
    oT = run_dsa(x, c, ada_w[0], ada_b[0], f(norm1_g)[0], f(dsa_w_in)[0], f(dsa_q_gain)[0], f(dsa_k_gain)[0])
    x = run_moe(x, oT, f(dsa_w_out)[0], c, ada_w[0], ada_b[0], f(norm2_g)[0], f(router_group_w)[0], f(router_group_b)[0],
                f(router_expert_w)[0], f(router_expert_b)[0], f(expert_w_gate)[0], f(expert_w_up)[0], f(expert_w_down)[0])
    oT = run_mla(x, c, ada_w[1], ada_b[1], f(norm1_g)[1], f(mla_w_in)[0], f(mla_cq_gain)[0], f(mla_ckv_gain)[0],
                 f(mla_w_q_up)[0], f(mla_w_kv_up)[0], f(mla_q_gain)[0], f(mla_k_gain)[0])
    x = run_moe(x, oT, f(mla_w_out)[0], c, ada_w[1], ada_b[1], f(norm2_g)[1], f(router_group_w)[1], f(router_group_b)[1],
                f(router_expert_w)[1], f(router_expert_b)[1], f(expert_w_gate)[1], f(expert_w_up)[1], f(expert_w_down)[1])
    return x
```

```python
import numpy as np
import concourse.bass as bass
import concourse.mybir as mybir
from concourse.bass_utils import run_bass_kernel_spmd

F32 = mybir.dt.float32
BF16 = mybir.dt.bfloat16
ALU = mybir.AluOpType
AF = mybir.ActivationFunctionType
AX = mybir.AxisListType

D = 2048
NTOK = 1024
NCH = 8
EPS = 1e-6
NEXP = 64
DEXP = 768
CAP = 128
EB_ = 4
SLOT_ELEMS = 8192
NSLOT = 3


class Dep:
    __slots__ = ("w", "r")

    def __init__(self):
        self.w = None
        self.r = []


class Eng:
    def __init__(self, name, h, sem):
        self.name, self.h, self.sem = name, h, sem
        self.count = 0
        self.seen = {}

    def wait(self, ev):
        if ev is None:
            return
        sem, val, key = ev
        if key == "pe" and self.name == "pe":
            return
        if self.seen.get(key, 0) >= val:
            return
        self.h.wait_ge(sem, val)
        self.seen[key] = val


class FW:
    def __init__(self, nc):
        self.nc = nc
        self.engs = []
        for name, h in (("pe", nc.tensor), ("act", nc.scalar), ("dve", nc.vector),
                        ("pool", nc.gpsimd), ("sp", nc.sync)):
            e = Eng(name, h, nc.alloc_semaphore("sem_" + name))
            setattr(self, name, e)
            self.engs.append(e)
        self.dsems = []
        self.off = nc.sbuf_base
        self.top = nc.sbuf_top
        self.nt = 0

    def sb(self, shape, dtype, name=None):
        nbytes = int(np.prod(shape[1:])) * (2 if dtype == BF16 else 4)
        nbytes = (nbytes + 63) // 64 * 64
        off = (self.off + 63) // 64 * 64
        assert off + nbytes <= self.top, ("SBUF overflow", name, off + nbytes - self.top)
        self.nt += 1
        t = self.nc.alloc_sbuf_tensor_at("%s_%d" % (name or "t", self.nt), list(shape), dtype, offset=off)
        self.off = off + nbytes
        self.lastoff = off
        return t

    def mark(self):
        return self.off

    def release(self, m):
        self.off = m

    def _stamp(self, e, ins, reads, writes):
        e.count += 1
        assert e.count < 60000, e.name
        ins.then_inc(e.sem, 1)
        ev = (e.sem, e.count, e.name)
        for d in writes:
            d.w = ev
            d.r = []
        for d in reads:
            d.r.append(ev)

    def _waits(self, e, reads, writes):
        for d in reads:
            e.wait(d.w)
        for d in writes:
            e.wait(d.w)
            for ev in d.r:
                e.wait(ev)

    def op(self, e, fn, reads=(), writes=()):
        self._waits(e, reads, writes)
        ins = fn(e.h)
        self._stamp(e, ins, reads, writes)

    def mm(self, out_ap, pairs, reads, write, start=True, stop=True):
        e = self.pe
        self._waits(e, reads, [write])
        n = len(pairs)
        ins = None
        for i, (l, r) in enumerate(pairs):
            ins = e.h.matmul(out_ap, lhsT=l, rhs=r, start=(start and i == 0), stop=(stop and i == n - 1))
        self._stamp(e, ins, reads, [write])

    def tr(self, out_ap, in_ap, ident_ap, reads, write):
        e = self.pe
        self._waits(e, reads, [write])
        ins = e.h.transpose(out_ap, in_ap, ident_ap)
        self._stamp(e, ins, reads, [write])

    def new_dsem(self, name="d"):
        s = [self.nc.alloc_semaphore("ds_%s_%d" % (name, len(self.dsems))), 0, "ds%d" % len(self.dsems)]
        self.dsems.append(s)
        return s

    def dma(self, q, dsem, out_ap, in_ap, reads=(), writes=(), nowait=False):
        if not nowait:
            self._waits(q, reads, writes)
        ins = q.h.dma_start(out=out_ap, in_=in_ap)
        dsem[1] += 16
        ins.then_inc(dsem[0], 16)
        ev = (dsem[0], dsem[1], dsem[2])
        for d in writes:
            d.w = ev
            d.r = []
        for d in reads:
            d.r.append(ev)
        return ev

    def barrier(self):
        evs = [(e.sem, e.count, e.name) for e in self.engs if e.count > 0]
        evs += [(s[0], s[1], s[2]) for s in self.dsems if s[1] > 0]
        for e in self.engs:
            for ev in evs:
                if ev[2] != e.name:
                    e.wait(ev)


class Stream:
    def __init__(self, fw, slots, units):
        self.fw, self.slots, self.units = fw, slots, units
        self.ni = 0
        self.nu = 0

    def issue(self):
        if self.ni >= len(self.units):
            return
        t, dep, dsem = self.slots[self.ni % len(self.slots)]
        parts = self.units[self.ni](t)
        for j, (o, a) in enumerate(parts):
            self.fw.dma(self.fw.pool, dsem, o, a, writes=[dep], nowait=(j > 0))
        self.ni += 1

    def prime(self):
        for _ in range(len(self.slots)):
            self.issue()

    def use(self):
        s = self.slots[self.nu % len(self.slots)]
        self.nu += 1
        return s[0], s[1]

    def done(self):
        self.issue()


def _consts(fw, dr):
    c = {}
    ds = fw.new_dsem("c")
    c["dep"] = Dep()
    for name, shape, dt in (("ident_f", [128, 128], F32), ("iota_f", [128, 128], F32)):
        t = fw.sb(shape, dt, name)
        fw.dma(fw.sp, ds, t[:], dr[name], writes=[c["dep"]], nowait=True)
        c[name] = t
    for name in ("ident", "ustrict", "ones", "tri"):
        tf = fw.sb([128, 128], F32, name + "_f")
        fw.dma(fw.sp, ds, tf[:], dr[name], writes=[c["dep"]], nowait=True)
        tb = fw.sb([128, 128], BF16, name + "_b")
        c[name + "_f32"] = tf
        c[name] = tb
    for name in ("ident", "ustrict", "ones", "tri"):
        fw.op(fw.dve, lambda h, a=c[name], b=c[name + "_f32"]: h.tensor_copy(out=a[:], in_=b[:]),
              reads=[c["dep"]], writes=[c["dep"]])
    return c


def _adaln(fw, stream, ps, ps_dep, crep, crep_dep, tiles, b_dram, consume, bst):
    for j in tiles:
        bt, bd, bs = bst[j % 2]
        fw.dma(fw.sp, bs, bt[:], b_dram[:, j * 512:(j + 1) * 512], writes=[bd])
        w, wd = stream.use()
        wv = w[:, 0:8192].rearrange("p (k n) -> p k n", k=16)
        fw.mm(ps[:, 0:512], [(crep[:, kc, :], wv[:, kc, :]) for kc in range(16)], reads=[crep_dep, wd], write=ps_dep)
        stream.done()
        consume(j, ps[:, 0:512], bt, bd)


def build_moe(H):
    nc = bass.Bass("TRN2", target_bir_lowering=False)
    fw = FW(nc)
    dt = nc.dram_tensor
    x_in = dt("x_in", [NTOK, D], F32, kind="ExternalInput").ap()
    cT = dt("cT", [128, 16], F32, kind="ExternalInput").ap()
    ada_w = dt("ada_w", [D, 4 * D], F32, kind="ExternalInput").ap()
    ada_b = dt("ada_b", [128, 4 * D], F32, kind="ExternalInput").ap()
    oT_in = dt("oT_in", [H, 128, NTOK], F32, kind="ExternalInput").ap()
    w_out = dt("w_out", [H * 128, D], F32, kind="ExternalInput").ap()
    g_bc = dt("g_bc", [128, D], F32, kind="ExternalInput").ap()
    w_r = dt("w_r", [D, 68], F32, kind="ExternalInput").ap()
    b_r = dt("b_r", [128, 68], F32, kind="ExternalInput").ap()
    wg = dt("wg", [NEXP, D, DEXP], F32, kind="ExternalInput").ap()
    wu = dt("wu", [NEXP, D, DEXP], F32, kind="ExternalInput").ap()
    wd = dt("wd", [NEXP, DEXP, D], F32, kind="ExternalInput").ap()
    cdr = {n: dt(n, [128, 128], F32, kind="ExternalInput").ap()
           for n in ("ident_f", "iota_f", "ident", "ustrict", "ones", "tri")}
    x_out = dt("x_out", [NTOK, D], F32, kind="ExternalOutput").ap()

    pP = nc.alloc_psum_tensor("pP", [128, 512], F32)
    pA = nc.alloc_psum_tensor("pA", [128, 512], F32)
    pB = nc.alloc_psum_tensor("pB", [128, 512], F32)
    pT1 = nc.alloc_psum_tensor("pT1", [128, 1024], BF16)
    pT2 = nc.alloc_psum_tensor("pT2", [128, 1024], BF16)
    pD0 = nc.alloc_psum_tensor("pD0", [128, 512], F32)
    pD1 = nc.alloc_psum_tensor("pD1", [128, 512], F32)
    pC = nc.alloc_psum_tensor("pC", [128, 512], F32)
    dP, dA, dB, dT1, dT2, dD0, dD1, dC = (Dep() for _ in range(8))

    x_res = fw.sb([128, NCH, D], F32, "x_res")
    x_dep = [Dep() for _ in range(NCH)]
    h2 = fw.sb([128, NCH, D], BF16, "h2")
    h2_dep = [Dep() for _ in range(NCH)]
    gate2b = fw.sb([128, D], BF16, "gate2b")
    gate2_dep = Dep()
    ring = [(fw.sb([128, SLOT_ELEMS], BF16, "ring"), Dep(), fw.new_dsem("ring")) for _ in range(NSLOT)]
    C = _consts(fw, cdr)
    A_f = fw.sb([128, NCH, NEXP], F32, "A_f")
    Wt = fw.sb([128, NCH, NEXP], F32, "Wt")
    rankp = fw.sb([128, NCH, NEXP], F32, "rankp")
    rt_dep = Dep()

    units = []

    def ada_unit(j):
        return lambda t, j=j: [(t[:, 0:8192].rearrange("p (k n) -> p k n", k=16),
                                ada_w[:, j * 512:(j + 1) * 512].rearrange("(k p) n -> p k n", p=128))]
    for j in range(4):
        units.append(ada_unit(j))
    for ft in range(4):
        units.append(lambda t, ft=ft: [(t[:, 0:H * 512].rearrange("p (h n) -> p h n", h=H),
                                        w_out[:, ft * 512:(ft + 1) * 512].rearrange("(h p) n -> p h n", p=128))])
    for j in range(4, 16):
        units.append(ada_unit(j))
    for e in range(NEXP):
        for c in range(2):
            for wsrc in (wg, wu):
                units.append(lambda t, e=e, c=c, wsrc=wsrc: [(
                    t[:, 0:6144].rearrange("p (k n) -> p k n", k=16),
                    wsrc[e, :, c * 384:(c + 1) * 384].rearrange("(k p) n -> p k n", p=128))])
        for c in range(2):
            units.append(lambda t, e=e, c=c: [(
                t[:, 0:6144].rearrange("p (k n) -> p k n", k=6),
                wd[e, :, c * 1024:(c + 1) * 1024].rearrange("(k p) n -> p k n", p=128))])
    stream = Stream(fw, ring, units)
    stream.prime()

    xs = fw.new_dsem("x")
    xv = x_in.rearrange("(c p) f -> p c f", p=128)
    for tc in range(NCH):
        fw.dma(fw.sp, xs, x_res[:, tc, :], xv[:, tc, :], writes=[x_dep[tc]], nowait=True)

    crep = fw.sb([128, 16, 128], BF16, "crep")
    cact = fw.sb([128, 16], F32, "cact")
    bst = [(fw.sb([128, 512], F32, "abias"), Dep(), fw.new_dsem("ab")) for _ in range(2)]
    d_crep, d_misc = Dep(), Dep()
    ms = fw.new_dsem("misc")
    fw.dma(fw.sp, ms, cact[:], cT, writes=[d_misc], nowait=True)
    fw.op(fw.act, lambda h: h.activation(out=cact[:], in_=cact[:], func=AF.Silu), reads=[d_misc], writes=[d_misc])
    for kc in range(16):
        fw.op(fw.dve, lambda h, kc=kc: h.tensor_scalar(out=crep[:, kc, :], in0=C["ones_f32"][:], scalar1=cact[:, kc:kc + 1],
                                                        scalar2=None, op0=ALU.mult),
              reads=[d_misc, C["dep"]], writes=[d_crep])
    m0 = fw.mark()

    gt1 = fw.sb([128, D], F32, "gt1")
    d_gt1 = Dep()
    oTc = [(fw.sb([128, H, 128], BF16, "oTc"), Dep(), fw.new_dsem("oTc")) for _ in range(2)]
    tmpb = fw.sb([128, 512], F32, "tmpb")
    d_tmpb = Dep()

    def consumeA(j, ps, bt, bd):
        cs = slice(j * 512, (j + 1) * 512)
        fw.op(fw.dve, lambda h: h.tensor_tensor(out=gt1[:, cs], in0=ps, in1=bt[:], op=ALU.add),
              reads=[dP, bd], writes=[d_gt1])
    _adaln(fw, stream, pP, dP, crep, d_crep, range(4), ada_b, consumeA, bst)
    pDl = [(pD0, dD0), (pD1, dD1)]
    n = 0
    for ft in range(4):
        wo, wod = stream.use()
        wov = wo[:, 0:H * 512].rearrange("p (h n) -> p h n", h=H)
        cs = slice(ft * 512, (ft + 1) * 512)
        for tc in range(NCH):
            ot, otd, ots = oTc[n % 2]
            pd, dd = pDl[n % 2]
            n += 1
            fw.dma(fw.pool, ots, ot[:], oT_in[:, :, tc * 128:(tc + 1) * 128].rearrange("h p t -> p h t"), writes=[otd])
            fw.mm(pd[:, 0:512], [(ot[:, hh, :], wov[:, hh, :]) for hh in range(H)], reads=[otd, wod], write=dd)
            fw.op(fw.dve, lambda h, pd=pd: h.tensor_tensor(out=tmpb[:], in0=pd[:, 0:512], in1=gt1[:, cs], op=ALU.mult),
                  reads=[dd, d_gt1], writes=[d_tmpb])
            fw.op(fw.dve, lambda h, tc=tc: h.tensor_tensor(out=x_res[:, tc, cs], in0=x_res[:, tc, cs], in1=tmpb[:], op=ALU.add),
                  reads=[d_tmpb, x_dep[tc]], writes=[x_dep[tc]])
        stream.done()
    fw.barrier()
    fw.release(m0)

    gs2 = fw.sb([128, D], F32, "gs2")
    sh2 = fw.sb([128, D], F32, "sh2")
    mod_dep = Dep()
    h2f = fw.sb([128, D], F32, "h2f")
    h2fT = fw.sb([128, 16, 128], F32, "h2fT")
    off_h2fT = fw.lastoff
    w_r_sb = fw.sb([128, 16, 68], F32, "w_r_sb")
    b_r_sb = fw.sb([128, 68], F32, "b_r_sb")
    gst = [(fw.sb([128, 512], F32, "gst"), Dep(), fw.new_dsem("gst")) for _ in range(2)]
    small = fw.sb([128, 256], F32, "small")
    d_small, d_h2f, d_h2fT = (Dep() for _ in range(3))
    d_misc2 = Dep()
    fw.dma(fw.sp, ms, w_r_sb[:], w_r.rearrange("(k p) n -> p k n", p=128), writes=[d_misc2], nowait=True)
    fw.dma(fw.sp, ms, b_r_sb[:], b_r, writes=[d_misc2], nowait=True)

    def consume(j, ps, bt, bd):
        j -= 4
        q = j // 4
        cs = slice((j % 4) * 512, (j % 4 + 1) * 512)
        if q == 0:
            fw.op(fw.dve, lambda h: h.tensor_tensor(out=sh2[:, cs], in0=ps, in1=bt[:], op=ALU.add),
                  reads=[dP, bd], writes=[mod_dep])
        elif q == 1:
            gt, gd, gsem = gst[j % 2]
            fw.dma(fw.sp, gsem, gt[:], g_bc[:, cs], writes=[gd])
            fw.op(fw.dve, lambda h: h.tensor_tensor(out=gs2[:, cs], in0=ps, in1=bt[:], op=ALU.add),
                  reads=[dP, bd], writes=[mod_dep])
            fw.op(fw.dve, lambda h: h.scalar_tensor_tensor(out=gs2[:, cs], in0=gs2[:, cs], scalar=1.0, in1=gt[:],
                                                           op0=ALU.add, op1=ALU.mult),
                  reads=[gd, mod_dep], writes=[mod_dep])
        else:
            fw.op(fw.dve, lambda h: h.tensor_tensor(out=gate2b[:, cs], in0=ps, in1=bt[:], op=ALU.add),
                  reads=[dP, bd], writes=[gate2_dep])

    _adaln(fw, stream, pP, dP, crep, d_crep, range(4, 16), ada_b, consume, bst)

    sm = small

    def col(i, n=1):
        return sm[:, i:i + n]
    for tc in range(NCH):
        xc = x_res[:, tc, :]
        fw.op(fw.dve, lambda h: h.memset(sm[:, 0:8], 0.0), reads=[d_small], writes=[d_small])
        fw.op(fw.act, lambda h: h.activation(out=h2f[:], in_=xc, func=AF.Square, accum_out=col(0)),
              reads=[x_dep[tc]], writes=[d_h2f, d_small])
        fw.op(fw.act, lambda h: h.activation(out=col(1), in_=col(0), func=AF.Sqrt, scale=1.0 / D, bias=EPS),
              reads=[d_small], writes=[d_small])
        fw.op(fw.dve, lambda h: h.reciprocal(out=col(1), in_=col(1)), reads=[d_small], writes=[d_small])
        fw.op(fw.dve, lambda h: h.scalar_tensor_tensor(out=h2f[:], in0=xc, scalar=col(1), in1=gs2[:],
                                                       op0=ALU.mult, op1=ALU.mult),
              reads=[x_dep[tc], d_small, mod_dep], writes=[d_h2f])
        fw.op(fw.dve, lambda h: h.tensor_tensor(out=h2f[:], in0=h2f[:], in1=sh2[:], op=ALU.add),
              reads=[d_h2f, mod_dep], writes=[d_h2f])
        fw.op(fw.act, lambda h: h.activation(out=h2[:, tc, :], in_=h2f[:], func=AF.Copy),
              reads=[d_h2f], writes=[h2_dep[tc]])
        for q in range(4):
            for j in range(4):
                kc = q * 4 + j
                fw.tr(pA[:, j * 128:(j + 1) * 128], h2f[:, kc * 128:(kc + 1) * 128], C["ident_f"][:],
                      reads=[d_h2f, C["dep"]], write=dA)
            fw.op(fw.act, lambda h, q=q: h.activation(out=h2fT[:, q * 4:(q + 1) * 4, :],
                                                      in_=pA[:, 0:512].rearrange("p (a b) -> p a b", a=4), func=AF.Copy),
                  reads=[dA], writes=[d_h2fT])
        fw.mm(pB[:, 0:68], [(h2fT[:, kc, :], w_r_sb[:, kc, :]) for kc in range(16)], reads=[d_h2fT, d_misc2], write=dB)
        lg = sm[:, 8:76]
        fw.op(fw.dve, lambda h: h.tensor_tensor(out=lg, in0=pB[:, 0:68], in1=b_r_sb[:], op=ALU.add),
              reads=[dB, d_misc2], writes=[d_small])
        R_ = [d_small]

        def dv(fn):
            fw.op(fw.dve, fn, reads=R_, writes=R_)

        def ac(fn):
            fw.op(fw.act, fn, reads=R_, writes=R_)
        gl = sm[:, 8:12]
        el = sm[:, 12:76]
        m64 = sm[:, 80:144]
        m64b = sm[:, 144:208]
        dv(lambda h: h.reduce_max(out=col(2), in_=gl, axis=AX.X))
        dv(lambda h: h.tensor_scalar(out=sm[:, 76:80], in0=gl, scalar1=col(2), scalar2=None, op0=ALU.is_equal))
        dv(lambda h: h.tensor_scalar(out=col(3), in0=col(2), scalar1=-1.0, scalar2=None, op0=ALU.mult))
        ac(lambda h: h.activation(out=sm[:, 208:212], in_=gl, func=AF.Exp, bias=col(3), scale=1.0, accum_out=col(4)))
        dv(lambda h: h.reciprocal(out=col(4), in_=col(4)))
        dv(lambda h: h.tensor_scalar(out=sm[:, 76:80], in0=sm[:, 76:80], scalar1=1e9, scalar2=-1e9,
                                     op0=ALU.mult, op1=ALU.add))
        for g in range(4):
            dv(lambda h, g=g: h.tensor_scalar(out=m64[:, g * 16:(g + 1) * 16], in0=el[:, g * 16:(g + 1) * 16],
                                              scalar1=sm[:, 76 + g:77 + g], scalar2=None, op0=ALU.add))
        A1 = A_f[:, tc, :]
        W1 = Wt[:, tc, :]
        oh2 = sm[:, 144:208]
        dv(lambda h: h.reduce_max(out=col(5), in_=m64, axis=AX.X))
        fw.op(fw.dve, lambda h: h.tensor_scalar(out=A1, in0=m64, scalar1=col(5), scalar2=None, op0=ALU.is_equal),
              reads=R_, writes=R_ + [rt_dep])
        dv(lambda h: h.scalar_tensor_tensor(out=m64b, in0=A1, scalar=-1e9, in1=m64, op0=ALU.mult, op1=ALU.add))
        dv(lambda h: h.reduce_max(out=col(6), in_=m64b, axis=AX.X))
        dv(lambda h: h.tensor_scalar(out=oh2, in0=m64b, scalar1=col(6), scalar2=None, op0=ALU.is_equal))
        dv(lambda h: h.tensor_tensor(out=col(7), in0=col(6), in1=col(5), op=ALU.subtract))
        ac(lambda h: h.activation(out=col(7), in_=col(7), func=AF.Exp))
        dv(lambda h: h.tensor_scalar(out=col(2), in0=col(7), scalar1=1.0, scalar2=None, op0=ALU.add))
        dv(lambda h: h.reciprocal(out=col(2), in_=col(2)))
        dv(lambda h: h.tensor_tensor(out=col(2), in0=col(2), in1=col(4), op=ALU.mult))
        dv(lambda h: h.tensor_tensor(out=col(3), in0=col(2), in1=col(7), op=ALU.mult))
        fw.op(fw.dve, lambda h: h.tensor_scalar(out=W1, in0=A1, scalar1=col(2), scalar2=None, op0=ALU.mult),
              reads=R_ + [rt_dep], writes=R_ + [rt_dep])
        fw.op(fw.dve, lambda h: h.scalar_tensor_tensor(out=W1, in0=oh2, scalar=col(3), in1=W1, op0=ALU.mult, op1=ALU.add),
              reads=R_ + [rt_dep], writes=R_ + [rt_dep])
        fw.op(fw.dve, lambda h: h.tensor_tensor(out=A1, in0=A1, in1=oh2, op=ALU.add),
              reads=R_ + [rt_dep], writes=R_ + [rt_dep])

    A_b = nc.alloc_sbuf_tensor_at("A_b_alias", [128, NCH, NEXP], BF16, offset=off_h2fT)
    fw.op(fw.dve, lambda h: h.tensor_copy(out=A_b[:], in_=A_f[:]), reads=[rt_dep], writes=[rt_dep, d_h2fT])
    for tc in range(NCH):
        pairs = [(C["ones"][:], A_b[:, t2, :]) for t2 in range(tc)] + [(C["ustrict"][:], A_b[:, tc, :])]
        fw.mm(pB[:, 0:64], pairs, reads=[rt_dep, C["dep"]], write=dB)
        fw.op(fw.dve, lambda h: h.tensor_scalar(out=rankp[:, tc, :], in0=A_f[:, tc, :], scalar1=-1e6, scalar2=1e6,
                                                op0=ALU.mult, op1=ALU.add), reads=[rt_dep], writes=[rt_dep])
        fw.op(fw.dve, lambda h: h.tensor_tensor(out=rankp[:, tc, :], in0=rankp[:, tc, :], in1=pB[:, 0:64], op=ALU.add),
              reads=[rt_dep, dB], writes=[rt_dep])

    fw.barrier()
    fw.release(m0)

    ybuf = fw.sb([128, EB_, D], BF16, "ybuf")
    selwt = fw.sb([128, EB_, NTOK], BF16, "selwt")
    sel = fw.sb([128, NCH, CAP], BF16, "sel")
    selw = fw.sb([128, NCH, CAP], BF16, "selw")
    xgT = fw.sb([128, 16, CAP], BF16, "xgT")
    hid = fw.sb([128, DEXP], BF16, "hid")
    sg = fw.sb([128, 384], F32, "sg")
    hidT = fw.sb([128, 6, CAP], BF16, "hidT")
    d_y = [Dep() for _ in range(EB_)]
    d_swt = [Dep() for _ in range(EB_)]
    d_sel, d_selw, d_xgT, d_hid, d_sg, d_hidT = (Dep() for _ in range(6))
    pD = [(pD0, dD0), (pD1, dD1)]
    iota = C["iota_f"]

    xgT2 = fw.sb([128, 16, CAP], BF16, "xgT2")
    xg = [(xgT, d_xgT), (xgT2, Dep())]
    gps = [(pP, dP), (pC, dC)]
    gcount = [0]

    def stage_sel(e):
        for tc in range(NCH):
            fw.op(fw.dve, lambda h, tc=tc: h.tensor_scalar(out=sel[:, tc, :], in0=iota[:], scalar1=rankp[:, tc, e:e + 1],
                                                           scalar2=None, op0=ALU.is_equal),
                  reads=[C["dep"], rt_dep], writes=[d_sel])
        for tc in range(NCH):
            fw.op(fw.dve, lambda h, tc=tc: h.tensor_scalar(out=selw[:, tc, :], in0=iota[:], scalar1=rankp[:, tc, e:e + 1],
                                                           scalar2=Wt[:, tc, e:e + 1], op0=ALU.is_equal, op1=ALU.mult),
                  reads=[C["dep"], rt_dep], writes=[d_selw])

    def stage_gather(e):
        xt, xd = xg[e % 2]
        for q in range(4):
            ps, dps = gps[gcount[0] % 2]
            gcount[0] += 1
            for j in range(4):
                fc = q * 4 + j
                fw.mm(ps[:, j * 128:(j + 1) * 128],
                      [(h2[:, tc, fc * 128:(fc + 1) * 128], sel[:, tc, :]) for tc in range(NCH)],
                      reads=h2_dep + [d_sel], write=dps)
            fw.op(fw.act, lambda h, q=q, ps=ps: h.activation(out=xt[:, q * 4:(q + 1) * 4, :],
                                                             in_=ps[:, 0:512].rearrange("p (a b) -> p a b", a=4), func=AF.Copy),
                  reads=[dps], writes=[xd])

    def stage_selT(e):
        eb = e % EB_
        for tc in range(NCH):
            fw.tr(pT1[:, tc * 128:(tc + 1) * 128], selw[:, tc, :], C["ident"][:], reads=[d_selw, C["dep"]], write=dT1)
        fw.op(fw.act, lambda h: h.activation(out=selwt[:, eb, :], in_=pT1[:, :], func=AF.Copy),
              reads=[dT1], writes=[d_swt[eb]])

    def stage_gateup(e):
        xt, xd = xg[e % 2]
        for c in range(2):
            wgt, wgd = stream.use()
            wgv = wgt[:, 0:6144].rearrange("p (k n) -> p k n", k=16)
            fw.mm(pA[:, 0:384], [(xt[:, kc, :], wgv[:, kc, :]) for kc in range(16)], reads=[xd, wgd], write=dA)
            stream.done()
            wut, wud = stream.use()
            wuv = wut[:, 0:6144].rearrange("p (k n) -> p k n", k=16)
            fw.mm(pB[:, 0:384], [(xt[:, kc, :], wuv[:, kc, :]) for kc in range(16)], reads=[xd, wud], write=dB)
            stream.done()
            fw.op(fw.act, lambda h: h.activation(out=sg[:], in_=pA[:, 0:384], func=AF.Silu), reads=[dA], writes=[d_sg])
            fw.op(fw.dve, lambda h, c=c: h.tensor_tensor(out=hid[:, c * 384:(c + 1) * 384], in0=pB[:, 0:384], in1=sg[:],
                                                         op=ALU.mult), reads=[dB, d_sg], writes=[d_hid])

    def stage_hidT(e):
        for j in range(6):
            fw.tr(pT2[:, j * 128:(j + 1) * 128], hid[:, j * 128:(j + 1) * 128], C["ident"][:], reads=[d_hid, C["dep"]], write=dT2)
        fw.op(fw.act, lambda h: h.activation(out=hidT[:], in_=pT2[:, 0:768].rearrange("p (a b) -> p a b", a=6), func=AF.Copy),
              reads=[dT2], writes=[d_hidT])

    def stage_down(e):
        eb = e % EB_
        for c in range(2):
            wdt, wdd = stream.use()
            wdv = wdt[:, 0:6144].rearrange("p (k n) -> p k n", k=6)
            for j in range(2):
                pd, dd = pD[j]
                fw.mm(pd[:, 0:512], [(hidT[:, kc, :], wdv[:, kc, j * 512:(j + 1) * 512]) for kc in range(6)],
                      reads=[d_hidT, wdd], write=dd)
                cs = slice(c * 1024 + j * 512, c * 1024 + (j + 1) * 512)
                fw.op(fw.dve, lambda h, cs=cs, pd=pd: h.tensor_tensor(out=ybuf[:, eb, cs], in0=pd[:, 0:512], in1=gate2b[:, cs],
                                                                      op=ALU.mult),
                      reads=[dd, gate2_dep], writes=[d_y[eb]])
            stream.done()

    def stage_combine():
        for tc in range(NCH):
            for ft in range(4):
                cs = slice(ft * 512, (ft + 1) * 512)
                ps, dps = gps[gcount[0] % 2]
                gcount[0] += 1
                fw.mm(ps[:, 0:512], [(selwt[:, b_, tc * 128:(tc + 1) * 128], ybuf[:, b_, cs]) for b_ in range(EB_)],
                      reads=d_swt + d_y, write=dps)
                fw.op(fw.dve, lambda h, tc=tc, cs=cs, ps=ps: h.tensor_tensor(out=x_res[:, tc, cs], in0=ps[:, 0:512],
                                                                             in1=x_res[:, tc, cs], op=ALU.add),
                      reads=[dps, x_dep[tc]], writes=[x_dep[tc]])

    stage_sel(0)
    stage_gather(0)
    stage_selT(0)
    for e in range(NEXP):
        stage_gateup(e)
        if e + 1 < NEXP:
            stage_sel(e + 1)
            stage_gather(e + 1)
        stage_hidT(e)
        stage_down(e)
        if e % EB_ == EB_ - 1:
            stage_combine()
        if e + 1 < NEXP:
            stage_selT(e + 1)

    os_ = fw.new_dsem("out")
    ov = x_out.rearrange("(c p) f -> p c f", p=128)
    ev = None
    for tc in range(NCH):
        ev = fw.dma(fw.sp, os_, ov[:, tc, :], x_res[:, tc, :], reads=[x_dep[tc]])
    fw.sp.wait(ev)
    return nc


def _const_inputs():
    i = np.arange(128)
    return {
        "ident_f": np.eye(128, dtype=np.float32),
        "ident": np.eye(128, dtype=np.float32),
        "iota_f": np.broadcast_to(i[None, :], (128, 128)).astype(np.float32).copy(),
        "ustrict": (i[:, None] < i[None, :]).astype(np.float32),
        "ones": np.ones((128, 128), np.float32),
        "tri": (i[None, :] >= i[:, None]).astype(np.float32),
    }


def _rep(v, n=128):
    return np.ascontiguousarray(np.broadcast_to(np.asarray(v, np.float32)[None, :], (n, v.shape[-1])))


_NC = {}


def run_moe(x_cur, oT_full, w_out, c, ada_w_l, ada_b_l, g2, w_rg, b_rg, w_re, b_re, wg, wu, wd):
    H = oT_full.shape[1]
    key = "moe%d" % H
    if key not in _NC:
        _NC[key] = build_moe(H)
    nc = _NC[key]
    consts = _const_inputs()
    ada_w2 = np.ascontiguousarray(ada_w_l[:, 2 * D:6 * D])
    ada_b2 = _rep(ada_b_l[2 * D:6 * D])
    g_bc = _rep(g2)
    w_r = np.ascontiguousarray(np.concatenate([w_rg, w_re], axis=1))
    b_r = _rep(np.concatenate([b_rg, b_re]))
    w_out = np.ascontiguousarray(w_out)
    wg = np.ascontiguousarray(wg)
    wu = np.ascontiguousarray(wu)
    wd = np.ascontiguousarray(wd)
    in_maps = []
    for core in range(8):
        b, p = core // 2, core % 2
        m = dict(consts)
        m.update({
            "x_in": np.ascontiguousarray(x_cur[b, p * NTOK:(p + 1) * NTOK, :]),
            "cT": np.ascontiguousarray(c[b].reshape(16, 128).T),
            "oT_in": np.ascontiguousarray(oT_full[b, :, :, p * NTOK:(p + 1) * NTOK]),
            "w_out": w_out,
            "ada_w": ada_w2, "ada_b": ada_b2, "g_bc": g_bc, "w_r": w_r, "b_r": b_r,
            "wg": wg, "wu": wu, "wd": wd,
        })
        in_maps.append(m)
    res = run_bass_kernel_spmd(nc, in_maps, core_ids=list(range(8)))
    out = np.empty_like(x_cur)
    for core in range(8):
        b, p = core // 2, core % 2
        out[b, p * NTOK:(p + 1) * NTOK, :] = res.results[core]["x_out"]
    return out


def _attn_prologue(nc, fw, C, stream, x_in, cT, ada_b, g_bc, hT, pP, dP, pT, dT):
    m0 = fw.mark()
    crep = fw.sb([128, 16, 128], BF16, "crep")
    cact = fw.sb([128, 16], F32, "cact")
    bst = [(fw.sb([128, 512], F32, "abias"), Dep(), fw.new_dsem("ab")) for _ in range(2)]
    gst = [(fw.sb([128, 512], F32, "gst"), Dep(), fw.new_dsem("gst")) for _ in range(2)]
    gs1 = fw.sb([128, D], F32, "gs1")
    sh1 = fw.sb([128, D], F32, "sh1")
    hf = fw.sb([128, D], F32, "hf")
    hb = fw.sb([128, D], BF16, "hb")
    small = fw.sb([128, 8], F32, "small")
    xst = [(fw.sb([128, D], F32, "xst"), Dep(), fw.new_dsem("xst")) for _ in range(2)]
    d_crep, d_misc, mod_dep, d_hf, d_hb, d_small = (Dep() for _ in range(6))
    ms = fw.new_dsem("misc")
    fw.dma(fw.sp, ms, cact[:], cT, writes=[d_misc], nowait=True)
    fw.op(fw.act, lambda h: h.activation(out=cact[:], in_=cact[:], func=AF.Silu), reads=[d_misc], writes=[d_misc])
    for kc in range(16):
        fw.op(fw.dve, lambda h, kc=kc: h.tensor_scalar(out=crep[:, kc, :], in0=C["ones_f32"][:], scalar1=cact[:, kc:kc + 1],
                                                        scalar2=None, op0=ALU.mult),
              reads=[d_misc, C["dep"]], writes=[d_crep])

    def consume(j, ps, bt, bd):
        q = j // 4
        cs = slice((j % 4) * 512, (j % 4 + 1) * 512)
        if q == 0:
            fw.op(fw.dve, lambda h: h.tensor_tensor(out=sh1[:, cs], in0=ps, in1=bt[:], op=ALU.add),
                  reads=[dP, bd], writes=[mod_dep])
        else:
            gt, gd, gsem = gst[j % 2]
            fw.dma(fw.sp, gsem, gt[:], g_bc[:, cs], writes=[gd])
            fw.op(fw.dve, lambda h: h.tensor_tensor(out=gs1[:, cs], in0=ps, in1=bt[:], op=ALU.add),
                  reads=[dP, bd], writes=[mod_dep])
            fw.op(fw.dve, lambda h: h.scalar_tensor_tensor(out=gs1[:, cs], in0=gs1[:, cs], scalar=1.0, in1=gt[:],
                                                           op0=ALU.add, op1=ALU.mult),
                  reads=[gd, mod_dep], writes=[mod_dep])
    _adaln(fw, stream, pP, dP, crep, d_crep, range(8), ada_b, consume, bst)

    hT_dep = Dep()
    for uc in range(16):
        xt, xd, xs = xst[uc % 2]
        fw.dma(fw.sp, xs, xt[:], x_in[uc * 128:(uc + 1) * 128, :], writes=[xd])
        fw.op(fw.dve, lambda h: h.memset(small[:, 0:2], 0.0), reads=[d_small], writes=[d_small])
        fw.op(fw.act, lambda h: h.activation(out=hf[:], in_=xt[:], func=AF.Square, accum_out=small[:, 0:1]),
              reads=[xd, d_small], writes=[d_hf, d_small])
        fw.op(fw.act, lambda h: h.activation(out=small[:, 1:2], in_=small[:, 0:1], func=AF.Sqrt, scale=1.0 / D, bias=EPS),
              reads=[d_small], writes=[d_small])
        fw.op(fw.dve, lambda h: h.reciprocal(out=small[:, 1:2], in_=small[:, 1:2]), reads=[d_small], writes=[d_small])
        fw.op(fw.dve, lambda h: h.scalar_tensor_tensor(out=hf[:], in0=xt[:], scalar=small[:, 1:2], in1=gs1[:],
                                                       op0=ALU.mult, op1=ALU.mult),
              reads=[xd, d_small, mod_dep, d_hf], writes=[d_hf])
        fw.op(fw.dve, lambda h: h.tensor_tensor(out=hb[:], in0=hf[:], in1=sh1[:], op=ALU.add),
              reads=[d_hf, mod_dep], writes=[d_hb])
        for q in range(2):
            for j in range(8):
                kc = q * 8 + j
                fw.tr(pT[:, j * 128:(j + 1) * 128], hb[:, kc * 128:(kc + 1) * 128], C["ident"][:],
                      reads=[d_hb, C["dep"]], write=dT)
            fw.op(fw.act, lambda h, q=q: h.activation(out=hT[:, q * 8:(q + 1) * 8, uc * 128:(uc + 1) * 128],
                                                      in_=pT[:, :].rearrange("p (a b) -> p a b", a=8), func=AF.Copy),
                  reads=[dT], writes=[hT_dep])
    fw.barrier()
    fw.release(m0)


S_ = 2048
DIL = (1, 4, 16)


def build_dsa():
    nc = bass.Bass("TRN2", target_bir_lowering=False)
    fw = FW(nc)
    dt = nc.dram_tensor
    x_in = dt("x_in", [S_, D], F32, kind="ExternalInput").ap()
    cT = dt("cT", [128, 16], F32, kind="ExternalInput").ap()
    ada_w = dt("ada_w", [D, 2 * D], F32, kind="ExternalInput").ap()
    ada_b = dt("ada_b", [128, 2 * D], F32, kind="ExternalInput").ap()
    g_bc = dt("g_bc", [128, D], F32, kind="ExternalInput").ap()
    w_perm = dt("w_perm", [4, D, 1152], F32, kind="ExternalInput").ap()
    qk_gain = dt("qk_gain", [128, 2], F32, kind="ExternalInput").ap()
    eb_in = dt("eb", [128, 12 * 256], F32, kind="ExternalInput").ap()
    cdr = {n: dt(n, [128, 128], F32, kind="ExternalInput").ap()
           for n in ("ident_f", "iota_f", "ident", "ustrict", "ones", "tri")}
    oT_out = dt("oT", [4, 128, S_], F32, kind="ExternalOutput").ap()

    pP = nc.alloc_psum_tensor("pP", [128, 512], F32)
    pSS = nc.alloc_psum_tensor("pSS", [128, 512], F32)
    pS0 = nc.alloc_psum_tensor("pS0", [128, 512], F32)
    pS1 = nc.alloc_psum_tensor("pS1", [128, 512], F32)
    pT = nc.alloc_psum_tensor("pT", [128, 1024], BF16)
    pV = nc.alloc_psum_tensor("pV", [128, 512], F32)
    pO = nc.alloc_psum_tensor("pO", [128, 512], F32)
    pDn = nc.alloc_psum_tensor("pDn", [128, 512], F32)
    dP, dSS, dS0, dS1, dT, dV, dO, dDn = (Dep() for _ in range(8))

    hT = fw.sb([128, 16, S_], BF16, "hT")
    ring = [(fw.sb([128, SLOT_ELEMS], BF16, "ring"), Dep(), fw.new_dsem("ring")) for _ in range(NSLOT)]
    C = _consts(fw, cdr)
    eb = fw.sb([128, 12, 2, 128], F32, "eb")
    gains = fw.sb([128, 2], F32, "gains")
    d_c2 = Dep()
    cs2 = fw.new_dsem("c2")
    fw.dma(fw.sp, cs2, eb[:], eb_in.rearrange("p (a v q) -> p a v q", a=12, v=2), writes=[d_c2], nowait=True)
    fw.dma(fw.sp, cs2, gains[:], qk_gain, writes=[d_c2], nowait=True)

    units = []
    for j in range(8):
        units.append(lambda t, j=j: [(t[:, 0:8192].rearrange("p (k n) -> p k n", k=16),
                                      ada_w[:, j * 512:(j + 1) * 512].rearrange("(k p) n -> p k n", p=128))])
    for sl in range(4):
        for g in range(3):
            units.append(lambda t, sl=sl, g=g: [(t[:, 0:6144].rearrange("p (k n) -> p k n", k=16),
                                                 w_perm[sl, :, g * 384:(g + 1) * 384].rearrange("(k p) n -> p k n", p=128))])
    stream = Stream(fw, ring, units)
    stream.prime()

    _attn_prologue(nc, fw, C, stream, x_in, cT, ada_b, g_bc, hT, pP, dP, pT, dT)

    QT = fw.sb([128, S_], BF16, "QT")
    KT = fw.sb([128, S_], BF16, "KT")
    V = fw.sb([128, 16, 128], BF16, "V")
    sq = fw.sb([128, 512], BF16, "sq")
    rstd = fw.sb([128, 512], F32, "rstd")
    expS = [fw.sb([128, 256], F32, "expS") for _ in range(2)]
    PT = [fw.sb([128, 256], BF16, "PT") for _ in range(2)]
    Oacc = fw.sb([128, 2, 1024], F32, "Oacc")
    Dacc = fw.sb([128, 2, 1024], F32, "Dacc")
    oTs = fw.sb([128, 1024], F32, "oTs")
    d_QT, d_KT, d_V, d_sq, d_rstd, d_acc, d_oTs = (Dep() for _ in range(7))
    d_expS = [Dep(), Dep()]
    d_PT = [Dep(), Dep()]
    os_ = fw.new_dsem("out")
    SC = 1.0 / float(np.sqrt(128.0))
    nblk = 0
    last_ev = None

    def flush(g, b, qh):
        d = DIL[g]
        for (ps, dps, acc) in ((pO, dO, Oacc), (pDn, dDn, Dacc)):
            if d == 1:
                ov = acc[:, qh, b * 512:(b + 1) * 512]
                iv = ps[:, 0:512]
            else:
                ov = acc[:, qh, :].rearrange("e (m r) -> e m r", r=d)[:, :, b * d // 2:(b + 1) * d // 2]
                iv = ps[:, 0:512].rearrange("e (r m) -> e m r", r=d // 2)
            if g == 0:
                fw.op(fw.dve, lambda h: h.tensor_copy(out=ov, in_=iv), reads=[dps], writes=[d_acc])
            else:
                fw.op(fw.dve, lambda h: h.tensor_tensor(out=ov, in0=ov, in1=iv, op=ALU.add), reads=[dps, d_acc], writes=[d_acc])

    for sl in range(4):
        for g in range(3):
            d = DIL[g]
            w, wdp = stream.use()
            wv = w[:, 0:6144].rearrange("p (k n) -> p k n", k=16)
            for which, dst, ddst, gcol in ((0, QT, d_QT, 0), (1, KT, d_KT, 1)):
                dview = dst[:, :].rearrange("e (r m) -> e m r", r=d)
                for i in range(4):
                    fw.mm(pP[:, 0:512], [(wv[:, kc, which * 128:(which + 1) * 128], hT[:, kc, i * 512:(i + 1) * 512])
                                         for kc in range(16)], reads=[wdp], write=dP)
                    fw.op(fw.act, lambda h: h.activation(out=sq[:], in_=pP[:, 0:512], func=AF.Square),
                          reads=[dP], writes=[d_sq])
                    fw.mm(pSS[:, 0:512], [(C["ones"][:], sq[:])], reads=[d_sq, C["dep"]], write=dSS)
                    fw.op(fw.act, lambda h: h.activation(out=rstd[:], in_=pSS[:, 0:512], func=AF.Sqrt, scale=1.0 / 128.0, bias=EPS),
                          reads=[dSS], writes=[d_rstd])
                    fw.op(fw.dve, lambda h: h.reciprocal(out=rstd[:], in_=rstd[:]), reads=[d_rstd], writes=[d_rstd])
                    fw.op(fw.dve, lambda h, i=i, dview=dview, gcol=gcol: h.scalar_tensor_tensor(
                        out=dview[:, i * 512 // d:(i + 1) * 512 // d, :],
                        in0=pP[:, 0:512].rearrange("e (m r) -> e m r", r=d), scalar=gains[:, gcol:gcol + 1],
                        in1=rstd[:, :].rearrange("e (m r) -> e m r", r=d), op0=ALU.mult, op1=ALU.mult),
                        reads=[dP, d_rstd, d_c2], writes=[ddst])
            nb = 16 // d
            for vb in range(16):
                r, mb = divmod(vb, nb)
                st = mb * 128 * d + r
                fw.mm(pV[:, (vb % 4) * 128:(vb % 4 + 1) * 128],
                      [(hT[:, kc, st:st + 127 * d + 1:d], wv[:, kc, 256:384]) for kc in range(16)], reads=[wdp], write=dV)
                if vb % 4 == 3:
                    fw.op(fw.act, lambda h, vb=vb: h.activation(out=V[:, vb - 3:vb + 1, :],
                                                                in_=pV[:, 0:512].rearrange("p (a b) -> p a b", a=4), func=AF.Copy),
                          reads=[dV], writes=[d_V])
            stream.done()
            ebi = sl * 3 + g
            for qh in range(2):
                blocks = []
                if g == 0:
                    for qb in range(8):
                        gb = 8 * qh + qb
                        blocks.append((gb * 128, 128, (gb - 1) * 128 if gb > 0 else None, gb * 128, 128, gb - 1, gb, qb * 128, 0))
                elif g == 1:
                    for r in range(4):
                        for j in range(2):
                            mb = 2 * qh + j
                            col = r * 512 + mb * 128
                            blocks.append((col, 128, col - 128 if mb > 0 else None, col, 128, r * 4 + mb - 1, r * 4 + mb,
                                           r * 256 + j * 128, 0))
                else:
                    for r in range(16):
                        nk = 64 if qh == 0 else 128
                        blocks.append((r * 128 + 64 * qh, 64, None, r * 128, nk, None, r, r * 64, 64 * qh))
                for (qcol, nq, pk, ck, nk, vp, vc, ocol, ebq0) in blocks:
                    pS, dS = (pS0, dS0) if nblk % 2 == 0 else (pS1, dS1)
                    eS, deS = expS[nblk % 2], d_expS[nblk % 2]
                    pt, dpt = PT[nblk % 2], d_PT[nblk % 2]
                    nblk += 1
                    qv = QT[:, qcol:qcol + nq]
                    if pk is not None:
                        fw.mm(pS[:, 0:nq], [(KT[:, pk:pk + 128], qv)], reads=[d_QT, d_KT], write=dS)
                    fw.mm(pS[0:nk, 128:128 + nq], [(KT[:, ck:ck + nk], qv)], reads=[d_QT, d_KT], write=dS)
                    if pk is not None:
                        fw.op(fw.act, lambda h: h.activation(out=eS[:, 0:256], in_=pS[:, 0:256], func=AF.Exp, scale=SC),
                              reads=[dS], writes=[deS])
                        fw.op(fw.dve, lambda h: h.tensor_tensor(out=pt[:, 0:256], in0=eS[:, 0:256],
                                                                in1=eb[:, ebi, :, :].rearrange("p v q -> p (v q)"), op=ALU.mult),
                              reads=[deS, d_c2], writes=[dpt])
                        pairs_o = [(V[:, vp, :], pt[:, 0:128]), (V[:, vc, :], pt[:, 128:256])]
                        pairs_d = [(C["ones"][:], pt[:, 0:128]), (C["ones"][:], pt[:, 128:256])]
                    else:
                        fw.op(fw.act, lambda h: h.activation(out=eS[0:nk, 128:128 + nq], in_=pS[0:nk, 128:128 + nq],
                                                             func=AF.Exp, scale=SC), reads=[dS], writes=[deS])
                        fw.op(fw.dve, lambda h: h.tensor_tensor(out=pt[0:nk, 128:128 + nq], in0=eS[0:nk, 128:128 + nq],
                                                                in1=eb[0:nk, ebi, 1, ebq0:ebq0 + nq], op=ALU.mult),
                              reads=[deS, d_c2], writes=[dpt])
                        pairs_o = [(V[0:nk, vc, :], pt[0:nk, 128:128 + nq])]
                        pairs_d = [(C["ones"][0:nk, :], pt[0:nk, 128:128 + nq])]
                    if ocol == 512:
                        flush(g, 0, qh)
                    oc = ocol % 512
                    fw.mm(pO[:, oc:oc + nq], pairs_o, reads=[d_V, dpt], write=dO)
                    fw.mm(pDn[:, oc:oc + nq], pairs_d, reads=[dpt, C["dep"]], write=dDn)
                flush(g, 1, qh)
                if g == 2:
                    fw.op(fw.dve, lambda h: h.reciprocal(out=Dacc[:, qh, :], in_=Dacc[:, qh, :]), reads=[d_acc], writes=[d_acc])
                    fw.op(fw.dve, lambda h: h.tensor_tensor(out=oTs[:], in0=Oacc[:, qh, :], in1=Dacc[:, qh, :], op=ALU.mult),
                          reads=[d_acc], writes=[d_oTs])
                    last_ev = fw.dma(fw.sp, os_, oT_out[sl, :, qh * 1024:(qh + 1) * 1024], oTs[:], reads=[d_oTs])
    fw.sp.wait(last_ev)
    return nc


def _alibi_eb(slots):
    k = np.arange(128)[:, None].astype(np.float64)
    q = np.arange(128)[None, :].astype(np.float64)
    out = np.zeros((128, len(slots) * 3, 2, 128), np.float32)
    for i, s in enumerate(slots):
        for g, d in enumerate(DIL):
            slope = 2.0 ** (-8.0 * (s * 3 + g + 1.0) / 24.0)
            dp = q + 128 - k
            out[:, i * 3 + g, 0, :] = np.where(dp <= 128, np.exp(-slope * d * dp), 0.0)
            dc = q - k
            out[:, i * 3 + g, 1, :] = np.where(dc >= 0, np.exp(-slope * d * np.maximum(dc, 0)), 0.0)
    return out.reshape(128, -1)


def run_dsa(x_cur, c, ada_w_l, ada_b_l, g1, w_in, q_gain, k_gain):
    if "dsa" not in _NC:
        _NC["dsa"] = build_dsa()
    nc = _NC["dsa"]
    consts = _const_inputs()
    ada_w1 = np.ascontiguousarray(ada_w_l[:, 0:2 * D])
    ada_b1 = _rep(ada_b_l[0:2 * D])
    g_bc = _rep(g1)
    wp = np.ascontiguousarray(w_in.reshape(D, 3, 3, 8, 128).transpose(3, 0, 1, 2, 4).reshape(8, D, 1152))
    gains = np.ascontiguousarray(np.stack([q_gain, k_gain], axis=1).astype(np.float32))
    in_maps = []
    for core in range(8):
        b, p = core // 2, core % 2
        m = dict(consts)
        m.update({"x_in": np.ascontiguousarray(x_cur[b]), "cT": np.ascontiguousarray(c[b].reshape(16, 128).T),
                  "ada_w": ada_w1, "ada_b": ada_b1, "g_bc": g_bc,
                  "w_perm": np.ascontiguousarray(wp[4 * p:4 * p + 4]), "qk_gain": gains,
                  "eb": _alibi_eb(range(4 * p, 4 * p + 4))})
        in_maps.append(m)
    res = run_bass_kernel_spmd(nc, in_maps, core_ids=list(range(8)))
    oT = np.empty((4, 8, 128, S_), np.float32)
    for core in range(8):
        b, p = core // 2, core % 2
        oT[b, 4 * p:4 * p + 4] = res.results[core]["oT"]
    return oT


def build_mla():
    nc = bass.Bass("TRN2", target_bir_lowering=False)
    fw = FW(nc)
    dt = nc.dram_tensor
    x_in = dt("x_in", [S_, D], F32, kind="ExternalInput").ap()
    cT = dt("cT", [128, 16], F32, kind="ExternalInput").ap()
    ada_w = dt("ada_w", [D, 2 * D], F32, kind="ExternalInput").ap()
    ada_b = dt("ada_b", [128, 2 * D], F32, kind="ExternalInput").ap()
    g_bc = dt("g_bc", [128, D], F32, kind="ExternalInput").ap()
    w_in = dt("w_in", [D, 1088], F32, kind="ExternalInput").ap()
    lat_gain = dt("lat_gain", [128, 1024], F32, kind="ExternalInput").ap()
    w_q = dt("w_q", [512, 8 * 192], F32, kind="ExternalInput").ap()
    w_kv = dt("w_kv", [512, 8 * 256], F32, kind="ExternalInput").ap()
    hg_in = dt("hg", [128, 4], F32, kind="ExternalInput").ap()
    cs_in = dt("rope_cs", [64, S_], F32, kind="ExternalInput").ap()
    sn_in = dt("rope_sn", [64, S_], F32, kind="ExternalInput").ap()
    cdr = {n: dt(n, [128, 128], F32, kind="ExternalInput").ap()
           for n in ("ident_f", "iota_f", "ident", "ustrict", "ones", "tri")}
    oT_out = dt("oT", [8, 128, S_], F32, kind="ExternalOutput").ap()

    pP = nc.alloc_psum_tensor("pP", [128, 512], F32)
    pSS = nc.alloc_psum_tensor("pSS", [128, 512], F32)
    pS0 = nc.alloc_psum_tensor("pS0", [128, 512], F32)
    pS1 = nc.alloc_psum_tensor("pS1", [128, 512], F32)
    pO0 = nc.alloc_psum_tensor("pO0", [128, 512], F32)
    pO1 = nc.alloc_psum_tensor("pO1", [128, 512], F32)
    pD0 = nc.alloc_psum_tensor("pD0", [128, 512], F32)
    pT = nc.alloc_psum_tensor("pT", [128, 1024], BF16)
    pD1 = pT[:, :].bitcast(F32)
    dP, dSS, dS0, dS1, dO0, dO1, dD0, dT = (Dep() for _ in range(8))
    dD1 = dT

    ring = [(fw.sb([128, SLOT_ELEMS], BF16, "ring"), Dep(), fw.new_dsem("ring")) for _ in range(NSLOT)]
    C = _consts(fw, cdr)
    hg = fw.sb([128, 4], F32, "hg")
    d_c2 = Dep()
    cs2 = fw.new_dsem("c2")
    fw.dma(fw.sp, cs2, hg[:], hg_in, writes=[d_c2], nowait=True)
    fw.top -= 65536
    hT = nc.alloc_sbuf_tensor_at("hT_top", [128, 16, S_], BF16, offset=fw.top)

    units = []
    for j in range(8):
        units.append(lambda t, j=j: [(t[:, 0:8192].rearrange("p (k n) -> p k n", k=16),
                                      ada_w[:, j * 512:(j + 1) * 512].rearrange("(k p) n -> p k n", p=128))])
    for (c0, n) in ((0, 512), (512, 512), (1024, 64)):
        units.append(lambda t, c0=c0, n=n: [(t[:, 0:16 * n].rearrange("p (k n) -> p k n", k=16),
                                             w_in[:, c0:c0 + n].rearrange("(k p) n -> p k n", p=128))])
    for hd in range(8):
        units.append(lambda t, hd=hd: [
            (t[:, 0:768].rearrange("p (k n) -> p k n", k=4), w_q[:, hd * 192:(hd + 1) * 192].rearrange("(k p) n -> p k n", p=128)),
            (t[:, 768:1792].rearrange("p (k n) -> p k n", k=4), w_kv[:, hd * 256:(hd + 1) * 256].rearrange("(k p) n -> p k n", p=128))])
    stream = Stream(fw, ring, units)
    stream.prime()

    _attn_prologue(nc, fw, C, stream, x_in, cT, ada_b, g_bc, hT, pP, dP, pT, dT)

    cs = fw.sb([64, S_], F32, "cs")
    sn = fw.sb([64, S_], F32, "sn")
    fw.dma(fw.sp, cs2, cs[:], cs_in, writes=[d_c2], nowait=True)
    fw.dma(fw.sp, cs2, sn[:], sn_in, writes=[d_c2], nowait=True)
    cqT = fw.sb([128, 4, S_], BF16, "cqT")
    ckvT = fw.sb([128, 4, S_], BF16, "ckvT")
    kraw = fw.sb([64, S_], F32, "kraw")
    sqpe = fw.sb([64, S_], BF16, "sqpe")
    kper = fw.sb([64, S_], F32, "kper")
    ksw = fw.sb([64, S_], F32, "ksw")
    d_cq, d_ckv, d_kraw, d_kpe = (Dep() for _ in range(4))
    m1 = fw.mark()
    lg = fw.sb([128, 1024], F32, "lat_gain")
    fw.dma(fw.sp, cs2, lg[:], lat_gain, writes=[d_c2], nowait=True)
    tmpn = fw.sb([128, 512], BF16, "tmpn")
    junk = fw.sb([128, 512], F32, "junk")
    ktok = fw.sb([128, 64], F32, "ktok")
    small = fw.sb([128, 8], F32, "small")
    d_tmpn, d_junk, d_ktok, d_small = (Dep() for _ in range(4))
    wq_t, wq_d = stream.use()
    wkv_t, wkv_d = stream.use()
    wpe_t, wpe_d = stream.use()
    wq_v = wq_t[:, 0:8192].rearrange("p (k n) -> p k n", k=16)
    wkv_v = wkv_t[:, 0:8192].rearrange("p (k n) -> p k n", k=16)
    wpe_v = wpe_t[:, 0:1024].rearrange("p (k n) -> p k n", k=16)
    for uc in range(16):
        tok = slice(uc * 128, (uc + 1) * 128)
        for (wv_, wd_, dstT, ddst, goff) in ((wq_v, wq_d, cqT, d_cq, 0), (wkv_v, wkv_d, ckvT, d_ckv, 512)):
            fw.mm(pP[:, 0:512], [(hT[:, kc, tok], wv_[:, kc, :]) for kc in range(16)], reads=[wd_], write=dP)
            fw.op(fw.dve, lambda h: h.memset(small[:, 0:2], 0.0), reads=[d_small], writes=[d_small])
            fw.op(fw.act, lambda h: h.activation(out=junk[:], in_=pP[:, 0:512], func=AF.Square, accum_out=small[:, 0:1]),
                  reads=[dP, d_small], writes=[d_junk, d_small])
            fw.op(fw.act, lambda h: h.activation(out=small[:, 1:2], in_=small[:, 0:1], func=AF.Sqrt, scale=1.0 / 512.0, bias=EPS),
                  reads=[d_small], writes=[d_small])
            fw.op(fw.dve, lambda h: h.reciprocal(out=small[:, 1:2], in_=small[:, 1:2]), reads=[d_small], writes=[d_small])
            fw.op(fw.dve, lambda h, goff=goff: h.scalar_tensor_tensor(out=tmpn[:], in0=pP[:, 0:512], scalar=small[:, 1:2],
                                                                      in1=lg[:, goff:goff + 512], op0=ALU.mult, op1=ALU.mult),
                  reads=[dP, d_small, d_c2], writes=[d_tmpn])
            for j in range(4):
                fw.tr(pT[:, j * 128:(j + 1) * 128], tmpn[:, j * 128:(j + 1) * 128], C["ident"][:], reads=[d_tmpn, C["dep"]], write=dT)
            fw.op(fw.act, lambda h, dstT=dstT: h.activation(out=dstT[:, :, tok], in_=pT[:, 0:512].rearrange("p (a b) -> p a b", a=4),
                                                            func=AF.Copy), reads=[dT], writes=[ddst])
        if uc % 4 == 0 and uc > 0:
            pass
        fw.mm(pS0[:, 0:64], [(hT[:, kc, tok], wpe_v[:, kc, :]) for kc in range(16)], reads=[wpe_d], write=dS0)
        fw.op(fw.act, lambda h: h.activation(out=ktok[:], in_=pS0[:, 0:64], func=AF.Copy), reads=[dS0], writes=[d_ktok])
        fw.tr(pSS[0:64, 0:128], ktok[:, 0:64], C["ident_f"][:], reads=[d_ktok, C["dep"]], write=dSS)
        fw.op(fw.act, lambda h: h.activation(out=kraw[:, tok], in_=pSS[0:64, 0:128], func=AF.Copy), reads=[dSS], writes=[d_kraw])
    stream.done()
    stream.done()
    stream.done()
    fw.op(fw.act, lambda h: h.activation(out=sqpe[:], in_=kraw[:], func=AF.Square), reads=[d_kraw], writes=[d_kpe])
    fw.op(fw.dve, lambda h: h.tensor_scalar(out=kper[:], in0=kraw[:], scalar1=hg[0:64, 3:4], scalar2=None, op0=ALU.mult),
          reads=[d_kraw, d_c2], writes=[d_kpe])
    fw.op(fw.dve, lambda h: h.tensor_copy(out=ksw[0:32, :], in_=kper[32:64, :]), reads=[d_kpe], writes=[d_kpe])
    fw.op(fw.dve, lambda h: h.tensor_copy(out=ksw[32:64, :], in_=kper[0:32, :]), reads=[d_kpe], writes=[d_kpe])
    fw.op(fw.dve, lambda h: h.tensor_tensor(out=kper[:], in0=kper[:], in1=cs[:], op=ALU.mult), reads=[d_kpe, d_c2], writes=[d_kpe])
    fw.op(fw.dve, lambda h: h.tensor_tensor(out=ksw[:], in0=ksw[:], in1=sn[:], op=ALU.mult), reads=[d_kpe, d_c2], writes=[d_kpe])
    fw.op(fw.dve, lambda h: h.tensor_tensor(out=kper[:], in0=kper[:], in1=ksw[:], op=ALU.add), reads=[d_kpe], writes=[d_kpe])
    fw.barrier()
    fw.release(m1)
    fw.top += 65536

    QTn = fw.sb([128, S_], BF16, "QTn")
    QTr = fw.sb([64, S_], BF16, "QTr")
    KTn = fw.sb([128, S_], BF16, "KTn")
    KTr = fw.sb([64, S_], BF16, "KTr")
    V = fw.sb([128, 16, 128], BF16, "V")
    sqn = fw.sb([128, 512], BF16, "sqn")
    sqr = fw.sb([64, 512], BF16, "sqr")
    rstd = fw.sb([128, 512], F32, "rstd")
    qg = fw.sb([64, 512], F32, "qg")
    qsw = fw.sb([64, 512], F32, "qsw")
    PT = [fw.sb([128, 512], BF16, "PT") for _ in range(2)]
    rden = fw.sb([128, 1024], F32, "rden")
    oTs = fw.sb([128, 1024], F32, "oTs")
    d_QT, d_KT, d_V, d_sq, d_rstd, d_qg, d_rden, d_oTs = (Dep() for _ in range(8))
    d_PT = [Dep(), Dep()]
    os_ = fw.new_dsem("out")
    SCm = 1.0 / float(np.sqrt(192.0))
    ntile = 0
    last_ev = None
    pOb = [(pO0, dO0), (pO1, dO1)]
    pDb = [(pD0, dD0), (pD1, dD1)]
    pSb = [(pS0, dS0), (pS1, dS1)]

    for hd in range(8):
        w, wdp = stream.use()
        wqv = w[:, 0:768].rearrange("p (k n) -> p k n", k=4)
        wkvv = w[:, 768:1792].rearrange("p (k n) -> p k n", k=4)
        for i in range(4):
            cols = slice(i * 512, (i + 1) * 512)
            fw.mm(pP[:, 0:512], [(wqv[:, j, 0:128], cqT[:, j, cols]) for j in range(4)], reads=[wdp, d_cq], write=dP)
            fw.mm(pS1[0:64, 0:512], [(wqv[:, j, 128:192], cqT[:, j, cols]) for j in range(4)], reads=[wdp, d_cq], write=dS1)
            fw.op(fw.act, lambda h: h.activation(out=sqn[:], in_=pP[:, 0:512], func=AF.Square), reads=[dP], writes=[d_sq])
            fw.op(fw.act, lambda h: h.activation(out=sqr[:], in_=pS1[0:64, 0:512], func=AF.Square), reads=[dS1], writes=[d_sq])
            fw.mm(pSS[:, 0:512], [(C["ones"][:], sqn[:]), (C["ones"][0:64, :], sqr[:])], reads=[d_sq, C["dep"]], write=dSS)
            fw.op(fw.act, lambda h: h.activation(out=rstd[:], in_=pSS[:, 0:512], func=AF.Sqrt, scale=1.0 / 192.0, bias=EPS),
                  reads=[dSS], writes=[d_rstd])
            fw.op(fw.dve, lambda h: h.reciprocal(out=rstd[:], in_=rstd[:]), reads=[d_rstd], writes=[d_rstd])
            fw.op(fw.dve, lambda h: h.scalar_tensor_tensor(out=QTn[:, cols], in0=pP[:, 0:512], scalar=hg[:, 0:1], in1=rstd[:],
                                                           op0=ALU.mult, op1=ALU.mult), reads=[dP, d_rstd, d_c2], writes=[d_QT])
            fw.op(fw.dve, lambda h: h.scalar_tensor_tensor(out=qg[:], in0=pS1[0:64, 0:512], scalar=hg[0:64, 2:3], in1=rstd[0:64, :],
                                                           op0=ALU.mult, op1=ALU.mult), reads=[dS1, d_rstd, d_c2], writes=[d_qg])
            fw.op(fw.dve, lambda h: h.tensor_copy(out=qsw[0:32, :], in_=qg[32:64, :]), reads=[d_qg], writes=[d_qg])
            fw.op(fw.dve, lambda h: h.tensor_copy(out=qsw[32:64, :], in_=qg[0:32, :]), reads=[d_qg], writes=[d_qg])
            fw.op(fw.dve, lambda h: h.tensor_tensor(out=qg[:], in0=qg[:], in1=cs[:, cols], op=ALU.mult), reads=[d_qg, d_c2], writes=[d_qg])
            fw.op(fw.dve, lambda h: h.tensor_tensor(out=qsw[:], in0=qsw[:], in1=sn[:, cols], op=ALU.mult), reads=[d_qg, d_c2], writes=[d_qg])
            fw.op(fw.dve, lambda h: h.tensor_tensor(out=QTr[:, cols], in0=qg[:], in1=qsw[:], op=ALU.add), reads=[d_qg], writes=[d_QT])
            fw.mm(pP[:, 0:512], [(wkvv[:, j, 0:128], ckvT[:, j, cols]) for j in range(4)], reads=[wdp, d_ckv], write=dP)
            fw.op(fw.act, lambda h: h.activation(out=sqn[:], in_=pP[:, 0:512], func=AF.Square), reads=[dP], writes=[d_sq])
            fw.mm(pSS[:, 0:512], [(C["ones"][:], sqn[:]), (C["ones"][0:64, :], sqpe[:, cols])], reads=[d_sq, d_kpe, C["dep"]], write=dSS)
            fw.op(fw.act, lambda h: h.activation(out=rstd[:], in_=pSS[:, 0:512], func=AF.Sqrt, scale=1.0 / 192.0, bias=EPS),
                  reads=[dSS], writes=[d_rstd])
            fw.op(fw.dve, lambda h: h.reciprocal(out=rstd[:], in_=rstd[:]), reads=[d_rstd], writes=[d_rstd])
            fw.op(fw.dve, lambda h: h.scalar_tensor_tensor(out=KTn[:, cols], in0=pP[:, 0:512], scalar=hg[:, 1:2], in1=rstd[:],
                                                           op0=ALU.mult, op1=ALU.mult), reads=[dP, d_rstd, d_c2], writes=[d_KT])
            fw.op(fw.dve, lambda h: h.tensor_tensor(out=KTr[:, cols], in0=kper[:, cols], in1=rstd[0:64, :], op=ALU.mult),
                  reads=[d_kpe, d_rstd], writes=[d_KT])
        for uc in range(16):
            fw.mm(pS0[:, (uc % 4) * 128:(uc % 4 + 1) * 128],
                  [(ckvT[:, j, uc * 128:(uc + 1) * 128], wkvv[:, j, 128:256]) for j in range(4)], reads=[wdp, d_ckv], write=dS0)
            if uc % 4 == 3:
                fw.op(fw.act, lambda h, uc=uc: h.activation(out=V[:, uc - 3:uc + 1, :],
                                                            in_=pS0[:, 0:512].rearrange("p (a b) -> p a b", a=4), func=AF.Copy),
                      reads=[dS0], writes=[d_V])
        stream.done()
        for qh in range(2):
            nkb = 8 * qh + 8
            for kb in range(nkb):
                if kb < 8 * qh:
                    ranges, diag = [(0, 512), (512, 1024)], False
                else:
                    a = (kb - 8 * qh) * 128
                    ranges = ([(a, 512)] if a < 512 else []) + [(max(a, 512), 1024)]
                    diag = True
                for ri, (c0, c1) in enumerate(ranges):
                    nq = c1 - c0
                    pS, dS = pSb[ntile % 2]
                    pt, dpt = PT[ntile % 2], d_PT[ntile % 2]
                    ntile += 1
                    q0 = qh * 1024 + c0
                    kc_ = slice(kb * 128, (kb + 1) * 128)
                    fw.mm(pS[:, 0:nq], [(KTn[:, kc_], QTn[:, q0:q0 + nq]), (KTr[:, kc_], QTr[:, q0:q0 + nq])],
                          reads=[d_QT, d_KT], write=dS)
                    fw.op(fw.act, lambda h: h.activation(out=pt[:, 0:nq], in_=pS[:, 0:nq], func=AF.Exp, scale=SCm),
                          reads=[dS], writes=[dpt])
                    if diag and ri == 0:
                        fw.op(fw.dve, lambda h: h.tensor_tensor(out=pt[:, 0:128], in0=pt[:, 0:128], in1=C["tri"][:], op=ALU.mult),
                              reads=[dpt, C["dep"]], writes=[dpt])
                    bk = c0 // 512
                    oc = c0 % 512
                    last_kb = 8 * qh + (3 if bk == 0 else 7)
                    po, dpo = pOb[bk]
                    pd, dpd = pDb[bk]
                    fw.mm(po[:, oc:oc + nq], [(V[:, kb, :], pt[:, 0:nq])], reads=[d_V, dpt], write=dpo,
                          start=(kb == 0), stop=(kb == last_kb))
                    fw.mm(pd[:, oc:oc + nq], [(C["ones"][:], pt[:, 0:nq])], reads=[dpt, C["dep"]], write=dpd,
                          start=(kb == 0), stop=(kb == last_kb))
            for bk in range(2):
                po, dpo = pOb[bk]
                pd, dpd = pDb[bk]
                sl_ = slice(bk * 512, (bk + 1) * 512)
                fw.op(fw.dve, lambda h: h.reciprocal(out=rden[:, sl_], in_=pd[:, 0:512]), reads=[dpd], writes=[d_rden])
                fw.op(fw.dve, lambda h: h.tensor_tensor(out=oTs[:, sl_], in0=po[:, 0:512], in1=rden[:, sl_], op=ALU.mult),
                      reads=[dpo, d_rden], writes=[d_oTs])
            last_ev = fw.dma(fw.sp, os_, oT_out[hd, :, qh * 1024:(qh + 1) * 1024], oTs[:], reads=[d_oTs])
    fw.sp.wait(last_ev)
    return nc


def _rope_tables():
    half = 32
    inv = 10000.0 ** (-np.arange(half, dtype=np.float64) / half)
    ang = np.arange(S_, dtype=np.float64)[None, :] * inv[:, None]
    cs = np.concatenate([np.cos(ang), np.cos(ang)], axis=0).astype(np.float32)
    sn = np.concatenate([-np.sin(ang), np.sin(ang)], axis=0).astype(np.float32)
    return np.ascontiguousarray(cs), np.ascontiguousarray(sn)


def run_mla(x_cur, c, ada_w_l, ada_b_l, g1, w_in, cq_gain, ckv_gain, w_q_up, w_kv_up, q_gain, k_gain):
    if "mla" not in _NC:
        _NC["mla"] = build_mla()
    nc = _NC["mla"]
    consts = _const_inputs()
    ada_w1 = np.ascontiguousarray(ada_w_l[:, 0:2 * D])
    ada_b1 = _rep(ada_b_l[0:2 * D])
    g_bc = _rep(g1)
    lat_gain = _rep(np.concatenate([cq_gain, ckv_gain]))
    hg = np.zeros((128, 4), np.float32)
    hg[:, 0] = q_gain[0:128]
    hg[:, 1] = k_gain[0:128]
    hg[0:64, 2] = q_gain[128:192]
    hg[0:64, 3] = k_gain[128:192]
    cs, sn = _rope_tables()
    in_maps = []
    for core in range(8):
        b, p = core // 2, core % 2
        m = dict(consts)
        m.update({"x_in": np.ascontiguousarray(x_cur[b]), "cT": np.ascontiguousarray(c[b].reshape(16, 128).T),
                  "ada_w": ada_w1, "ada_b": ada_b1, "g_bc": g_bc, "w_in": np.ascontiguousarray(w_in),
                  "lat_gain": lat_gain,
                  "w_q": np.ascontiguousarray(w_q_up[:, p * 1536:(p + 1) * 1536]),
                  "w_kv": np.ascontiguousarray(w_kv_up[:, p * 2048:(p + 1) * 2048]),
                  "hg": hg, "rope_cs": cs, "rope_sn": sn})
        in_maps.append(m)
    res = run_bass_kernel_spmd(nc, in_maps, core_ids=list(range(8)))
    oT = np.empty((4, 16, 128, S_), np.float32)
    for core in range(8):
        b, p = core // 2, core % 2
        oT[b, 8 * p:8 * p + 8] = res.results[core]["oT"]
    return oT


def kernel(x, c, ada_w, ada_b, norm1_g, norm2_g, dsa_w_in, dsa_q_gain, dsa_k_gain, dsa_w_out, mla_w_in,
           mla_cq_gain, mla_ckv_gain, mla_w_q_up, mla_w_kv_up, mla_q_gain, mla_k_gain, mla_w_out,
           router_group_w, router_group_b, router_expert_w, router_expert_b, expert_w_gate, expert_w_up,
           expert_w_down):
    f = lambda a: np.asarray(a, dtype=np.float32)
    x = f(x); c = f(c); ada_w = f(ada_w); ada_b = f(ada_b)
    oT = run_dsa(x, c, ada_w[0], ada_b[0], f(norm1_g)[0], f(dsa_w_in)[0], f(dsa_q_gain)[0], f(dsa_k_gain)[0])
    x = run_moe(x, oT, f(dsa_w_out)[0], c, ada_w[0], ada_b[0], f(norm2_g)[0], f(router_group_w)[0], f(router_group_b)[0],
                f(router_expert_w)[0], f(router_expert_b)[0], f(expert_w_gate)[0], f(expert_w_up)[0], f(expert_w_down)[0])
    oT = run_mla(x, c, ada_w[1], ada_b[1], f(norm1_g)[1], f(mla_w_in)[0], f(mla_cq_gain)[0], f(mla_ckv_gain)[0],
                 f(mla_w_q_up)[0], f(mla_w_kv_up)[0], f(mla_q_gain)[0], f(mla_k_gain)[0])
    x = run_moe(x, oT, f(mla_w_out)[0], c, ada_w[1], ada_b[1], f(norm2_g)[1], f(router_group_w)[1], f(router_group_b)[1],
                f(router_expert_w)[1], f(router_expert_b)[1], f(expert_w_gate)[1], f(expert_w_up)[1], f(expert_w_down)[1])
    return x
```

```python
import numpy as np
import concourse.bass as bass
import concourse.mybir as mybir
from concourse.bass_utils import run_bass_kernel_spmd

F32 = mybir.dt.float32
BF16 = mybir.dt.bfloat16
ALU = mybir.AluOpType
AF = mybir.ActivationFunctionType
AX = mybir.AxisListType

D = 2048
NTOK = 1024
NCH = 8
EPS = 1e-6
NEXP = 64
DEXP = 768
CAP = 128
EB_ = 4
SLOT_ELEMS = 8192
NSLOT = 3


class Dep:
    __slots__ = ("w", "r")

    def __init__(self):
        self.w = None
        self.r = []


class Eng:
    def __init__(self, name, h, sem):
        self.name, self.h, self.sem = name, h, sem
        self.count = 0
        self.seen = {}

    def wait(self, ev):
        if ev is None:
            return
        sem, val, key = ev
        if key == "pe" and self.name == "pe":
            return
        if self.seen.get(key, 0) >= val:
            return
        self.h.wait_ge(sem, val)
        self.seen[key] = val


class FW:
    def __init__(self, nc):
        self.nc = nc
        self.engs = []
        for name, h in (("pe", nc.tensor), ("act", nc.scalar), ("dve", nc.vector),
                        ("pool", nc.gpsimd), ("sp", nc.sync)):
            e = Eng(name, h, nc.alloc_semaphore("sem_" + name))
            setattr(self, name, e)
            self.engs.append(e)
        self.dsems = []
        self.off = nc.sbuf_base
        self.top = nc.sbuf_top
        self.nt = 0

    def sb(self, shape, dtype, name=None):
        nbytes = int(np.prod(shape[1:])) * (2 if dtype == BF16 else 4)
        nbytes = (nbytes + 63) // 64 * 64
        off = (self.off + 63) // 64 * 64
        assert off + nbytes <= self.top, ("SBUF overflow", name, off + nbytes - self.top)
        self.nt += 1
        t = self.nc.alloc_sbuf_tensor_at("%s_%d" % (name or "t", self.nt), list(shape), dtype, offset=off)
        self.off = off + nbytes
        self.lastoff = off
        return t

    def mark(self):
        return self.off

    def release(self, m):
        self.off = m

    def _stamp(self, e, ins, reads, writes):
        e.count += 1
        assert e.count < 60000, e.name
        ins.then_inc(e.sem, 1)
        ev = (e.sem, e.count, e.name)
        for d in writes:
            d.w = ev
            d.r = []
        for d in reads:
            d.r.append(ev)

    def _waits(self, e, reads, writes):
        for d in reads:
            e.wait(d.w)
        for d in writes:
            e.wait(d.w)
            for ev in d.r:
                e.wait(ev)

    def op(self, e, fn, reads=(), writes=()):
        self._waits(e, reads, writes)
        ins = fn(e.h)
        self._stamp(e, ins, reads, writes)

    def mm(self, out_ap, pairs, reads, write, start=True, stop=True):
        e = self.pe
        self._waits(e, reads, [write])
        n = len(pairs)
        ins = None
        for i, (l, r) in enumerate(pairs):
            ins = e.h.matmul(out_ap, lhsT=l, rhs=r, start=(start and i == 0), stop=(stop and i == n - 1))
        self._stamp(e, ins, reads, [write])

    def tr(self, out_ap, in_ap, ident_ap, reads, write):
        e = self.pe
        self._waits(e, reads, [write])
        ins = e.h.transpose(out_ap, in_ap, ident_ap)
        self._stamp(e, ins, reads, [write])

    def new_dsem(self, name="d"):
        s = [self.nc.alloc_semaphore("ds_%s_%d" % (name, len(self.dsems))), 0, "ds%d" % len(self.dsems)]
        self.dsems.append(s)
        return s

    def dma(self, q, dsem, out_ap, in_ap, reads=(), writes=(), nowait=False):
        if not nowait:
            self._waits(q, reads, writes)
        ins = q.h.dma_start(out=out_ap, in_=in_ap)
        dsem[1] += 16
        ins.then_inc(dsem[0], 16)
        ev = (dsem[0], dsem[1], dsem[2])
        for d in writes:
            d.w = ev
            d.r = []
        for d in reads:
            d.r.append(ev)
        return ev

    def barrier(self):
        evs = [(e.sem, e.count, e.name) for e in self.engs if e.count > 0]
        evs += [(s[0], s[1], s[2]) for s in self.dsems if s[1] > 0]
        for e in self.engs:
            for ev in evs:
                if ev[2] != e.name:
                    e.wait(ev)


class Stream:
    def __init__(self, fw, slots, units):
        self.fw, self.slots, self.units = fw, slots, units
        self.ni = 0
        self.nu = 0

    def issue(self):
        if self.ni >= len(self.units):
            return
        t, dep, dsem = self.slots[self.ni % len(self.slots)]
        parts = self.units[self.ni](t)
        for j, (o, a) in enumerate(parts):
            self.fw.dma(self.fw.pool, dsem, o, a, writes=[dep], nowait=(j > 0))
        self.ni += 1

    def prime(self):
        for _ in range(len(self.slots)):
            self.issue()

    def use(self):
        s = self.slots[self.nu % len(self.slots)]
        self.nu += 1
        return s[0], s[1]

    def done(self):
        self.issue()


def _consts(fw, dr):
    c = {}
    ds = fw.new_dsem("c")
    c["dep"] = Dep()
    for name, shape, dt in (("ident_f", [128, 128], F32), ("iota_f", [128, 128], F32)):
        t = fw.sb(shape, dt, name)
        fw.dma(fw.sp, ds, t[:], dr[name], writes=[c["dep"]], nowait=True)
        c[name] = t
    for name in ("ident", "ustrict", "ones", "tri"):
        tf = fw.sb([128, 128], F32, name + "_f")
        fw.dma(fw.sp, ds, tf[:], dr[name], writes=[c["dep"]], nowait=True)
        tb = fw.sb([128, 128], BF16, name + "_b")
        c[name + "_f32"] = tf
        c[name] = tb
    for name in ("ident", "ustrict", "ones", "tri"):
        fw.op(fw.dve, lambda h, a=c[name], b=c[name + "_f32"]: h.tensor_copy(out=a[:], in_=b[:]),
              reads=[c["dep"]], writes=[c["dep"]])
    return c


def _adaln(fw, stream, ps, ps_dep, crep, crep_dep, tiles, b_dram, consume, bst):
    for j in tiles:
        bt, bd, bs = bst[j % 2]
        fw.dma(fw.sp, bs, bt[:], b_dram[:, j * 512:(j + 1) * 512], writes=[bd])
        w, wd = stream.use()
        wv = w[:, 0:8192].rearrange("p (k n) -> p k n", k=16)
        fw.mm(ps[:, 0:512], [(crep[:, kc, :], wv[:, kc, :]) for kc in range(16)], reads=[crep_dep, wd], write=ps_dep)
        stream.done()
        consume(j, ps[:, 0:512], bt, bd)


def build_moe(H):
    nc = bass.Bass("TRN2", target_bir_lowering=False)
    fw = FW(nc)
    dt = nc.dram_tensor
    x_in = dt("x_in", [NTOK, D], F32, kind="ExternalInput").ap()
    cT = dt("cT", [128, 16], F32, kind="ExternalInput").ap()
    ada_w = dt("ada_w", [D, 4 * D], F32, kind="ExternalInput").ap()
    ada_b = dt("ada_b", [128, 4 * D], F32, kind="ExternalInput").ap()
    oT_in = dt("oT_in", [H, 128, NTOK], F32, kind="ExternalInput").ap()
    w_out = dt("w_out", [H * 128, D], F32, kind="ExternalInput").ap()
    g_bc = dt("g_bc", [128, D], F32, kind="ExternalInput").ap()
    w_r = dt("w_r", [D, 68], F32, kind="ExternalInput").ap()
    b_r = dt("b_r", [128, 68], F32, kind="ExternalInput").ap()
    wg = dt("wg", [NEXP, D, DEXP], F32, kind="ExternalInput").ap()
    wu = dt("wu", [NEXP, D, DEXP], F32, kind="ExternalInput").ap()
    wd = dt("wd", [NEXP, DEXP, D], F32, kind="ExternalInput").ap()
    cdr = {n: dt(n, [128, 128], F32, kind="ExternalInput").ap()
           for n in ("ident_f", "iota_f", "ident", "ustrict", "ones", "tri")}
    x_out = dt("x_out", [NTOK, D], F32, kind="ExternalOutput").ap()

    pP = nc.alloc_psum_tensor("pP", [128, 512], F32)
    pA = nc.alloc_psum_tensor("pA", [128, 512], F32)
    pB = nc.alloc_psum_tensor("pB", [128, 512], F32)
    pT1 = nc.alloc_psum_tensor("pT1", [128, 1024], BF16)
    pT2 = nc.alloc_psum_tensor("pT2", [128, 1024], BF16)
    pD0 = nc.alloc_psum_tensor("pD0", [128, 512], F32)
    pD1 = nc.alloc_psum_tensor("pD1", [128, 512], F32)
    pC = nc.alloc_psum_tensor("pC", [128, 512], F32)
    dP, dA, dB, dT1, dT2, dD0, dD1, dC = (Dep() for _ in range(8))

    x_res = fw.sb([128, NCH, D], F32, "x_res")
    x_dep = [Dep() for _ in range(NCH)]
    h2 = fw.sb([128, NCH, D], BF16, "h2")
    h2_dep = [Dep() for _ in range(NCH)]
    gate2b = fw.sb([128, D], BF16, "gate2b")
    gate2_dep = Dep()
    ring = [(fw.sb([128, SLOT_ELEMS], BF16, "ring"), Dep(), fw.new_dsem("ring")) for _ in range(NSLOT)]
    C = _consts(fw, cdr)
    A_f = fw.sb([128, NCH, NEXP], F32, "A_f")
    Wt = fw.sb([128, NCH, NEXP], F32, "Wt")
    rankp = fw.sb([128, NCH, NEXP], F32, "rankp")
    rt_dep = Dep()

    units = []

    def ada_unit(j):
        return lambda t, j=j: [(t[:, 0:8192].rearrange("p (k n) -> p k n", k=16),
                                ada_w[:, j * 512:(j + 1) * 512].rearrange("(k p) n -> p k n", p=128))]
    for j in range(4):
        units.append(ada_unit(j))
    for ft in range(4):
        units.append(lambda t, ft=ft: [(t[:, 0:H * 512].rearrange("p (h n) -> p h n", h=H),
                                        w_out[:, ft * 512:(ft + 1) * 512].rearrange("(h p) n -> p h n", p=128))])
    for j in range(4, 16):
        units.append(ada_unit(j))
    for e in range(NEXP):
        for c in range(2):
            for wsrc in (wg, wu):
                units.append(lambda t, e=e, c=c, wsrc=wsrc: [(
                    t[:, 0:6144].rearrange("p (k n) -> p k n", k=16),
                    wsrc[e, :, c * 384:(c + 1) * 384].rearrange("(k p) n -> p k n", p=128))])
        for c in range(2):
            units.append(lambda t, e=e, c=c: [(
                t[:, 0:6144].rearrange("p (k n) -> p k n", k=6),
                wd[e, :, c * 1024:(c + 1) * 1024].rearrange("(k p) n -> p k n", p=128))])
    stream = Stream(fw, ring, units)
    stream.prime()

    xs = fw.new_dsem("x")
    xv = x_in.rearrange("(c p) f -> p c f", p=128)
    for tc in range(NCH):
        fw.dma(fw.sp, xs, x_res[:, tc, :], xv[:, tc, :], writes=[x_dep[tc]], nowait=True)

    crep = fw.sb([128, 16, 128], BF16, "crep")
    cact = fw.sb([128, 16], F32, "cact")
    bst = [(fw.sb([128, 512], F32, "abias"), Dep(), fw.new_dsem("ab")) for _ in range(2)]
    d_crep, d_misc = Dep(), Dep()
    ms = fw.new_dsem("misc")
    fw.dma(fw.sp, ms, cact[:], cT, writes=[d_misc], nowait=True)
    fw.op(fw.act, lambda h: h.activation(out=cact[:], in_=cact[:], func=AF.Silu), reads=[d_misc], writes=[d_misc])
    for kc in range(16):
        fw.op(fw.dve, lambda h, kc=kc: h.tensor_scalar(out=crep[:, kc, :], in0=C["ones_f32"][:], scalar1=cact[:, kc:kc + 1],
                                                        scalar2=None, op0=ALU.mult),
              reads=[d_misc, C["dep"]], writes=[d_crep])
    m0 = fw.mark()

    gt1 = fw.sb([128, D], F32, "gt1")
    d_gt1 = Dep()
    oTc = [(fw.sb([128, H, 128], BF16, "oTc"), Dep(), fw.new_dsem("oTc")) for _ in range(2)]
    tmpb = fw.sb([128, 512], F32, "tmpb")
    d_tmpb = Dep()

    def consumeA(j, ps, bt, bd):
        cs = slice(j * 512, (j + 1) * 512)
        fw.op(fw.dve, lambda h: h.tensor_tensor(out=gt1[:, cs], in0=ps, in1=bt[:], op=ALU.add),
              reads=[dP, bd], writes=[d_gt1])
    _adaln(fw, stream, pP, dP, crep, d_crep, range(4), ada_b, consumeA, bst)
    pDl = [(pD0, dD0), (pD1, dD1)]
    n = 0
    for ft in range(4):
        wo, wod = stream.use()
        wov = wo[:, 0:H * 512].rearrange("p (h n) -> p h n", h=H)
        cs = slice(ft * 512, (ft + 1) * 512)
        for tc in range(NCH):
            ot, otd, ots = oTc[n % 2]
            pd, dd = pDl[n % 2]
            n += 1
            fw.dma(fw.pool, ots, ot[:], oT_in[:, :, tc * 128:(tc + 1) * 128].rearrange("h p t -> p h t"), writes=[otd])
            fw.mm(pd[:, 0:512], [(ot[:, hh, :], wov[:, hh, :]) for hh in range(H)], reads=[otd, wod], write=dd)
            fw.op(fw.dve, lambda h, pd=pd: h.tensor_tensor(out=tmpb[:], in0=pd[:, 0:512], in1=gt1[:, cs], op=ALU.mult),
                  reads=[dd, d_gt1], writes=[d_tmpb])
            fw.op(fw.dve, lambda h, tc=tc: h.tensor_tensor(out=x_res[:, tc, cs], in0=x_res[:, tc, cs], in1=tmpb[:], op=ALU.add),
                  reads=[d_tmpb, x_dep[tc]], writes=[x_dep[tc]])
        stream.done()
    fw.barrier()
    fw.release(m0)

    gs2 = fw.sb([128, D], F32, "gs2")
    sh2 = fw.sb([128, D], F32, "sh2")
    mod_dep = Dep()
    h2f = fw.sb([128, D], F32, "h2f")
    h2fT = fw.sb([128, 16, 128], F32, "h2fT")
    off_h2fT = fw.lastoff
    w_r_sb = fw.sb([128, 16, 68], F32, "w_r_sb")
    b_r_sb = fw.sb([128, 68], F32, "b_r_sb")
    gst = [(fw.sb([128, 512], F32, "gst"), Dep(), fw.new_dsem("gst")) for _ in range(2)]
    small = fw.sb([128, 256], F32, "small")
    d_small, d_h2f, d_h2fT = (Dep() for _ in range(3))
    d_misc2 = Dep()
    fw.dma(fw.sp, ms, w_r_sb[:], w_r.rearrange("(k p) n -> p k n", p=128), writes=[d_misc2], nowait=True)
    fw.dma(fw.sp, ms, b_r_sb[:], b_r, writes=[d_misc2], nowait=True)

    def consume(j, ps, bt, bd):
        j -= 4
        q = j // 4
        cs = slice((j % 4) * 512, (j % 4 + 1) * 512)
        if q == 0:
            fw.op(fw.dve, lambda h: h.tensor_tensor(out=sh2[:, cs], in0=ps, in1=bt[:], op=ALU.add),
                  reads=[dP, bd], writes=[mod_dep])
        elif q == 1:
            gt, gd, gsem = gst[j % 2]
            fw.dma(fw.sp, gsem, gt[:], g_bc[:, cs], writes=[gd])
            fw.op(fw.dve, lambda h: h.tensor_tensor(out=gs2[:, cs], in0=ps, in1=bt[:], op=ALU.add),
                  reads=[dP, bd], writes=[mod_dep])
            fw.op(fw.dve, lambda h: h.scalar_tensor_tensor(out=gs2[:, cs], in0=gs2[:, cs], scalar=1.0, in1=gt[:],
                                                           op0=ALU.add, op1=ALU.mult),
                  reads=[gd, mod_dep], writes=[mod_dep])
        else:
            fw.op(fw.dve, lambda h: h.tensor_tensor(out=gate2b[:, cs], in0=ps, in1=bt[:], op=ALU.add),
                  reads=[dP, bd], writes=[gate2_dep])

    _adaln(fw, stream, pP, dP, crep, d_crep, range(4, 16), ada_b, consume, bst)

    sm = small

    def col(i, n=1):
        return sm[:, i:i + n]
    for tc in range(NCH):
        xc = x_res[:, tc, :]
        fw.op(fw.dve, lambda h: h.memset(sm[:, 0:8], 0.0), reads=[d_small], writes=[d_small])
        fw.op(fw.act, lambda h: h.activation(out=h2f[:], in_=xc, func=AF.Square, accum_out=col(0)),
              reads=[x_dep[tc]], writes=[d_h2f, d_small])
        fw.op(fw.act, lambda h: h.activation(out=col(1), in_=col(0), func=AF.Sqrt, scale=1.0 / D, bias=EPS),
              reads=[d_small], writes=[d_small])
        fw.op(fw.dve, lambda h: h.reciprocal(out=col(1), in_=col(1)), reads=[d_small], writes=[d_small])
        fw.op(fw.dve, lambda h: h.scalar_tensor_tensor(out=h2f[:], in0=xc, scalar=col(1), in1=gs2[:],
                                                       op0=ALU.mult, op1=ALU.mult),
              reads=[x_dep[tc], d_small, mod_dep], writes=[d_h2f])
        fw.op(fw.dve, lambda h: h.tensor_tensor(out=h2f[:], in0=h2f[:], in1=sh2[:], op=ALU.add),
              reads=[d_h2f, mod_dep], writes=[d_h2f])
        fw.op(fw.act, lambda h: h.activation(out=h2[:, tc, :], in_=h2f[:], func=AF.Copy),
              reads=[d_h2f], writes=[h2_dep[tc]])
        for q in range(4):
            for j in range(4):
                kc = q * 4 + j
                fw.tr(pA[:, j * 128:(j + 1) * 128], h2f[:, kc * 128:(kc + 1) * 128], C["ident_f"][:],
                      reads=[d_h2f, C["dep"]], write=dA)
            fw.op(fw.act, lambda h, q=q: h.activation(out=h2fT[:, q * 4:(q + 1) * 4, :],
                                                      in_=pA[:, 0:512].rearrange("p (a b) -> p a b", a=4), func=AF.Copy),
                  reads=[dA], writes=[d_h2fT])
        fw.mm(pB[:, 0:68], [(h2fT[:, kc, :], w_r_sb[:, kc, :]) for kc in range(16)], reads=[d_h2fT, d_misc2], write=dB)
        lg = sm[:, 8:76]
        fw.op(fw.dve, lambda h: h.tensor_tensor(out=lg, in0=pB[:, 0:68], in1=b_r_sb[:], op=ALU.add),
              reads=[dB, d_misc2], writes=[d_small])
        R_ = [d_small]

        def dv(fn):
            fw.op(fw.dve, fn, reads=R_, writes=R_)

        def ac(fn):
            fw.op(fw.act, fn, reads=R_, writes=R_)
        gl = sm[:, 8:12]
        el = sm[:, 12:76]
        m64 = sm[:, 80:144]
        m64b = sm[:, 144:208]
        dv(lambda h: h.reduce_max(out=col(2), in_=gl, axis=AX.X))
        dv(lambda h: h.tensor_scalar(out=sm[:, 76:80], in0=gl, scalar1=col(2), scalar2=None, op0=ALU.is_equal))
        dv(lambda h: h.tensor_scalar(out=col(3), in0=col(2), scalar1=-1.0, scalar2=None, op0=ALU.mult))
        ac(lambda h: h.activation(out=sm[:, 208:212], in_=gl, func=AF.Exp, bias=col(3), scale=1.0, accum_out=col(4)))
        dv(lambda h: h.reciprocal(out=col(4), in_=col(4)))
        dv(lambda h: h.tensor_scalar(out=sm[:, 76:80], in0=sm[:, 76:80], scalar1=1e9, scalar2=-1e9,
                                     op0=ALU.mult, op1=ALU.add))
        for g in range(4):
            dv(lambda h, g=g: h.tensor_scalar(out=m64[:, g * 16:(g + 1) * 16], in0=el[:, g * 16:(g + 1) * 16],
                                              scalar1=sm[:, 76 + g:77 + g], scalar2=None, op0=ALU.add))
        A1 = A_f[:, tc, :]
        W1 = Wt[:, tc, :]
        oh2 = sm[:, 144:208]
        dv(lambda h: h.reduce_max(out=col(5), in_=m64, axis=AX.X))
        fw.op(fw.dve, lambda h: h.tensor_scalar(out=A1, in0=m64, scalar1=col(5), scalar2=None, op0=ALU.is_equal),
              reads=R_, writes=R_ + [rt_dep])
        dv(lambda h: h.scalar_tensor_tensor(out=m64b, in0=A1, scalar=-1e9, in1=m64, op0=ALU.mult, op1=ALU.add))
        dv(lambda h: h.reduce_max(out=col(6), in_=m64b, axis=AX.X))
        dv(lambda h: h.tensor_scalar(out=oh2, in0=m64b, scalar1=col(6), scalar2=None, op0=ALU.is_equal))
        dv(lambda h: h.tensor_tensor(out=col(7), in0=col(6), in1=col(5), op=ALU.subtract))
        ac(lambda h: h.activation(out=col(7), in_=col(7), func=AF.Exp))
        dv(lambda h: h.tensor_scalar(out=col(2), in0=col(7), scalar1=1.0, scalar2=None, op0=ALU.add))
        dv(lambda h: h.reciprocal(out=col(2), in_=col(2)))
        dv(lambda h: h.tensor_tensor(out=col(2), in0=col(2), in1=col(4), op=ALU.mult))
        dv(lambda h: h.tensor_tensor(out=col(3), in0=col(2), in1=col(7), op=ALU.mult))
        fw.op(fw.dve, lambda h: h.tensor_scalar(out=W1, in0=A1, scalar1=col(2), scalar2=None, op0=ALU.mult),
              reads=R_ + [rt_dep], writes=R_ + [rt_dep])
        fw.op(fw.dve, lambda h: h.scalar_tensor_tensor(out=W1, in0=oh2, scalar=col(3), in1=W1, op0=ALU.mult, op1=ALU.add),
              reads=R_ + [rt_dep], writes=R_ + [rt_dep])
        fw.op(fw.dve, lambda h: h.tensor_tensor(out=A1, in0=A1, in1=oh2, op=ALU.add),
              reads=R_ + [rt_dep], writes=R_ + [rt_dep])

    A_b = nc.alloc_sbuf_tensor_at("A_b_alias", [128, NCH, NEXP], BF16, offset=off_h2fT)
    fw.op(fw.dve, lambda h: h.tensor_copy(out=A_b[:], in_=A_f[:]), reads=[rt_dep], writes=[rt_dep, d_h2fT])
    for tc in range(NCH):
        pairs = [(C["ones"][:], A_b[:, t2, :]) for t2 in range(tc)] + [(C["ustrict"][:], A_b[:, tc, :])]
        fw.mm(pB[:, 0:64], pairs, reads=[rt_dep, C["dep"]], write=dB)
        fw.op(fw.dve, lambda h: h.tensor_scalar(out=rankp[:, tc, :], in0=A_f[:, tc, :], scalar1=-1e6, scalar2=1e6,
                                                op0=ALU.mult, op1=ALU.add), reads=[rt_dep], writes=[rt_dep])
        fw.op(fw.dve, lambda h: h.tensor_tensor(out=rankp[:, tc, :], in0=rankp[:, tc, :], in1=pB[:, 0:64], op=ALU.add),
              reads=[rt_dep, dB], writes=[rt_dep])

    fw.barrier()
    fw.release(m0)

    ybuf = fw.sb([128, EB_, D], BF16, "ybuf")
    selwt = fw.sb([128, EB_, NTOK], BF16, "selwt")
    sel = fw.sb([128, NCH, CAP], BF16, "sel")
    selw = fw.sb([128, NCH, CAP], BF16, "selw")
    xgT = fw.sb([128, 16, CAP], BF16, "xgT")
    hid = fw.sb([128, DEXP], BF16, "hid")
    sg = fw.sb([128, 384], F32, "sg")
    hidT = fw.sb([128, 6, CAP], BF16, "hidT")
    d_y = [Dep() for _ in range(EB_)]
    d_swt = [Dep() for _ in range(EB_)]
    d_sel, d_selw, d_xgT, d_hid, d_sg, d_hidT = (Dep() for _ in range(6))
    pD = [(pD0, dD0), (pD1, dD1)]
    iota = C["iota_f"]

    xgT2 = fw.sb([128, 16, CAP], BF16, "xgT2")
    xg = [(xgT, d_xgT), (xgT2, Dep())]
    gps = [(pP, dP), (pC, dC)]
    gcount = [0]

    def stage_sel(e):
        for tc in range(NCH):
            fw.op(fw.dve, lambda h, tc=tc: h.tensor_scalar(out=sel[:, tc, :], in0=iota[:], scalar1=rankp[:, tc, e:e + 1],
                                                           scalar2=None, op0=ALU.is_equal),
                  reads=[C["dep"], rt_dep], writes=[d_sel])
        for tc in range(NCH):
            fw.op(fw.dve, lambda h, tc=tc: h.tensor_scalar(out=selw[:, tc, :], in0=iota[:], scalar1=rankp[:, tc, e:e + 1],
                                                           scalar2=Wt[:, tc, e:e + 1], op0=ALU.is_equal, op1=ALU.mult),
                  reads=[C["dep"], rt_dep], writes=[d_selw])

    def stage_gather(e):
        xt, xd = xg[e % 2]
        for q in range(4):
            ps, dps = gps[gcount[0] % 2]
            gcount[0] += 1
            for j in range(4):
                fc = q * 4 + j
                fw.mm(ps[:, j * 128:(j + 1) * 128],
                      [(h2[:, tc, fc * 128:(fc + 1) * 128], sel[:, tc, :]) for tc in range(NCH)],
                      reads=h2_dep + [d_sel], write=dps)
            fw.op(fw.act, lambda h, q=q, ps=ps: h.activation(out=xt[:, q * 4:(q + 1) * 4, :],
                                                             in_=ps[:, 0:512].rearrange("p (a b) -> p a b", a=4), func=AF.Copy),
                  reads=[dps], writes=[xd])

    def stage_selT(e):
        eb = e % EB_
        for tc in range(NCH):
            fw.tr(pT1[:, tc * 128:(tc + 1) * 128], selw[:, tc, :], C["ident"][:], reads=[d_selw, C["dep"]], write=dT1)
        fw.op(fw.act, lambda h: h.activation(out=selwt[:, eb, :], in_=pT1[:, :], func=AF.Copy),
              reads=[dT1], writes=[d_swt[eb]])

    def stage_gateup(e):
        xt, xd = xg[e % 2]
        for c in range(2):
            wgt, wgd = stream.use()
            wgv = wgt[:, 0:6144].rearrange("p (k n) -> p k n", k=16)
            fw.mm(pA[:, 0:384], [(xt[:, kc, :], wgv[:, kc, :]) for kc in range(16)], reads=[xd, wgd], write=dA)
            stream.done()
            wut, wud = stream.use()
            wuv = wut[:, 0:6144].rearrange("p (k n) -> p k n", k=16)
            fw.mm(pB[:, 0:384], [(xt[:, kc, :], wuv[:, kc, :]) for kc in range(16)], reads=[xd, wud], write=dB)
            stream.done()
            fw.op(fw.act, lambda h: h.activation(out=sg[:], in_=pA[:, 0:384], func=AF.Silu), reads=[dA], writes=[d_sg])
            fw.op(fw.dve, lambda h, c=c: h.tensor_tensor(out=hid[:, c * 384:(c + 1) * 384], in0=pB[:, 0:384], in1=sg[:],
                                                         op=ALU.mult), reads=[dB, d_sg], writes=[d_hid])

    def stage_hidT(e):
        for j in range(6):
            fw.tr(pT2[:, j * 128:(j + 1) * 128], hid[:, j * 128:(j + 1) * 128], C["ident"][:], reads=[d_hid, C["dep"]], write=dT2)
        fw.op(fw.act, lambda h: h.activation(out=hidT[:], in_=pT2[:, 0:768].rearrange("p (a b) -> p a b", a=6), func=AF.Copy),
              reads=[dT2], writes=[d_hidT])

    def stage_down(e):
        eb = e % EB_
        for c in range(2):
            wdt, wdd = stream.use()
            wdv = wdt[:, 0:6144].rearrange("p (k n) -> p k n", k=6)
            for j in range(2):
                pd, dd = pD[j]
                fw.mm(pd[:, 0:512], [(hidT[:, kc, :], wdv[:, kc, j * 512:(j + 1) * 512]) for kc in range(6)],
                      reads=[d_hidT, wdd], write=dd)
                cs = slice(c * 1024 + j * 512, c * 1024 + (j + 1) * 512)
                fw.op(fw.dve, lambda h, cs=cs, pd=pd: h.tensor_tensor(out=ybuf[:, eb, cs], in0=pd[:, 0:512], in1=gate2b[:, cs],
                                                                      op=ALU.mult),
                      reads=[dd, gate2_dep], writes=[d_y[eb]])
            stream.done()

    def stage_combine():
        for tc in range(NCH):
            for ft in range(4):
                cs = slice(ft * 512, (ft + 1) * 512)
                ps, dps = gps[gcount[0] % 2]
                gcount[0] += 1
                fw.mm(ps[:, 0:512], [(selwt[:, b_, tc * 128:(tc + 1) * 128], ybuf[:, b_, cs]) for b_ in range(EB_)],
                      reads=d_swt + d_y, write=dps)
                fw.op(fw.dve, lambda h, tc=tc, cs=cs, ps=ps: h.tensor_tensor(out=x_res[:, tc, cs], in0=ps[:, 0:512],
                                                                             in1=x_res[:, tc, cs], op=ALU.add),
                      reads=[dps, x_dep[tc]], writes=[x_dep[tc]])

    stage_sel(0)
    stage_gather(0)
    stage_selT(0)
    for e in range(NEXP):
        stage_gateup(e)
        if e + 1 < NEXP:
            stage_sel(e + 1)
            stage_gather(e + 1)
        stage_hidT(e)
        stage_down(e)
        if e % EB_ == EB_ - 1:
            stage_combine()
        if e + 1 < NEXP:
            stage_selT(e + 1)

    os_ = fw.new_dsem("out")
    ov = x_out.rearrange("(c p) f -> p c f", p=128)
    ev = None
    for tc in range(NCH):
        ev = fw.dma(fw.sp, os_, ov[:, tc, :], x_res[:, tc, :], reads=[x_dep[tc]])
    fw.sp.wait(ev)
    return nc


def _const_inputs():
    i = np.arange(128)
    return {
        "ident_f": np.eye(128, dtype=np.float32),
        "ident": np.eye(128, dtype=np.float32),
        "iota_f": np.broadcast_to(i[None, :], (128, 128)).astype(np.float32).copy(),
        "ustrict": (i[:, None] < i[None, :]).astype(np.float32),
        "ones": np.ones((128, 128), np.float32),
        "tri": (i[None, :] >= i[:, None]).astype(np.float32),
    }


def _rep(v, n=128):
    return np.ascontiguousarray(np.broadcast_to(np.asarray(v, np.float32)[None, :], (n, v.shape[-1])))


_NC = {}


def run_moe(x_cur, oT_full, w_out, c, ada_w_l, ada_b_l, g2, w_rg, b_rg, w_re, b_re, wg, wu, wd):
    H = oT_full.shape[1]
    key = "moe%d" % H
    if key not in _NC:
        _NC[key] = build_moe(H)
    nc = _NC[key]
    consts = _const_inputs()
    ada_w2 = np.ascontiguousarray(ada_w_l[:, 2 * D:6 * D])
    ada_b2 = _rep(ada_b_l[2 * D:6 * D])
    g_bc = _rep(g2)
    w_r = np.ascontiguousarray(np.concatenate([w_rg, w_re], axis=1))
    b_r = _rep(np.concatenate([b_rg, b_re]))
    w_out = np.ascontiguousarray(w_out)
    wg = np.ascontiguousarray(wg)
    wu = np.ascontiguousarray(wu)
    wd = np.ascontiguousarray(wd)
    in_maps = []
    for core in range(8):
        b, p = core // 2, core % 2
        m = dict(consts)
        m.update({
            "x_in": np.ascontiguousarray(x_cur[b, p * NTOK:(p + 1) * NTOK, :]),
            "cT": np.ascontiguousarray(c[b].reshape(16, 128).T),
            "oT_in": np.ascontiguousarray(oT_full[b, :, :, p * NTOK:(p + 1) * NTOK]),
            "w_out": w_out,
            "ada_w": ada_w2, "ada_b": ada_b2, "g_bc": g_bc, "w_r": w_r, "b_r": b_r,
            "wg": wg, "wu": wu, "wd": wd,
        })
        in_maps.append(m)
    res = run_bass_kernel_spmd(nc, in_maps, core_ids=list(range(8)))
    out = np.empty_like(x_cur)
    for core in range(8):
        b, p = core // 2, core % 2
        out[b, p * NTOK:(p + 1) * NTOK, :] = res.results[core]["x_out"]
    return out


def _attn_prologue(nc, fw, C, stream, x_in, cT, ada_b, g_bc, hT, pP, dP, pT, dT):
    m0 = fw.mark()
    crep = fw.sb([128, 16, 128], BF16, "crep")
    cact = fw.sb([128, 16], F32, "cact")
    bst = [(fw.sb([128, 512], F32, "abias"), Dep(), fw.new_dsem("ab")) for _ in range(2)]
    gst = [(fw.sb([128, 512], F32, "gst"), Dep(), fw.new_dsem("gst")) for _ in range(2)]
    gs1 = fw.sb([128, D], F32, "gs1")
    sh1 = fw.sb([128, D], F32, "sh1")
    hf = fw.sb([128, D], F32, "hf")
    hb = fw.sb([128, D], BF16, "hb")
    small = fw.sb([128, 8], F32, "small")
    xst = [(fw.sb([128, D], F32, "xst"), Dep(), fw.new_dsem("xst")) for _ in range(2)]
    d_crep, d_misc, mod_dep, d_hf, d_hb, d_small = (Dep() for _ in range(6))
    ms = fw.new_dsem("misc")
    fw.dma(fw.sp, ms, cact[:], cT, writes=[d_misc], nowait=True)
    fw.op(fw.act, lambda h: h.activation(out=cact[:], in_=cact[:], func=AF.Silu), reads=[d_misc], writes=[d_misc])
    for kc in range(16):
        fw.op(fw.dve, lambda h, kc=kc: h.tensor_scalar(out=crep[:, kc, :], in0=C["ones_f32"][:], scalar1=cact[:, kc:kc + 1],
                                                        scalar2=None, op0=ALU.mult),
              reads=[d_misc, C["dep"]], writes=[d_crep])

    def consume(j, ps, bt, bd):
        q = j // 4
        cs = slice((j % 4) * 512, (j % 4 + 1) * 512)
        if q == 0:
            fw.op(fw.dve, lambda h: h.tensor_tensor(out=sh1[:, cs], in0=ps, in1=bt[:], op=ALU.add),
                  reads=[dP, bd], writes=[mod_dep])
        else:
            gt, gd, gsem = gst[j % 2]
            fw.dma(fw.sp, gsem, gt[:], g_bc[:, cs], writes=[gd])
            fw.op(fw.dve, lambda h: h.tensor_tensor(out=gs1[:, cs], in0=ps, in1=bt[:], op=ALU.add),
                  reads=[dP, bd], writes=[mod_dep])
            fw.op(fw.dve, lambda h: h.scalar_tensor_tensor(out=gs1[:, cs], in0=gs1[:, cs], scalar=1.0, in1=gt[:],
                                                           op0=ALU.add, op1=ALU.mult),
                  reads=[gd, mod_dep], writes=[mod_dep])
    _adaln(fw, stream, pP, dP, crep, d_crep, range(8), ada_b, consume, bst)

    hT_dep = Dep()
    for uc in range(16):
        xt, xd, xs = xst[uc % 2]
        fw.dma(fw.sp, xs, xt[:], x_in[uc * 128:(uc + 1) * 128, :], writes=[xd])
        fw.op(fw.dve, lambda h: h.memset(small[:, 0:2], 0.0), reads=[d_small], writes=[d_small])
        fw.op(fw.act, lambda h: h.activation(out=hf[:], in_=xt[:], func=AF.Square, accum_out=small[:, 0:1]),
              reads=[xd, d_small], writes=[d_hf, d_small])
        fw.op(fw.act, lambda h: h.activation(out=small[:, 1:2], in_=small[:, 0:1], func=AF.Sqrt, scale=1.0 / D, bias=EPS),
              reads=[d_small], writes=[d_small])
        fw.op(fw.dve, lambda h: h.reciprocal(out=small[:, 1:2], in_=small[:, 1:2]), reads=[d_small], writes=[d_small])
        fw.op(fw.dve, lambda h: h.scalar_tensor_tensor(out=hf[:], in0=xt[:], scalar=small[:, 1:2], in1=gs1[:],
                                                       op0=ALU.mult, op1=ALU.mult),
              reads=[xd, d_small, mod_dep, d_hf], writes=[d_hf])
        fw.op(fw.dve, lambda h: h.tensor_tensor(out=hb[:], in0=hf[:], in1=sh1[:], op=ALU.add),
              reads=[d_hf, mod_dep], writes=[d_hb])
        for q in range(2):
            for j in range(8):
                kc = q * 8 + j
                fw.tr(pT[:, j * 128:(j + 1) * 128], hb[:, kc * 128:(kc + 1) * 128], C["ident"][:],
                      reads=[d_hb, C["dep"]], write=dT)
            fw.op(fw.act, lambda h, q=q: h.activation(out=hT[:, q * 8:(q + 1) * 8, uc * 128:(uc + 1) * 128],
                                                      in_=pT[:, :].rearrange("p (a b) -> p a b", a=8), func=AF.Copy),
                  reads=[dT], writes=[hT_dep])
    fw.barrier()
    fw.release(m0)


S_ = 2048
DIL = (1, 4, 16)


def build_dsa():
    nc = bass.Bass("TRN2", target_bir_lowering=False)
    fw = FW(nc)
    dt = nc.dram_tensor
    x_in = dt("x_in", [S_, D], F32, kind="ExternalInput").ap()
    cT = dt("cT", [128, 16], F32, kind="ExternalInput").ap()
    ada_w = dt("ada_w", [D, 2 * D], F32, kind="ExternalInput").ap()
    ada_b = dt("ada_b", [128, 2 * D], F32, kind="ExternalInput").ap()
    g_bc = dt("g_bc", [128, D], F32, kind="ExternalInput").ap()
    w_perm = dt("w_perm", [4, D, 1152], F32, kind="ExternalInput").ap()
    qk_gain = dt("qk_gain", [128, 2], F32, kind="ExternalInput").ap()
    eb_in = dt("eb", [128, 12 * 256], F32, kind="ExternalInput").ap()
    cdr = {n: dt(n, [128, 128], F32, kind="ExternalInput").ap()
           for n in ("ident_f", "iota_f", "ident", "ustrict", "ones", "tri")}
    oT_out = dt("oT", [4, 128, S_], F32, kind="ExternalOutput").ap()

    pP = nc.alloc_psum_tensor("pP", [128, 512], F32)
    pSS = nc.alloc_psum_tensor("pSS", [128, 512], F32)
    pS0 = nc.alloc_psum_tensor("pS0", [128, 512], F32)
    pS1 = nc.alloc_psum_tensor("pS1", [128, 512], F32)
    pT = nc.alloc_psum_tensor("pT", [128, 1024], BF16)
    pV = nc.alloc_psum_tensor("pV", [128, 512], F32)
    pO = nc.alloc_psum_tensor("pO", [128, 512], F32)
    pDn = nc.alloc_psum_tensor("pDn", [128, 512], F32)
    dP, dSS, dS0, dS1, dT, dV, dO, dDn = (Dep() for _ in range(8))

    hT = fw.sb([128, 16, S_], BF16, "hT")
    ring = [(fw.sb([128, SLOT_ELEMS], BF16, "ring"), Dep(), fw.new_dsem("ring")) for _ in range(NSLOT)]
    C = _consts(fw, cdr)
    eb = fw.sb([128, 12, 2, 128], F32, "eb")
    gains = fw.sb([128, 2], F32, "gains")
    d_c2 = Dep()
    cs2 = fw.new_dsem("c2")
    fw.dma(fw.sp, cs2, eb[:], eb_in.rearrange("p (a v q) -> p a v q", a=12, v=2), writes=[d_c2], nowait=True)
    fw.dma(fw.sp, cs2, gains[:], qk_gain, writes=[d_c2], nowait=True)

    units = []
    for j in range(8):
        units.append(lambda t, j=j: [(t[:, 0:8192].rearrange("p (k n) -> p k n", k=16),
                                      ada_w[:, j * 512:(j + 1) * 512].rearrange("(k p) n -> p k n", p=128))])
    for sl in range(4):
        for g in range(3):
            units.append(lambda t, sl=sl, g=g: [(t[:, 0:6144].rearrange("p (k n) -> p k n", k=16),
                                                 w_perm[sl, :, g * 384:(g + 1) * 384].rearrange("(k p) n -> p k n", p=128))])
    stream = Stream(fw, ring, units)
    stream.prime()

    _attn_prologue(nc, fw, C, stream, x_in, cT, ada_b, g_bc, hT, pP, dP, pT, dT)

    QT = fw.sb([128, S_], BF16, "QT")
    KT = fw.sb([128, S_], BF16, "KT")
    V = fw.sb([128, 16, 128], BF16, "V")
    sq = fw.sb([128, 512], BF16, "sq")
    rstd = fw.sb([128, 512], F32, "rstd")
    expS = [fw.sb([128, 256], F32, "expS") for _ in range(2)]
    PT = [fw.sb([128, 256], BF16, "PT") for _ in range(2)]
    Oacc = fw.sb([128, 2, 1024], F32, "Oacc")
    Dacc = fw.sb([128, 2, 1024], F32, "Dacc")
    oTs = fw.sb([128, 1024], F32, "oTs")
    d_QT, d_KT, d_V, d_sq, d_rstd, d_acc, d_oTs = (Dep() for _ in range(7))
    d_expS = [Dep(), Dep()]
    d_PT = [Dep(), Dep()]
    os_ = fw.new_dsem("out")
    SC = 1.0 / float(np.sqrt(128.0))
    nblk = 0
    last_ev = None

    pbufs = [(pP, dP), (pT[:, :].bitcast(F32), dT)]
    npb = [0]

    def flush(g, b, qh):
        d = DIL[g]
        for (ps, dps, acc) in ((pO, dO, Oacc), (pDn, dDn, Dacc)):
            if d == 1:
                ov = acc[:, qh, b * 512:(b + 1) * 512]
                iv = ps[:, 0:512]
            else:
                ov = acc[:, qh, :].rearrange("e (m r) -> e m r", r=d)[:, :, b * d // 2:(b + 1) * d // 2]
                iv = ps[:, 0:512].rearrange("e (r m) -> e m r", r=d // 2)
            if g == 0:
                fw.op(fw.dve, lambda h: h.tensor_copy(out=ov, in_=iv), reads=[dps], writes=[d_acc])
            else:
                fw.op(fw.dve, lambda h: h.tensor_tensor(out=ov, in0=ov, in1=iv, op=ALU.add), reads=[dps, d_acc], writes=[d_acc])

    for sl in range(4):
        for g in range(3):
            d = DIL[g]
            w, wdp = stream.use()
            wv = w[:, 0:6144].rearrange("p (k n) -> p k n", k=16)
            for which, dst, ddst, gcol in ((0, QT, d_QT, 0), (1, KT, d_KT, 1)):
                dview = dst[:, :].rearrange("e (r m) -> e m r", r=d)
                for i in range(4):
                    pq, dpq = pbufs[npb[0] % 2]
                    npb[0] += 1
                    fw.mm(pq[:, 0:512], [(wv[:, kc, which * 128:(which + 1) * 128], hT[:, kc, i * 512:(i + 1) * 512])
                                         for kc in range(16)], reads=[wdp], write=dpq)
                    fw.op(fw.act, lambda h: h.activation(out=sq[:], in_=pq[:, 0:512], func=AF.Square),
                          reads=[dpq], writes=[d_sq])
                    fw.mm(pSS[:, 0:512], [(C["ones"][:], sq[:])], reads=[d_sq, C["dep"]], write=dSS)
                    fw.op(fw.act, lambda h: h.activation(out=rstd[:], in_=pSS[:, 0:512], func=AF.Ln, scale=1.0 / 128.0, bias=EPS),
                          reads=[dSS], writes=[d_rstd])
                    fw.op(fw.act, lambda h: h.activation(out=rstd[:], in_=rstd[:], func=AF.Exp, scale=-0.5),
                          reads=[d_rstd], writes=[d_rstd])
                    fw.op(fw.dve, lambda h, i=i, dview=dview, gcol=gcol, pq=pq: h.scalar_tensor_tensor(
                        out=dview[:, i * 512 // d:(i + 1) * 512 // d, :],
                        in0=pq[:, 0:512].rearrange("e (m r) -> e m r", r=d), scalar=gains[:, gcol:gcol + 1],
                        in1=rstd[:, :].rearrange("e (m r) -> e m r", r=d), op0=ALU.mult, op1=ALU.mult),
                        reads=[dpq, d_rstd, d_c2], writes=[ddst])
            nb = 16 // d
            for vb in range(16):
                r, mb = divmod(vb, nb)
                st = mb * 128 * d + r
                fw.mm(pV[:, (vb % 4) * 128:(vb % 4 + 1) * 128],
                      [(hT[:, kc, st:st + 127 * d + 1:d], wv[:, kc, 256:384]) for kc in range(16)], reads=[wdp], write=dV)
                if vb % 4 == 3:
                    fw.op(fw.act, lambda h, vb=vb: h.activation(out=V[:, vb - 3:vb + 1, :],
                                                                in_=pV[:, 0:512].rearrange("p (a b) -> p a b", a=4), func=AF.Copy),
                          reads=[dV], writes=[d_V])
            stream.done()
            ebi = sl * 3 + g
            for qh in range(2):
                blocks = []
                if g == 0:
                    for qb in range(8):
                        gb = 8 * qh + qb
                        blocks.append((gb * 128, 128, (gb - 1) * 128 if gb > 0 else None, gb * 128, 128, gb - 1, gb, qb * 128, 0))
                elif g == 1:
                    for r in range(4):
                        for j in range(2):
                            mb = 2 * qh + j
                            col = r * 512 + mb * 128
                            blocks.append((col, 128, col - 128 if mb > 0 else None, col, 128, r * 4 + mb - 1, r * 4 + mb,
                                           r * 256 + j * 128, 0))
                else:
                    for r in range(16):
                        nk = 64 if qh == 0 else 128
                        blocks.append((r * 128 + 64 * qh, 64, None, r * 128, nk, None, r, r * 64, 64 * qh))
                for (qcol, nq, pk, ck, nk, vp, vc, ocol, ebq0) in blocks:
                    pS, dS = (pS0, dS0) if nblk % 2 == 0 else (pS1, dS1)
                    eS, deS = expS[nblk % 2], d_expS[nblk % 2]
                    pt, dpt = PT[nblk % 2], d_PT[nblk % 2]
                    nblk += 1
                    qv = QT[:, qcol:qcol + nq]
                    if pk is not None:
                        fw.mm(pS[:, 0:nq], [(KT[:, pk:pk + 128], qv)], reads=[d_QT, d_KT], write=dS)
                    fw.mm(pS[0:nk, 128:128 + nq], [(KT[:, ck:ck + nk], qv)], reads=[d_QT, d_KT], write=dS)
                    if pk is not None:
                        fw.op(fw.act, lambda h: h.activation(out=eS[:, 0:256], in_=pS[:, 0:256], func=AF.Exp, scale=SC),
                              reads=[dS], writes=[deS])
                        fw.op(fw.dve, lambda h: h.tensor_tensor(out=pt[:, 0:256], in0=eS[:, 0:256],
                                                                in1=eb[:, ebi, :, :].rearrange("p v q -> p (v q)"), op=ALU.mult),
                              reads=[deS, d_c2], writes=[dpt])
                        pairs_o = [(V[:, vp, :], pt[:, 0:128]), (V[:, vc, :], pt[:, 128:256])]
                        pairs_d = [(C["ones"][:], pt[:, 0:128]), (C["ones"][:], pt[:, 128:256])]
                    else:
                        fw.op(fw.act, lambda h: h.activation(out=eS[0:nk, 128:128 + nq], in_=pS[0:nk, 128:128 + nq],
                                                             func=AF.Exp, scale=SC), reads=[dS], writes=[deS])
                        fw.op(fw.dve, lambda h: h.tensor_tensor(out=pt[0:nk, 128:128 + nq], in0=eS[0:nk, 128:128 + nq],
                                                                in1=eb[0:nk, ebi, 1, ebq0:ebq0 + nq], op=ALU.mult),
                              reads=[deS, d_c2], writes=[dpt])
                        pairs_o = [(V[0:nk, vc, :], pt[0:nk, 128:128 + nq])]
                        pairs_d = [(C["ones"][0:nk, :], pt[0:nk, 128:128 + nq])]
                    if ocol == 512:
                        flush(g, 0, qh)
                    oc = ocol % 512
                    fw.mm(pO[:, oc:oc + nq], pairs_o, reads=[d_V, dpt], write=dO)
                    fw.mm(pDn[:, oc:oc + nq], pairs_d, reads=[dpt, C["dep"]], write=dDn)
                flush(g, 1, qh)
                if g == 2:
                    fw.op(fw.dve, lambda h: h.reciprocal(out=Dacc[:, qh, :], in_=Dacc[:, qh, :]), reads=[d_acc], writes=[d_acc])
                    fw.op(fw.dve, lambda h: h.tensor_tensor(out=oTs[:], in0=Oacc[:, qh, :], in1=Dacc[:, qh, :], op=ALU.mult),
                          reads=[d_acc], writes=[d_oTs])
                    last_ev = fw.dma(fw.sp, os_, oT_out[sl, :, qh * 1024:(qh + 1) * 1024], oTs[:], reads=[d_oTs])
    fw.sp.wait(last_ev)
    return nc


def _alibi_eb(slots):
    k = np.arange(128)[:, None].astype(np.float64)
    q = np.arange(128)[None, :].astype(np.float64)
    out = np.zeros((128, len(slots) * 3, 2, 128), np.float32)
    for i, s in enumerate(slots):
        for g, d in enumerate(DIL):
            slope = 2.0 ** (-8.0 * (s * 3 + g + 1.0) / 24.0)
            dp = q + 128 - k
            out[:, i * 3 + g, 0, :] = np.where(dp <= 128, np.exp(-slope * d * dp), 0.0)
            dc = q - k
            out[:, i * 3 + g, 1, :] = np.where(dc >= 0, np.exp(-slope * d * np.maximum(dc, 0)), 0.0)
    return out.reshape(128, -1)


def run_dsa(x_cur, c, ada_w_l, ada_b_l, g1, w_in, q_gain, k_gain):
    if "dsa" not in _NC:
        _NC["dsa"] = build_dsa()
    nc = _NC["dsa"]
    consts = _const_inputs()
    ada_w1 = np.ascontiguousarray(ada_w_l[:, 0:2 * D])
    ada_b1 = _rep(ada_b_l[0:2 * D])
    g_bc = _rep(g1)
    wp = np.ascontiguousarray(w_in.reshape(D, 3, 3, 8, 128).transpose(3, 0, 1, 2, 4).reshape(8, D, 1152))
    gains = np.ascontiguousarray(np.stack([q_gain, k_gain], axis=1).astype(np.float32))
    in_maps = []
    for core in range(8):
        b, p = core // 2, core % 2
        m = dict(consts)
        m.update({"x_in": np.ascontiguousarray(x_cur[b]), "cT": np.ascontiguousarray(c[b].reshape(16, 128).T),
                  "ada_w": ada_w1, "ada_b": ada_b1, "g_bc": g_bc,
                  "w_perm": np.ascontiguousarray(wp[4 * p:4 * p + 4]), "qk_gain": gains,
                  "eb": _alibi_eb(range(4 * p, 4 * p + 4))})
        in_maps.append(m)
    res = run_bass_kernel_spmd(nc, in_maps, core_ids=list(range(8)))
    oT = np.empty((4, 8, 128, S_), np.float32)
    for core in range(8):
        b, p = core // 2, core % 2
        oT[b, 4 * p:4 * p + 4] = res.results[core]["oT"]
    return oT


def build_mla():
    nc = bass.Bass("TRN2", target_bir_lowering=False)
    fw = FW(nc)
    dt = nc.dram_tensor
    x_in = dt("x_in", [S_, D], F32, kind="ExternalInput").ap()
    cT = dt("cT", [128, 16], F32, kind="ExternalInput").ap()
    ada_w = dt("ada_w", [D, 2 * D], F32, kind="ExternalInput").ap()
    ada_b = dt("ada_b", [128, 2 * D], F32, kind="ExternalInput").ap()
    g_bc = dt("g_bc", [128, D], F32, kind="ExternalInput").ap()
    w_in = dt("w_in", [D, 1088], F32, kind="ExternalInput").ap()
    lat_gain = dt("lat_gain", [128, 1024], F32, kind="ExternalInput").ap()
    w_q = dt("w_q", [512, 8 * 192], F32, kind="ExternalInput").ap()
    w_kv = dt("w_kv", [512, 8 * 256], F32, kind="ExternalInput").ap()
    hg_in = dt("hg", [128, 4], F32, kind="ExternalInput").ap()
    cs_in = dt("rope_cs", [64, S_], F32, kind="ExternalInput").ap()
    sn_in = dt("rope_sn", [64, S_], F32, kind="ExternalInput").ap()
    cdr = {n: dt(n, [128, 128], F32, kind="ExternalInput").ap()
           for n in ("ident_f", "iota_f", "ident", "ustrict", "ones", "tri")}
    oT_out = dt("oT", [8, 128, S_], F32, kind="ExternalOutput").ap()

    pP = nc.alloc_psum_tensor("pP", [128, 512], F32)
    pSS = nc.alloc_psum_tensor("pSS", [128, 512], F32)
    pS0 = nc.alloc_psum_tensor("pS0", [128, 512], F32)
    pS1 = nc.alloc_psum_tensor("pS1", [128, 512], F32)
    pO0 = nc.alloc_psum_tensor("pO0", [128, 512], F32)
    pO1 = nc.alloc_psum_tensor("pO1", [128, 512], F32)
    pD0 = nc.alloc_psum_tensor("pD0", [128, 512], F32)
    pT = nc.alloc_psum_tensor("pT", [128, 1024], BF16)
    pD1 = pT[:, :].bitcast(F32)
    dP, dSS, dS0, dS1, dO0, dO1, dD0, dT = (Dep() for _ in range(8))
    dD1 = dT

    ring = [(fw.sb([128, SLOT_ELEMS], BF16, "ring"), Dep(), fw.new_dsem("ring")) for _ in range(NSLOT)]
    C = _consts(fw, cdr)
    hg = fw.sb([128, 4], F32, "hg")
    d_c2 = Dep()
    cs2 = fw.new_dsem("c2")
    fw.dma(fw.sp, cs2, hg[:], hg_in, writes=[d_c2], nowait=True)
    fw.top -= 65536
    hT = nc.alloc_sbuf_tensor_at("hT_top", [128, 16, S_], BF16, offset=fw.top)

    units = []
    for j in range(8):
        units.append(lambda t, j=j: [(t[:, 0:8192].rearrange("p (k n) -> p k n", k=16),
                                      ada_w[:, j * 512:(j + 1) * 512].rearrange("(k p) n -> p k n", p=128))])
    for (c0, n) in ((0, 512), (512, 512), (1024, 64)):
        units.append(lambda t, c0=c0, n=n: [(t[:, 0:16 * n].rearrange("p (k n) -> p k n", k=16),
                                             w_in[:, c0:c0 + n].rearrange("(k p) n -> p k n", p=128))])
    for hd in range(8):
        units.append(lambda t, hd=hd: [
            (t[:, 0:768].rearrange("p (k n) -> p k n", k=4), w_q[:, hd * 192:(hd + 1) * 192].rearrange("(k p) n -> p k n", p=128)),
            (t[:, 768:1792].rearrange("p (k n) -> p k n", k=4), w_kv[:, hd * 256:(hd + 1) * 256].rearrange("(k p) n -> p k n", p=128))])
    stream = Stream(fw, ring, units)
    stream.prime()

    _attn_prologue(nc, fw, C, stream, x_in, cT, ada_b, g_bc, hT, pP, dP, pT, dT)

    cs = fw.sb([64, S_], F32, "cs")
    sn = fw.sb([64, S_], F32, "sn")
    fw.dma(fw.sp, cs2, cs[:], cs_in, writes=[d_c2], nowait=True)
    fw.dma(fw.sp, cs2, sn[:], sn_in, writes=[d_c2], nowait=True)
    cqT = fw.sb([128, 4, S_], BF16, "cqT")
    ckvT = fw.sb([128, 4, S_], BF16, "ckvT")
    kraw = fw.sb([64, S_], F32, "kraw")
    sqpe = fw.sb([64, S_], BF16, "sqpe")
    kper = fw.sb([64, S_], F32, "kper")
    ksw = fw.sb([64, S_], F32, "ksw")
    d_cq, d_ckv, d_kraw, d_kpe = (Dep() for _ in range(4))
    m1 = fw.mark()
    lg = fw.sb([128, 1024], F32, "lat_gain")
    fw.dma(fw.sp, cs2, lg[:], lat_gain, writes=[d_c2], nowait=True)
    tmpn = fw.sb([128, 512], BF16, "tmpn")
    junk = fw.sb([128, 512], F32, "junk")
    ktok = fw.sb([128, 64], F32, "ktok")
    small = fw.sb([128, 8], F32, "small")
    d_tmpn, d_junk, d_ktok, d_small = (Dep() for _ in range(4))
    wq_t, wq_d = stream.use()
    wkv_t, wkv_d = stream.use()
    wpe_t, wpe_d = stream.use()
    wq_v = wq_t[:, 0:8192].rearrange("p (k n) -> p k n", k=16)
    wkv_v = wkv_t[:, 0:8192].rearrange("p (k n) -> p k n", k=16)
    wpe_v = wpe_t[:, 0:1024].rearrange("p (k n) -> p k n", k=16)
    for uc in range(16):
        tok = slice(uc * 128, (uc + 1) * 128)
        for (wv_, wd_, dstT, ddst, goff) in ((wq_v, wq_d, cqT, d_cq, 0), (wkv_v, wkv_d, ckvT, d_ckv, 512)):
            fw.mm(pP[:, 0:512], [(hT[:, kc, tok], wv_[:, kc, :]) for kc in range(16)], reads=[wd_], write=dP)
            fw.op(fw.dve, lambda h: h.memset(small[:, 0:2], 0.0), reads=[d_small], writes=[d_small])
            fw.op(fw.act, lambda h: h.activation(out=junk[:], in_=pP[:, 0:512], func=AF.Square, accum_out=small[:, 0:1]),
                  reads=[dP, d_small], writes=[d_junk, d_small])
            fw.op(fw.act, lambda h: h.activation(out=small[:, 1:2], in_=small[:, 0:1], func=AF.Sqrt, scale=1.0 / 512.0, bias=EPS),
                  reads=[d_small], writes=[d_small])
            fw.op(fw.dve, lambda h: h.reciprocal(out=small[:, 1:2], in_=small[:, 1:2]), reads=[d_small], writes=[d_small])
            fw.op(fw.dve, lambda h, goff=goff: h.scalar_tensor_tensor(out=tmpn[:], in0=pP[:, 0:512], scalar=small[:, 1:2],
                                                                      in1=lg[:, goff:goff + 512], op0=ALU.mult, op1=ALU.mult),
                  reads=[dP, d_small, d_c2], writes=[d_tmpn])
            for j in range(4):
                fw.tr(pT[:, j * 128:(j + 1) * 128], tmpn[:, j * 128:(j + 1) * 128], C["ident"][:], reads=[d_tmpn, C["dep"]], write=dT)
            fw.op(fw.act, lambda h, dstT=dstT: h.activation(out=dstT[:, :, tok], in_=pT[:, 0:512].rearrange("p (a b) -> p a b", a=4),
                                                            func=AF.Copy), reads=[dT], writes=[ddst])
        if uc % 4 == 0 and uc > 0:
            pass
        fw.mm(pS0[:, 0:64], [(hT[:, kc, tok], wpe_v[:, kc, :]) for kc in range(16)], reads=[wpe_d], write=dS0)
        fw.op(fw.act, lambda h: h.activation(out=ktok[:], in_=pS0[:, 0:64], func=AF.Copy), reads=[dS0], writes=[d_ktok])
        fw.tr(pSS[0:64, 0:128], ktok[:, 0:64], C["ident_f"][:], reads=[d_ktok, C["dep"]], write=dSS)
        fw.op(fw.act, lambda h: h.activation(out=kraw[:, tok], in_=pSS[0:64, 0:128], func=AF.Copy), reads=[dSS], writes=[d_kraw])
    stream.done()
    stream.done()
    stream.done()
    fw.op(fw.act, lambda h: h.activation(out=sqpe[:], in_=kraw[:], func=AF.Square), reads=[d_kraw], writes=[d_kpe])
    fw.op(fw.dve, lambda h: h.tensor_scalar(out=kper[:], in0=kraw[:], scalar1=hg[0:64, 3:4], scalar2=None, op0=ALU.mult),
          reads=[d_kraw, d_c2], writes=[d_kpe])
    fw.op(fw.dve, lambda h: h.tensor_copy(out=ksw[0:32, :], in_=kper[32:64, :]), reads=[d_kpe], writes=[d_kpe])
    fw.op(fw.dve, lambda h: h.tensor_copy(out=ksw[32:64, :], in_=kper[0:32, :]), reads=[d_kpe], writes=[d_kpe])
    fw.op(fw.dve, lambda h: h.tensor_tensor(out=kper[:], in0=kper[:], in1=cs[:], op=ALU.mult), reads=[d_kpe, d_c2], writes=[d_kpe])
    fw.op(fw.dve, lambda h: h.tensor_tensor(out=ksw[:], in0=ksw[:], in1=sn[:], op=ALU.mult), reads=[d_kpe, d_c2], writes=[d_kpe])
    fw.op(fw.dve, lambda h: h.tensor_tensor(out=kper[:], in0=kper[:], in1=ksw[:], op=ALU.add), reads=[d_kpe], writes=[d_kpe])
    fw.barrier()
    fw.release(m1)
    fw.top += 65536

    QTn = fw.sb([128, S_], BF16, "QTn")
    QTr = fw.sb([64, S_], BF16, "QTr")
    KTn = fw.sb([128, S_], BF16, "KTn")
    KTr = fw.sb([64, S_], BF16, "KTr")
    V = fw.sb([128, 16, 128], BF16, "V")
    sqn = fw.sb([128, 512], BF16, "sqn")
    sqr = fw.sb([64, 512], BF16, "sqr")
    rstd = fw.sb([128, 512], F32, "rstd")
    qg = fw.sb([64, 512], F32, "qg")
    qsw = fw.sb([64, 512], F32, "qsw")
    PT = [fw.sb([128, 512], BF16, "PT") for _ in range(2)]
    rden = fw.sb([128, 1024], F32, "rden")
    oTs = fw.sb([128, 1024], F32, "oTs")
    d_QT, d_KT, d_V, d_sq, d_rstd, d_qg, d_rden, d_oTs = (Dep() for _ in range(8))
    d_PT = [Dep(), Dep()]
    os_ = fw.new_dsem("out")
    SCm = 1.0 / float(np.sqrt(192.0))
    ntile = 0
    last_ev = None
    pOb = [(pO0, dO0), (pO1, dO1)]
    pDb = [(pD0, dD0), (pD1, dD1)]
    pSb = [(pS0, dS0), (pS1, dS1)]

    for hd in range(8):
        w, wdp = stream.use()
        wqv = w[:, 0:768].rearrange("p (k n) -> p k n", k=4)
        wkvv = w[:, 768:1792].rearrange("p (k n) -> p k n", k=4)
        for i in range(4):
            cols = slice(i * 512, (i + 1) * 512)
            fw.mm(pP[:, 0:512], [(wqv[:, j, 0:128], cqT[:, j, cols]) for j in range(4)], reads=[wdp, d_cq], write=dP)
            fw.mm(pS1[0:64, 0:512], [(wqv[:, j, 128:192], cqT[:, j, cols]) for j in range(4)], reads=[wdp, d_cq], write=dS1)
            fw.op(fw.act, lambda h: h.activation(out=sqn[:], in_=pP[:, 0:512], func=AF.Square), reads=[dP], writes=[d_sq])
            fw.op(fw.act, lambda h: h.activation(out=sqr[:], in_=pS1[0:64, 0:512], func=AF.Square), reads=[dS1], writes=[d_sq])
            fw.mm(pSS[:, 0:512], [(C["ones"][:], sqn[:]), (C["ones"][0:64, :], sqr[:])], reads=[d_sq, C["dep"]], write=dSS)
            fw.op(fw.act, lambda h: h.activation(out=rstd[:], in_=pSS[:, 0:512], func=AF.Ln, scale=1.0 / 192.0, bias=EPS),
                  reads=[dSS], writes=[d_rstd])
            fw.op(fw.act, lambda h: h.activation(out=rstd[:], in_=rstd[:], func=AF.Exp, scale=-0.5), reads=[d_rstd], writes=[d_rstd])
            fw.op(fw.dve, lambda h: h.scalar_tensor_tensor(out=QTn[:, cols], in0=pP[:, 0:512], scalar=hg[:, 0:1], in1=rstd[:],
                                                           op0=ALU.mult, op1=ALU.mult), reads=[dP, d_rstd, d_c2], writes=[d_QT])
            fw.op(fw.dve, lambda h: h.scalar_tensor_tensor(out=qg[:], in0=pS1[0:64, 0:512], scalar=hg[0:64, 2:3], in1=rstd[0:64, :],
                                                           op0=ALU.mult, op1=ALU.mult), reads=[dS1, d_rstd, d_c2], writes=[d_qg])
            fw.op(fw.dve, lambda h: h.tensor_copy(out=qsw[0:32, :], in_=qg[32:64, :]), reads=[d_qg], writes=[d_qg])
            fw.op(fw.dve, lambda h: h.tensor_copy(out=qsw[32:64, :], in_=qg[0:32, :]), reads=[d_qg], writes=[d_qg])
            fw.op(fw.dve, lambda h: h.tensor_tensor(out=qg[:], in0=qg[:], in1=cs[:, cols], op=ALU.mult), reads=[d_qg, d_c2], writes=[d_qg])
            fw.op(fw.dve, lambda h: h.tensor_tensor(out=qsw[:], in0=qsw[:], in1=sn[:, cols], op=ALU.mult), reads=[d_qg, d_c2], writes=[d_qg])
            fw.op(fw.dve, lambda h: h.tensor_tensor(out=QTr[:, cols], in0=qg[:], in1=qsw[:], op=ALU.add), reads=[d_qg], writes=[d_QT])
            fw.mm(pP[:, 0:512], [(wkvv[:, j, 0:128], ckvT[:, j, cols]) for j in range(4)], reads=[wdp, d_ckv], write=dP)
            fw.op(fw.act, lambda h: h.activation(out=sqn[:], in_=pP[:, 0:512], func=AF.Square), reads=[dP], writes=[d_sq])
            fw.mm(pSS[:, 0:512], [(C["ones"][:], sqn[:]), (C["ones"][0:64, :], sqpe[:, cols])], reads=[d_sq, d_kpe, C["dep"]], write=dSS)
            fw.op(fw.act, lambda h: h.activation(out=rstd[:], in_=pSS[:, 0:512], func=AF.Ln, scale=1.0 / 192.0, bias=EPS),
                  reads=[dSS], writes=[d_rstd])
            fw.op(fw.act, lambda h: h.activation(out=rstd[:], in_=rstd[:], func=AF.Exp, scale=-0.5), reads=[d_rstd], writes=[d_rstd])
            fw.op(fw.dve, lambda h: h.scalar_tensor_tensor(out=KTn[:, cols], in0=pP[:, 0:512], scalar=hg[:, 1:2], in1=rstd[:],
                                                           op0=ALU.mult, op1=ALU.mult), reads=[dP, d_rstd, d_c2], writes=[d_KT])
            fw.op(fw.dve, lambda h: h.tensor_tensor(out=KTr[:, cols], in0=kper[:, cols], in1=rstd[0:64, :], op=ALU.mult),
                  reads=[d_kpe, d_rstd], writes=[d_KT])
        for uc in range(16):
            fw.mm(pS0[:, (uc % 4) * 128:(uc % 4 + 1) * 128],
                  [(ckvT[:, j, uc * 128:(uc + 1) * 128], wkvv[:, j, 128:256]) for j in range(4)], reads=[wdp, d_ckv], write=dS0)
            if uc % 4 == 3:
                fw.op(fw.act, lambda h, uc=uc: h.activation(out=V[:, uc - 3:uc + 1, :],
                                                            in_=pS0[:, 0:512].rearrange("p (a b) -> p a b", a=4), func=AF.Copy),
                      reads=[dS0], writes=[d_V])
        stream.done()
        for qh in range(2):
            nkb = 8 * qh + 8
            for kb in range(nkb):
                if kb < 8 * qh:
                    ranges, diag = [(0, 512), (512, 1024)], False
                else:
                    a = (kb - 8 * qh) * 128
                    ranges = ([(a, 512)] if a < 512 else []) + [(max(a, 512), 1024)]
                    diag = True
                for ri, (c0, c1) in enumerate(ranges):
                    nq = c1 - c0
                    pS, dS = pSb[ntile % 2]
                    pt, dpt = PT[ntile % 2], d_PT[ntile % 2]
                    ntile += 1
                    q0 = qh * 1024 + c0
                    kc_ = slice(kb * 128, (kb + 1) * 128)
                    fw.mm(pS[:, 0:nq], [(KTn[:, kc_], QTn[:, q0:q0 + nq]), (KTr[:, kc_], QTr[:, q0:q0 + nq])],
                          reads=[d_QT, d_KT], write=dS)
                    fw.op(fw.act, lambda h: h.activation(out=pt[:, 0:nq], in_=pS[:, 0:nq], func=AF.Exp, scale=SCm),
                          reads=[dS], writes=[dpt])
                    if diag and ri == 0:
                        fw.op(fw.dve, lambda h: h.tensor_tensor(out=pt[:, 0:128], in0=pt[:, 0:128], in1=C["tri"][:], op=ALU.mult),
                              reads=[dpt, C["dep"]], writes=[dpt])
                    bk = c0 // 512
                    oc = c0 % 512
                    last_kb = 8 * qh + (3 if bk == 0 else 7)
                    po, dpo = pOb[bk]
                    pd, dpd = pDb[bk]
                    fw.mm(po[:, oc:oc + nq], [(V[:, kb, :], pt[:, 0:nq])], reads=[d_V, dpt], write=dpo,
                          start=(kb == 0), stop=(kb == last_kb))
                    fw.mm(pd[:, oc:oc + nq], [(C["ones"][:], pt[:, 0:nq])], reads=[dpt, C["dep"]], write=dpd,
                          start=(kb == 0), stop=(kb == last_kb))
            for bk in range(2):
                po, dpo = pOb[bk]
                pd, dpd = pDb[bk]
                sl_ = slice(bk * 512, (bk + 1) * 512)
                fw.op(fw.dve, lambda h: h.reciprocal(out=rden[:, sl_], in_=pd[:, 0:512]), reads=[dpd], writes=[d_rden])
                fw.op(fw.dve, lambda h: h.tensor_tensor(out=oTs[:, sl_], in0=po[:, 0:512], in1=rden[:, sl_], op=ALU.mult),
                      reads=[dpo, d_rden], writes=[d_oTs])
            last_ev = fw.dma(fw.sp, os_, oT_out[hd, :, qh * 1024:(qh + 1) * 1024], oTs[:], reads=[d_oTs])
    fw.sp.wait(last_ev)
    return nc


def _rope_tables():
    half = 32
    inv = 10000.0 ** (-np.arange(half, dtype=np.float64) / half)
    ang = np.arange(S_, dtype=np.float64)[None, :] * inv[:, None]
    cs = np.concatenate([np.cos(ang), np.cos(ang)], axis=0).astype(np.float32)
    sn = np.concatenate([-np.sin(ang), np.sin(ang)], axis=0).astype(np.float32)
    return np.ascontiguousarray(cs), np.ascontiguousarray(sn)


def run_mla(x_cur, c, ada_w_l, ada_b_l, g1, w_in, cq_gain, ckv_gain, w_q_up, w_kv_up, q_gain, k_gain):
    if "mla" not in _NC:
        _NC["mla"] = build_mla()
    nc = _NC["mla"]
    consts = _const_inputs()
    ada_w1 = np.ascontiguousarray(ada_w_l[:, 0:2 * D])
    ada_b1 = _rep(ada_b_l[0:2 * D])
    g_bc = _rep(g1)
    lat_gain = _rep(np.concatenate([cq_gain, ckv_gain]))
    hg = np.zeros((128, 4), np.float32)
    hg[:, 0] = q_gain[0:128]
    hg[:, 1] = k_gain[0:128]
    hg[0:64, 2] = q_gain[128:192]
    hg[0:64, 3] = k_gain[128:192]
    cs, sn = _rope_tables()
    in_maps = []
    for core in range(8):
        b, p = core // 2, core % 2
        m = dict(consts)
        m.update({"x_in": np.ascontiguousarray(x_cur[b]), "cT": np.ascontiguousarray(c[b].reshape(16, 128).T),
                  "ada_w": ada_w1, "ada_b": ada_b1, "g_bc": g_bc, "w_in": np.ascontiguousarray(w_in),
                  "lat_gain": lat_gain,
                  "w_q": np.ascontiguousarray(w_q_up[:, p * 1536:(p + 1) * 1536]),
                  "w_kv": np.ascontiguousarray(w_kv_up[:, p * 2048:(p + 1) * 2048]),
                  "hg": hg, "rope_cs": cs, "rope_sn": sn})
        in_maps.append(m)
    res = run_bass_kernel_spmd(nc, in_maps, core_ids=list(range(8)))
    oT = np.empty((4, 16, 128, S_), np.float32)
    for core in range(8):
        b, p = core // 2, core % 2
        oT[b, 8 * p:8 * p + 8] = res.results[core]["oT"]
    return oT


def kernel(x, c, ada_w, ada_b, norm1_g, norm2_g, dsa_w_in, dsa_q_gain, dsa_k_gain, dsa_w_out, mla_w_in,
           mla_cq_gain, mla_ckv_gain, mla_w_q_up, mla_w_kv_up, mla_q_gain, mla_k_gain, mla_w_out,
           router_group_w, router_group_b, router_expert_w, router_expert_b, expert_w_gate, expert_w_up,
           expert_w_down):
    f = lambda a: np.asarray(a, dtype=np.float32)
    x = f(x); c = f(c); ada_w = f(ada_w); ada_b = f(ada_b)
    oT = run_dsa(x, c, ada_w[0], ada_b[0], f(norm1_g)[0], f(dsa_w_in)[0], f(dsa_q_gain)[0], f(dsa_k_gain)[0])
    x = run_moe(x, oT, f(dsa_w_out)[0], c, ada_w[0], ada_b[0], f(norm2_g)[0], f(router_group_w)[0], f(router_group_b)[0],
                f(router_expert_w)[0], f(router_expert_b)[0], f(expert_w_gate)[0], f(expert_w_up)[0], f(expert_w_down)[0])
    oT = run_mla(x, c, ada_w[1], ada_b[1], f(norm1_g)[1], f(mla_w_in)[0], f(mla_cq_gain)[0], f(mla_ckv_gain)[0],
                 f(mla_w_q_up)[0], f(mla_w_kv_up)[0], f(mla_q_gain)[0], f(mla_k_gain)[0])
    x = run_moe(x, oT, f(mla_w_out)[0], c, ada_w[1], ada_b[1], f(norm2_g)[1], f(router_group_w)[1], f(router_group_b)[1],
                f(router_expert_w)[1], f(router_expert_b)[1], f(expert_w_gate)[1], f(expert_w_up)[1], f(expert_w_down)[1])
    return x
```
